# Optimizing a Trainium2 kernel written in Bass

```python
import jax, jax.numpy as jnp
from jax import lax
import numpy as np

D_MODEL = 1024
BATCH = 2
SEQ = 8192
DEPTH = 4

GRID_W = 64
CTX_LEN = 256
N_BRANCH = 3
BRANCH_W = 512
POOL_WINDOWS = (2, 4, 8, 16)
POOL_GROUPS = 4
POOL_GW = BRANCH_W // POOL_GROUPS
RWKV_HEADS = 8
RWKV_HD = BRANCH_W // RWKV_HEADS
DECAY_LORA = 32
ICLR_LORA = 32
GATE_LORA = 96
GN_EPS = 64e-5
NAT_HEADS = 8
NAT_HD = BRANCH_W // NAT_HEADS
NAT_KH_MAX = 8
NAT_KW = 16
NAT_SCALE = NAT_HD ** -0.5
D_FF = 2816
N_EXPERTS = 8
TOP_K = 2
N_DENSE = (DEPTH + 1) // 2
N_MOE = DEPTH // 2
RMS_EPS = 1e-6
IN_RWKV = 3 * BRANCH_W + 2 * DECAY_LORA + 2 * ICLR_LORA + GATE_LORA
OFF_RWKV = BRANCH_W
OFF_Q = OFF_RWKV + IN_RWKV
OFF_K = OFF_Q + BRANCH_W
OFF_V = OFF_K + BRANCH_W
OFF_GATE = OFF_V + BRANCH_W
IN_TOTAL = OFF_GATE + N_BRANCH * D_MODEL

kernel_name = 'hybrid_pool_rwkv7_nat_moe_dit'


def rms_norm(x, g):
    xf = x.astype(jnp.float32)
    y = xf * lax.rsqrt(jnp.mean(xf * xf, axis=-1, keepdims=True) + RMS_EPS)
    return (y * g.astype(jnp.float32)).astype(x.dtype)


def split_heads(t, n_heads):
    return t.reshape(*t.shape[:-1], n_heads, t.shape[-1] // n_heads)


def centred_pool_minus_identity(z):
    B, T, _ = z.shape
    zf = z.astype(jnp.float32)
    csum = jnp.concatenate([jnp.zeros((B, 1, BRANCH_W), jnp.float32), jnp.cumsum(zf, axis=1)], axis=1)
    t = jnp.arange(T)
    groups = []
    for gi, win in enumerate(POOL_WINDOWS):
        lo = jnp.clip(t - win // 2, 0, T)
        hi = jnp.clip(t - win // 2 + win, 0, T)
        sl = slice(gi * POOL_GW, (gi + 1) * POOL_GW)
        cg = csum[:, :, sl]
        mean = (cg[:, hi] - cg[:, lo]) / (hi - lo).astype(jnp.float32)[None, :, None]
        groups.append(mean - zf[:, :, sl])
    return jnp.stack(groups, axis=2)


def pool_branch(z, pool_w, pool_scale):
    B, T, _ = z.shape
    p = centred_pool_minus_identity(z)
    return jnp.einsum('btgc,gcd->btgd', p, pool_w).reshape(B, T, BRANCH_W) * pool_scale


def centred_shift(z, mu_prev, mu_next):
    zp = jnp.pad(z, ((0, 0), (1, 0), (0, 0)))[:, :-1]
    zn = jnp.pad(z, ((0, 0), (0, 1), (0, 0)))[:, 1:]
    return z + mu_prev * (zp - z) + mu_next * (zn - z)


def rwkv_prepare(zr, w0, w2, a0, a2, k_kk, k_ka):
    B, T, _ = zr.shape
    f32 = jnp.float32
    r, k, v, wlo, alo, glo = jnp.split(zr, [BRANCH_W, 2 * BRANCH_W, 3 * BRANCH_W, 3 * BRANCH_W + 2 * DECAY_LORA, 3 * BRANCH_W + 2 * DECAY_LORA + 2 * ICLR_LORA], axis=-1)
    wlo = wlo.reshape(B, T, 2, DECAY_LORA)
    alo = alo.reshape(B, T, 2, ICLR_LORA)
    w_log = -jax.nn.softplus(-(w0 + jnp.einsum('btdr,drc->btdc', jnp.tanh(wlo), w2)).astype(f32)) - 0.5
    decay = jnp.exp(-jnp.exp(w_log))
    a = jax.nn.sigmoid((a0 + jnp.einsum('btdr,drc->btdc', alo, a2)).astype(f32))
    kk = split_heads((k * k_kk).astype(f32), RWKV_HEADS)
    kk = kk / jnp.maximum(jnp.sqrt(jnp.sum(kk * kk, axis=-1, keepdims=True)), 1e-12)
    k_dir = k.astype(f32)[:, :, None] * (1 + (a - 1) * k_ka.astype(f32))
    hd = lambda t: t.reshape(B, T, 2, RWKV_HEADS, RWKV_HD)
    kka = hd(a) * kk[:, :, None]
    return (split_heads(r.astype(f32), RWKV_HEADS), split_heads(v.astype(f32), RWKV_HEADS), kk, hd(decay), hd(k_dir), kka, glo)


def wkv_scan(state0, decay, k, v, kk, kka, r, reverse):
    seq = lambda t: jnp.moveaxis(t, 1, 0)
    xs = (seq(decay), seq(k), seq(v), seq(kk), seq(kka))
    if r is not None:
        xs = xs + (seq(r),)

    def step(S, inp):
        w_t, k_t, v_t, kk_t, kka_t = inp[:5]
        sa = jnp.einsum('bhij,bhj->bhi', S, kk_t)
        S = S * w_t[:, :, None, :] - sa[..., None] * kka_t[:, :, None, :] + v_t[..., None] * k_t[:, :, None, :]
        out = jnp.einsum('bhij,bhj->bhi', S, inp[5]) if len(inp) == 6 else None
        return S, out

    S, outs = lax.scan(step, state0, xs, reverse=reverse)
    return S, (None if r is None else jnp.moveaxis(outs, 0, 1))


def bidirectional_wkv(lat, cx, need_ctx_out):
    r, v, kk, decay, k_dir, kka, _ = lat
    rc, vc, kkc, decayc, k_dirc, kkac, _ = cx
    B = r.shape[0]
    outs, outs_c = [], []
    for d, reverse in enumerate((False, True)):
        S0 = jnp.zeros((B, RWKV_HEADS, RWKV_HD, RWKV_HD), jnp.float32)
        S_ctx, o_c = wkv_scan(S0, decayc[:, :, d], k_dirc[:, :, d], vc, kkc, kkac[:, :, d], rc if need_ctx_out else None, reverse)
        _, o = wkv_scan(S_ctx, decay[:, :, d], k_dir[:, :, d], v, kk, kka[:, :, d], r, reverse)
        outs.append(o)
        outs_c.append(o_c)
    return outs[0] + outs[1], (outs_c[0] + outs_c[1] if need_ctx_out else None)


def rwkv_readout(wkv, prep, bonus_rk, gn_g, gn_b, gate_g2):
    r, v, _, _, k_dir, _, glo = prep
    B, T = wkv.shape[:2]
    mu = jnp.mean(wkv, axis=-1, keepdims=True)
    var = jnp.mean(jnp.square(wkv - mu), axis=-1, keepdims=True)
    y = ((wkv - mu) * lax.rsqrt(var + GN_EPS)).reshape(B, T, BRANCH_W) * gn_g + gn_b
    bonus = jnp.sum(r[:, :, None] * k_dir * bonus_rk, axis=(2, 4))[..., None] * v
    gate = jax.nn.sigmoid(glo) @ gate_g2
    return (y + bonus.reshape(B, T, BRANCH_W)) * gate


def neighbourhood_attention(q, k, v, kc, vc, rpb):
    B, T, H, hd = q.shape
    rows = T // GRID_W
    kh = min(NAT_KH_MAX, rows)
    qg = jnp.moveaxis(q.reshape(B, rows, GRID_W, H, hd), 1, 0)
    kg = k.reshape(B, rows, GRID_W, H, hd)
    vg = v.reshape(B, rows, GRID_W, H, hd)
    cols = jnp.arange(GRID_W)
    cstart = jnp.clip(cols - NAT_KW // 2, 0, GRID_W - NAT_KW)
    col_idx = cstart[:, None] + jnp.arange(NAT_KW)
    col_off = col_idx - cols[:, None] + (NAT_KW - 1)
    n_loc = kh * NAT_KW

    def row_fn(args):
        r, q_r = args
        rs = jnp.clip(r - kh // 2, 0, rows - kh)
        k_win = lax.dynamic_slice_in_dim(kg, rs, kh, axis=1)[:, :, col_idx]
        v_win = lax.dynamic_slice_in_dim(vg, rs, kh, axis=1)[:, :, col_idx]
        row_off = rs + jnp.arange(kh) - r + (NAT_KH_MAX - 1)
        bias = rpb[:, row_off[None, :, None], col_off[:, None, :]]
        s_loc = jnp.einsum('bqhd,brqchd->bhqrc', q_r, k_win).astype(jnp.float32) + bias.astype(jnp.float32)
        s_ctx = jnp.einsum('bqhd,bchd->bhqc', q_r, kc).astype(jnp.float32)
        s = jnp.concatenate([s_loc.reshape(B, H, GRID_W, n_loc), s_ctx], axis=-1)
        p = jax.nn.softmax(s, axis=-1).astype(v.dtype)
        p_loc = p[..., :n_loc].reshape(B, H, GRID_W, kh, NAT_KW)
        return jnp.einsum('bhqrc,brqchd->bqhd', p_loc, v_win) + jnp.einsum('bhqc,bchd->bqhd', p[..., n_loc:], vc)

    o = lax.map(row_fn, (jnp.arange(rows), qg))
    return jnp.moveaxis(o, 0, 1).reshape(B, T, H * hd)


def context_attention(qc, kc, vc):
    B, L, H, hd = qc.shape
    s = jnp.einsum('bqhd,bkhd->bhqk', qc, kc).astype(jnp.float32)
    p = jax.nn.softmax(s, axis=-1).astype(vc.dtype)
    return jnp.einsum('bhqk,bkhd->bqhd', p, vc).reshape(B, L, H * hd)


def gated_merge(branches, gate_pre, w_branch, w_out):
    B, T, _ = gate_pre.shape
    proj = jnp.einsum('btnc,ncd->btnd', jnp.stack(branches, axis=2), w_branch)
    gates = jax.nn.sigmoid(gate_pre.reshape(B, T, N_BRANCH, D_MODEL))
    return jnp.sum(gates * proj, axis=2) @ w_out


def token_mixer(h, hc, w_in, pool_w, pool_scale, shift_mu, decay_w0, decay_w2, iclr_a0, iclr_a2, key_kk, key_ka, bonus_rk, gn_g, gn_b, gate_g2, nat_qn_g, nat_kn_g, nat_rpb, w_branch, w_out, need_ctx_out):
    splits = [OFF_RWKV, OFF_Q, OFF_K, OFF_V, OFF_GATE]
    zp, zr, zq, zk, zv, zg = jnp.split(h @ w_in, splits, axis=-1)
    if need_ctx_out:
        zpc, zrc, zqc, zkc, zvc, zgc = jnp.split(hc @ w_in, splits, axis=-1)
    else:
        zrc = hc @ w_in[:, OFF_RWKV:OFF_Q]
        zkc, zvc = jnp.split(hc @ w_in[:, OFF_K:OFF_GATE], 2, axis=-1)
    rwkv_p = (decay_w0, decay_w2, iclr_a0, iclr_a2, key_kk, key_ka)
    lat = rwkv_prepare(centred_shift(zr, shift_mu[0], shift_mu[1]), *rwkv_p)
    cx = rwkv_prepare(centred_shift(zrc, shift_mu[0], shift_mu[1]), *rwkv_p)
    wkv, wkv_c = bidirectional_wkv(lat, cx, need_ctx_out)
    kc = rms_norm(split_heads(zkc, NAT_HEADS), nat_kn_g)
    vc = split_heads(zvc, NAT_HEADS)
    q = rms_norm(split_heads(zq, NAT_HEADS), nat_qn_g) * NAT_SCALE
    k = rms_norm(split_heads(zk, NAT_HEADS), nat_kn_g)
    nat = neighbourhood_attention(q, k, split_heads(zv, NAT_HEADS), kc, vc, nat_rpb)
    branches = (pool_branch(zp, pool_w, pool_scale), rwkv_readout(wkv, lat, bonus_rk, gn_g, gn_b, gate_g2), nat)
    y = gated_merge(branches, zg, w_branch, w_out)
    if not need_ctx_out:
        return y, None
    qc = rms_norm(split_heads(zqc, NAT_HEADS), nat_qn_g) * NAT_SCALE
    branches_c = (pool_branch(zpc, pool_w, pool_scale), rwkv_readout(wkv_c, cx, bonus_rk, gn_g, gn_b, gate_g2), context_attention(qc, kc, vc))
    yc = gated_merge(branches_c, zgc, w_branch, w_out)
    return y, yc


def swiglu(h, w1, w3, w2):
    return (jax.nn.silu(h @ w1) * (h @ w3)) @ w2


def moe_swiglu(h, router, w1, w3, w2):
    logits = (h @ router).astype(jnp.float32)
    top_logit, top_idx = lax.top_k(logits, TOP_K)
    top_w = jax.nn.softmax(top_logit, axis=-1)
    gate = jnp.sum(jax.nn.one_hot(top_idx, N_EXPERTS, dtype=jnp.float32) * top_w[..., None], axis=-2)
    out = jnp.zeros_like(h)
    for e in range(N_EXPERTS):
        out = out + gate[..., e:e + 1].astype(h.dtype) * swiglu(h, w1[e], w3[e], w2[e])
    return out


def channel_mixer(h, li, ffn_w1, ffn_w3, ffn_w2, router, moe_w1, moe_w3, moe_w2):
    j = li // 2
    if li % 2 == 0:
        return swiglu(h, ffn_w1[j], ffn_w3[j], ffn_w2[j])
    return moe_swiglu(h, router[j], moe_w1[j], moe_w3[j], moe_w2[j])


def setup_inputs(seed: int = 0) -> dict:
    key = jax.random.key(seed)
    ks = iter(jax.random.split(key, 40))
    nrm = lambda shape, scale: jax.random.normal(next(ks), shape, jnp.float32) * scale
    unif = lambda shape, lo, hi: jax.random.uniform(next(ks), shape, jnp.float32, lo, hi)
    D = D_MODEL
    return {
        'x': nrm((BATCH, SEQ, D), 1.0),
        'c': nrm((BATCH, D), 1.0),
        'ctx': nrm((BATCH, CTX_LEN, D), 1.0),
        'c_ctx': nrm((D,), 1.0),
        'w_mod': nrm((DEPTH, D, 6 * D), 0.5 * D ** -0.5),
        'b_mod': nrm((DEPTH, 6 * D), 0.02),
        'norm_mix_g': 1.0 + nrm((DEPTH, D), 0.05),
        'norm_ffn_g': 1.0 + nrm((DEPTH, D), 0.05),
        'w_in': nrm((DEPTH, D, IN_TOTAL), D ** -0.5),
        'pool_w': nrm((DEPTH, POOL_GROUPS, POOL_GW, POOL_GW), POOL_GW ** -0.5),
        'pool_scale': 1.0 + nrm((DEPTH, BRANCH_W), 0.1),
        'shift_mu': unif((DEPTH, 2, IN_RWKV), 0.0, 0.5),
        'decay_w0': unif((DEPTH, 2, BRANCH_W), -6.0, 1.0),
        'decay_w2': nrm((DEPTH, 2, DECAY_LORA, BRANCH_W), 0.5 * DECAY_LORA ** -0.5),
        'iclr_a0': nrm((DEPTH, 2, BRANCH_W), 0.5),
        'iclr_a2': nrm((DEPTH, 2, ICLR_LORA, BRANCH_W), 0.5 * ICLR_LORA ** -0.5),
        'key_kk': 0.85 + nrm((DEPTH, BRANCH_W), 0.05),
        'key_ka': 1.0 + nrm((DEPTH, BRANCH_W), 0.05),
        'bonus_rk': nrm((DEPTH, RWKV_HEADS, RWKV_HD), 0.1),
        'gn_g': 1.0 + nrm((DEPTH, BRANCH_W), 0.05),
        'gn_b': nrm((DEPTH, BRANCH_W), 0.02),
        'gate_g2': nrm((DEPTH, GATE_LORA, BRANCH_W), GATE_LORA ** -0.5),
        'nat_qn_g': 1.0 + nrm((DEPTH, NAT_HD), 0.05),
        'nat_kn_g': 1.0 + nrm((DEPTH, NAT_HD), 0.05),
        'nat_rpb': nrm((DEPTH, NAT_HEADS, 2 * NAT_KH_MAX - 1, 2 * NAT_KW - 1), 0.2),
        'w_branch': nrm((DEPTH, N_BRANCH, BRANCH_W, D), BRANCH_W ** -0.5),
        'w_out': nrm((DEPTH, D, D), D ** -0.5),
        'ffn_w1': nrm((N_DENSE, D, D_FF), D ** -0.5),
        'ffn_w3': nrm((N_DENSE, D, D_FF), D ** -0.5),
        'ffn_w2': nrm((N_DENSE, D_FF, D), D_FF ** -0.5),
        'router': nrm((N_MOE, D, N_EXPERTS), D ** -0.5),
        'moe_w1': nrm((N_MOE, N_EXPERTS, D, D_FF), D ** -0.5),
        'moe_w3': nrm((N_MOE, N_EXPERTS, D, D_FF), D ** -0.5),
        'moe_w2': nrm((N_MOE, N_EXPERTS, D_FF, D), D_FF ** -0.5),
    }


def reference(x, c, ctx, c_ctx, w_mod, b_mod, norm_mix_g, norm_ffn_g, w_in, pool_w, pool_scale, shift_mu, decay_w0, decay_w2, iclr_a0, iclr_a2, key_kk, key_ka, bonus_rk, gn_g, gn_b, gate_g2, nat_qn_g, nat_kn_g, nat_rpb, w_branch, w_out, ffn_w1, ffn_w3, ffn_w2, router, moe_w1, moe_w3, moe_w2):
    xc = ctx
    for li in range(DEPTH):
        last = li == DEPTH - 1
        mod = jax.nn.silu(c) @ w_mod[li] + b_mod[li]
        mod_c = jax.nn.silu(c_ctx) @ w_mod[li] + b_mod[li]
        sh1, sc1, g1, sh2, sc2, g2 = jnp.split(mod[:, None, :], 6, axis=-1)
        sh1c, sc1c, g1c, sh2c, sc2c, g2c = jnp.split(mod_c, 6)
        h = rms_norm(x, norm_mix_g[li]) * (1 + sc1) + sh1
        hc = rms_norm(xc, norm_mix_g[li]) * (1 + sc1c) + sh1c
        y, yc = token_mixer(h, hc, w_in[li], pool_w[li], pool_scale[li], shift_mu[li], decay_w0[li], decay_w2[li], iclr_a0[li], iclr_a2[li], key_kk[li], key_ka[li], bonus_rk[li], gn_g[li], gn_b[li], gate_g2[li], nat_qn_g[li], nat_kn_g[li], nat_rpb[li], w_branch[li], w_out[li], not last)
        x = x + g1 * y
        h = rms_norm(x, norm_ffn_g[li]) * (1 + sc2) + sh2
        x = x + g2 * channel_mixer(h, li, ffn_w1, ffn_w3, ffn_w2, router, moe_w1, moe_w3, moe_w2)
        if not last:
            xc = xc + g1c * yc
            hc = rms_norm(xc, norm_ffn_g[li]) * (1 + sc2c) + sh2c
            xc = xc + g2c * channel_mixer(hc, li, ffn_w1, ffn_w3, ffn_w2, router, moe_w1, moe_w3, moe_w2)
    return x
```

```python
import os
from contextlib import ExitStack
import numpy as np
import concourse.bass as bass
import concourse.mybir as mybir
from concourse.bass_utils import run_bass_kernel_spmd


F32 = mybir.dt.float32
BF16 = mybir.dt.bfloat16
AF = mybir.ActivationFunctionType
ALU = mybir.AluOpType
AX = mybir.AxisListType

ENGS = ("tensor", "vector", "scalar", "gpsimd", "sync")
NDMA_SEM = 40
NDMA_HW = 32


class Sub:
    def __init__(self, ap, sub):
        self.ap = ap
        self.sub = sub


def _ak(x):
    if isinstance(x, Sub):
        return x.ap, (x.ap.name, x.sub)
    return x, (x.name, None)


class Prog:
    def __init__(self, nc, sems=None):
        self.nc = nc
        self.sems = sems
        self.ops = {e: [] for e in ENGS}
        self.cnt = {e: 0 for e in ENGS}
        self.st = {}
        self.waited = {e: {} for e in ENGS}
        self.ndma = 0
        self.ndma_sw = 0
        self.dma_cnt = [0] * NDMA_SEM
        self.out_tokens = []
        self.n_instr = 0

    def _deps(self, eng, reads, writes):
        raw = {}
        oth = {}

        def add(dst, tok):
            s, v = tok
            if dst.get(s, -1) < v:
                dst[s] = v

        for key in reads:
            name, sub = key
            d = self.st.setdefault(name, {})
            if sub not in d:
                d[sub] = {"w": None, "r": {}}
            subs = list(d.keys()) if sub is None else ([sub] + ([None] if None in d else []))
            for s in subs:
                if d[s]["w"] is not None:
                    add(raw, d[s]["w"])
        for key in writes:
            name, sub = key
            d = self.st.setdefault(name, {})
            if sub not in d:
                d[sub] = {"w": None, "r": {}}
            subs = list(d.keys()) if sub is None else ([sub] + ([None] if None in d else []))
            for s in subs:
                if d[s]["w"] is not None:
                    add(oth, d[s]["w"])
                for rs, rv in d[s]["r"].items():
                    add(oth, (rs, rv))
        return raw, oth

    def _commit(self, tok, reads, writes):
        for key in reads:
            name, sub = key
            e = self.st[name][sub]
            s, v = tok
            if e["r"].get(s, -1) < v:
                e["r"][s] = v
        for key in writes:
            name, sub = key
            d = self.st[name]
            if sub is None:
                for s in d:
                    d[s] = {"w": tok, "r": {}}
            else:
                d[sub] = {"w": tok, "r": {}}

    def _emit_waits(self, eng, raw, oth):
        own = "E_" + eng
        need = {}
        for s, v in raw.items():
            need[s] = max(need.get(s, -1), v)
        for s, v in oth.items():
            if s == own and eng == "tensor":
                continue
            need[s] = max(need.get(s, -1), v)
        wl = []
        for s, v in need.items():
            if self.waited[eng].get(s, -1) >= v:
                continue
            self.waited[eng][s] = v
            wl.append((s, v))
        return wl

    def op(self, eng, fn, reads, writes, is_dma=False, is_out=False):
        rk = [_ak(x)[1] for x in reads]
        wk = [_ak(x)[1] for x in writes]
        raw, oth = self._deps(eng, rk, wk)
        if is_dma:
            if eng == "gpsimd":
                i = NDMA_HW + (self.ndma_sw % (NDMA_SEM - NDMA_HW))
                self.ndma_sw += 1
            else:
                i = self.ndma % NDMA_HW
                self.ndma += 1
            prev = self.dma_cnt[i]
            self.dma_cnt[i] += 16
            tok = ("D_%d" % i, self.dma_cnt[i])
            if prev > 0:
                oth[tok[0]] = max(oth.get(tok[0], -1), prev)
        else:
            self.cnt[eng] += 1
            tok = ("E_" + eng, self.cnt[eng])
        wl = self._emit_waits(eng, raw, oth)
        eobj = getattr(self.nc, eng)
        for s_, v_ in wl:
            eobj.wait_ge(self.sems[s_], v_)
        ins = fn(eobj)
        ins.then_inc(self.sems[tok[0]], 16 if is_dma else 1)
        self._commit(tok, rk, wk)
        if is_out:
            self.out_tokens.append(tok)
        self.n_instr += 1
        return tok

    def dma(self, out, in_, q="sync", is_out=False, **kw):
        o, _ = _ak(out)
        i, _ = _ak(in_)
        return self.op(q, lambda e: e.dma_start(out=o, in_=i, **kw), [in_], [out], is_dma=True, is_out=is_out)

    def mm(self, out, lhsT, rhs, start=True, stop=True, **kw):
        o, _ = _ak(out)
        a, _ = _ak(lhsT)
        b, _ = _ak(rhs)
        rd = [lhsT, rhs] + ([] if start else [out])
        return self.op("tensor", lambda e: e.matmul(o, a, b, start=start, stop=stop, **kw), rd, [out])

    def tr(self, out, in_, ident):
        o, _ = _ak(out)
        a, _ = _ak(in_)
        b, _ = _ak(ident)
        return self.op("tensor", lambda e: e.transpose(o, a, b), [in_, ident], [out])

    def act(self, out, in_, func, bias=None, scale=None, accum_out=None, eng="scalar"):
        o, _ = _ak(out)
        a, _ = _ak(in_)
        rd = [in_]
        wr = [out]
        kw = {}
        if bias is not None:
            if isinstance(bias, (int, float)):
                kw["bias"] = float(bias)
            else:
                kw["bias"] = _ak(bias)[0]
                rd.append(bias)
        if scale is not None:
            if isinstance(scale, (int, float)):
                kw["scale"] = float(scale)
            else:
                kw["scale"] = _ak(scale)[0]
                rd.append(scale)
        if accum_out is not None:
            kw["accum_out"] = _ak(accum_out)[0]
            wr.append(accum_out)
        return self.op(eng, lambda e: e.activation(o, a, func, **kw), rd, wr)

    def tt(self, out, a, b, op, eng="vector"):
        o, _ = _ak(out)
        x, _ = _ak(a)
        y, _ = _ak(b)
        return self.op(eng, lambda e: e.tensor_tensor(o, x, y, op), [a, b], [out])

    def ts(self, out, a, s1, s2=None, op0=ALU.mult, op1=None, eng="vector", accum_out=None):
        o, _ = _ak(out)
        x, _ = _ak(a)
        rd = [a]
        wr = [out]

        def sc(s):
            if s is None or isinstance(s, (int, float)):
                return None if s is None else float(s)
            rd.append(s)
            return _ak(s)[0]

        v1 = sc(s1)
        v2 = sc(s2)
        kw = {}
        if op1 is not None:
            kw["op1"] = op1
        if accum_out is not None:
            kw["accum_out"] = _ak(accum_out)[0]
            wr.append(accum_out)
        return self.op(eng, lambda e: e.tensor_scalar(o, x, v1, v2, op0, **kw), rd, wr)

    def stt(self, out, in0, scalar, in1, op0, op1, eng="vector", accum_out=None):
        o, _ = _ak(out)
        x, _ = _ak(in0)
        y, _ = _ak(in1)
        rd = [in0, in1]
        wr = [out]
        if isinstance(scalar, (int, float)):
            sv = float(scalar)
        else:
            sv = _ak(scalar)[0]
            rd.append(scalar)
        kw = {}
        if accum_out is not None:
            kw["accum_out"] = _ak(accum_out)[0]
            wr.append(accum_out)
        return self.op(eng, lambda e: e.scalar_tensor_tensor(o, x, sv, y, op0, op1, **kw), rd, wr)

    def copy(self, out, in_, eng="vector"):
        o, _ = _ak(out)
        a, _ = _ak(in_)
        if eng == "scalar":
            return self.op(eng, lambda e: e.copy(o, a), [in_], [out])
        return self.op(eng, lambda e: e.tensor_copy(o, a), [in_], [out])

    def memset(self, out, val, eng="vector"):
        o, _ = _ak(out)
        return self.op(eng, lambda e: e.memset(o, val), [], [out])

    def reduce(self, out, in_, op, axis=AX.X, eng="vector"):
        o, _ = _ak(out)
        a, _ = _ak(in_)
        return self.op(eng, lambda e: e.tensor_reduce(o, a, axis, op), [in_], [out])

    def recip(self, out, in_):
        o, _ = _ak(out)
        a, _ = _ak(in_)
        return self.op("vector", lambda e: e.reciprocal(o, a), [in_], [out])

    def barrier(self):
        toks = {}
        for e in ENGS:
            if self.cnt[e] > 0:
                toks["E_" + e] = self.cnt[e]
        for i in range(NDMA_SEM):
            if self.dma_cnt[i] > 0:
                toks["D_%d" % i] = self.dma_cnt[i]
        for e in ENGS:
            eobj = getattr(self.nc, e)
            for s_, v_ in toks.items():
                if s_ == "E_" + e:
                    continue
                if self.waited[e].get(s_, -1) >= v_:
                    continue
                self.waited[e][s_] = v_
                eobj.wait_ge(self.sems[s_], v_)

    def finish(self):
        fin = {}
        for s_, v_ in self.out_tokens:
            fin[s_] = max(fin.get(s_, -1), v_)
        for s_, v_ in fin.items():
            self.nc.sync.wait_ge(self.sems[s_], v_)

    def finalize_old(self, block, sems):
        fin = {}
        for s, v in self.out_tokens:
            fin[s] = max(fin.get(s, -1), v)
        ops = self.ops

        def run(eng_name, eng):
            for wl, fn, tok, is_dma in ops[eng_name]:
                for s, v in wl:
                    eng.wait_ge(sems[s], v)
                ins = fn(eng)
                ins.then_inc(sems[tok[0]], 16 if is_dma else 1)
            if eng_name == "sync":
                for s, v in fin.items():
                    eng.wait_ge(sems[s], v)

        @block.tensor
        def _(e):
            run("tensor", e)

        @block.vector
        def _(e):
            run("vector", e)

        @block.scalar
        def _(e):
            run("scalar", e)

        @block.gpsimd
        def _(e):
            run("gpsimd", e)

        @block.sync
        def _(e):
            run("sync", e)


RMS_EPS = 1e-6


class Cfg:
    def __init__(self, T=8192, CTX=256, DFF=2816, NE=8, GW=64):
        self.D = 1024
        self.T = T
        self.CTX = CTX
        self.DFF = DFF
        self.NE = NE
        self.GW = GW
        self.TL = T // 4
        self.CL = CTX // 4
        self.NTOK = self.TL + 128
        self.TS = T + CTX
        blocks = []
        o = 0
        while o < self.TL:
            n = min(512, self.TL - o)
            blocks.append((o, n, 0))
            o += n
        blocks.append((self.TL, 128, 1))
        self.blocks = blocks
        h = len(blocks) // 2
        self.halves = [blocks[:h], blocks[h:]]


def mk_sems(nc, es):
    sems = {}
    for e in ENGS:
        sems["E_" + e] = es.enter_context(nc.semaphore("s_" + e))
    for i in range(NDMA_SEM):
        sems["D_%d" % i] = es.enter_context(nc.semaphore("d_%d" % i))
    return sems


def build_tok(cfg, mode, moe=False):
    D, NTOK, DFF, NE = cfg.D, cfg.NTOK, cfg.DFF, cfg.NE
    NF = DFF // 128
    nc = bass.Bass("TRN2", target_bir_lowering=False)
    dr = lambda n, s, d, k="ExternalInput": nc.dram_tensor(n, s, d, kind=k).ap()
    xT = dr("xT", [D, NTOK], F32)
    cvec = dr("cvec", [D, 2], F32)
    nmod = 2 * D if mode == "A" else 6 * D
    w_mod = dr("w_mod", [D, nmod], F32)
    b_mod = dr("b_mod", [nmod], F32)
    g_mix = dr("g_mix", [D], F32)
    ones_d = dr("ones", [128, 128], F32)
    ident_d = dr("ident", [128, 128], F32)
    if mode == "A":
        hT_o = dr("hT", [D, NTOK], BF16, "ExternalOutput")
    else:
        g_ffn = dr("g_ffn", [D], F32)
        brT = dr("brT", [3, 512, NTOK], BF16)
        w_gate = dr("w_gate", [D, 3 * D], F32)
        w_branch = dr("w_branch", [3, 512, D], F32)
        w_out = dr("w_out", [D, D], F32)
        if moe:
            router = dr("router", [D, NE], F32)
            w1 = dr("w1", [NE, D, DFF], F32)
            w3 = dr("w3", [NE, D, DFF], F32)
            w2 = dr("w2", [NE, DFF, D], F32)
        else:
            w1 = dr("w1", [1, D, DFF], F32)
            w3 = dr("w3", [1, D, DFF], F32)
            w2 = dr("w2", [1, DFF, D], F32)
        x_o = dr("xo", [D, NTOK], F32, "ExternalOutput")

    with ExitStack() as es:
        sb = lambda n, s, d: es.enter_context(nc.sbuf_tensor(n, s, d))
        ps = lambda n, s, d: es.enter_context(nc.psum_tensor(n, s, d))
        xs = sb("xs", [128, 8, NTOK], F32)
        hT = sb("hTs", [128, 8, NTOK], BF16)
        cv = sb("cv", [128, 8, 2], F32)
        siluc = sb("siluc", [128, 8, 2], BF16)
        nch = nmod // 128
        modT = sb("modT", [128, nch, 2], F32)
        bmod = sb("bmod", [128, nch], F32)
        gmix = sb("gmix", [128, 8], F32)
        A1 = sb("A1", [128, 8, 2], F32)
        ones = sb("ones_s", [128, 128], F32)
        ident = sb("ident_s", [128, 128], F32)
        sq = [sb("sq%d" % i, [128, 512], F32) for i in range(2)]
        rstd = sb("rstd", [128, 512], F32)
        tmpf = [sb("tmpf%d" % i, [128, 512], F32) for i in range(2)]
        PS = [ps("ps%d" % i, [128, 512], F32) for i in range(7)]
        if mode == "B":
            gffn = sb("gffn", [128, 8], F32)
            A2 = sb("A2", [128, 8, 2], F32)
            sig = [sb("sig%d" % i, [128, 512], F32) for i in range(2)]
            if moe:
                ntile = NTOK // 128
                gates = sb("gates", [128, ntile, NE], F32)
        sems = mk_sems(nc, es)
        P = Prog(nc, sems)
        psi = [0]

        def nps():
            psi[0] = (psi[0] + 1) % len(PS)
            return PS[psi[0]]

        P.dma(ones[:], ones_d)
        P.dma(ident[:], ident_d)
        for kc in range(8):
            P.dma(xs[:, kc, :], xT[kc * 128:(kc + 1) * 128, :], q=("sync" if kc % 2 == 0 else "gpsimd"))
        with nc.allow_non_contiguous_dma("small param loads"):
            P.dma(cv[:], cvec.rearrange("(c p) n -> p c n", p=128))
            P.dma(bmod[:], b_mod.rearrange("(c p) -> p c", p=128))
            P.dma(gmix[:], g_mix.rearrange("(c p) -> p c", p=128))
            if mode == "B":
                P.dma(gffn[:], g_ffn.rearrange("(c p) -> p c", p=128))
        P.act(siluc[:], cv[:], AF.Silu)
        wi = 0
        ph0 = ExitStack()
        wm = [ph0.enter_context(nc.sbuf_tensor("wm%d" % i, [128, 8, 512], BF16)) for i in range(2)]
        for g0 in range(0, nch, 4):
            w = wm[wi % 2]
            wi += 1
            P.dma(w[:], w_mod[:, g0 * 128:(g0 + 4) * 128].rearrange("(k p) m -> p k m", p=128), q="gpsimd")
            for j in range(4):
                mc = g0 + j
                pt = nps()
                for kc in range(8):
                    P.mm(pt[:, 0:2], w[:, kc, j * 128:(j + 1) * 128], siluc[:, kc, :], start=(kc == 0), stop=(kc == 7))
                P.ts(modT[:, mc, :], pt[:, 0:2], bmod[:, mc:mc + 1], None, op0=ALU.add)
        P.barrier()
        ph0.close()
        def mkA(Aout, gvec, sc_base):
            for kc in range(8):
                P.ts(Aout[:, kc, :], modT[:, sc_base + kc, :], 1.0, gvec[:, kc:kc + 1], op0=ALU.add, op1=ALU.mult)

        mkA(A1, gmix, 8)
        if mode == "B":
            mkA(A2, gffn, 32)

        def norm(Acoef, sh_base, want_f32=None):
            for (o, n, col) in cfg.blocks:
                pt = nps()
                for kc in range(8):
                    s = sq[kc % 2]
                    P.act(s[:, 0:n], xs[:, kc, o:o + n], AF.Square)
                    P.mm(pt[:, 0:n], ones[:], s[:, 0:n], start=(kc == 0), stop=(kc == 7))
                P.ts(rstd[:, 0:n], pt[:, 0:n], 1.0 / 1024.0, RMS_EPS, op0=ALU.mult, op1=ALU.add)
                P.recip(rstd[:, 0:n], rstd[:, 0:n])
                P.act(rstd[:, 0:n], rstd[:, 0:n], AF.Sqrt)
                for kc in range(8):
                    t = tmpf[kc % 2]
                    P.tt(t[:, 0:n], xs[:, kc, o:o + n], rstd[:, 0:n], ALU.mult)
                    P.act(hT[:, kc, o:o + n], t[:, 0:n], AF.Identity, bias=modT[:, sh_base + kc, col:col + 1], scale=Acoef[:, kc, col:col + 1])
                    if want_f32 is not None:
                        P.act(hf[:, kc, 0:n], t[:, 0:n], AF.Identity, bias=modT[:, sh_base + kc, col:col + 1], scale=Acoef[:, kc, col:col + 1])
                if want_f32 is not None:
                    want_f32(o, n)

        norm(A1, 0)
        if mode == "A":
            for kc in range(8):
                P.dma(hT_o[kc * 128:(kc + 1) * 128, :], hT[:, kc, :], is_out=True)
            P.barrier()
            P.finish()
            return nc

        P.barrier()
        with ExitStack() as ph:
            sb2 = lambda n, s_, d: ph.enter_context(nc.sbuf_tensor(n, s_, d))
            br = sb2("br", [128, 3, 4, 512], BF16)
            mT = sb2("mT", [128, 8, 512], BF16)
            macc = sb2("macc", [128, 512], F32)
            wg = [sb2("wg%d" % i, [128, 8, 128], BF16) for i in range(3)]
            wb = [sb2("wb%d" % i, [128, 4, 128], BF16) for i in range(3)]
            wi = 0
            for (o, n, col) in cfg.blocks:
                for n_ in range(3):
                    for g in range(4):
                        P.dma(br[:, n_, g, 0:n], brT[n_, g * 128:(g + 1) * 128, o:o + n], q="sync")
                for dc in range(8):
                    for n_ in range(3):
                        wgt = wg[wi % 3]
                        wbt = wb[wi % 3]
                        wi += 1
                        c0 = n_ * D + dc * 128
                        P.dma(wgt[:], w_gate[:, c0:c0 + 128].rearrange("(k p) m -> p k m", p=128), q="gpsimd")
                        P.dma(wbt[:], w_branch[n_, :, dc * 128:(dc + 1) * 128].rearrange("(k p) m -> p k m", p=128), q="gpsimd")
                        pg = nps()
                        for kc in range(8):
                            P.mm(pg[:, 0:n], wgt[:, kc, :], hT[:, kc, o:o + n], start=(kc == 0), stop=(kc == 7))
                        pp = nps()
                        for g in range(4):
                            P.mm(pp[:, 0:n], wbt[:, g, :], br[:, n_, g, 0:n], start=(g == 0), stop=(g == 3))
                        sg = sig[wi % 2]
                        P.act(sg[:, 0:n], pg[:, 0:n], AF.Sigmoid)
                        if n_ == 0:
                            P.tt(macc[:, 0:n], sg[:, 0:n], pp[:, 0:n], ALU.mult)
                        else:
                            P.tt(sg[:, 0:n], sg[:, 0:n], pp[:, 0:n], ALU.mult)
                            P.tt(macc[:, 0:n], macc[:, 0:n], sg[:, 0:n], ALU.add, eng="gpsimd")
                    P.copy(mT[:, dc, 0:n], macc[:, 0:n], eng="scalar")
                for dc in range(8):
                    wgt = wg[wi % 3]
                    wi += 1
                    P.dma(wgt[:], w_out[:, dc * 128:(dc + 1) * 128].rearrange("(k p) m -> p k m", p=128), q="gpsimd")
                    py = nps()
                    for kc in range(8):
                        P.mm(py[:, 0:n], wgt[:, kc, :], mT[:, kc, 0:n], start=(kc == 0), stop=(kc == 7))
                    P.stt(xs[:, dc, o:o + n], py[:, 0:n], modT[:, 16 + dc, col:col + 1], xs[:, dc, o:o + n], ALU.mult, ALU.add)
            P.barrier()

        ph = ExitStack()
        sb2 = lambda n, s_, d: ph.enter_context(nc.sbuf_tensor(n, s_, d))
        if moe:
            hf = sb2("hf", [128, 8, 512], F32)
            rt = sb2("rt", [128, 8, NE], F32)
            lg = sb2("lg", [128, NE], F32)
            l2 = sb2("l2", [128, NE], F32)
            m1 = sb2("m1", [128, 1], F32)
            m2 = sb2("m2", [128, 1], F32)
            nm1 = sb2("nm1", [128, 1], F32)
            den = sb2("den", [128, 1], F32)
            ex = sb2("ex", [128, NE], F32)
            with nc.allow_non_contiguous_dma("router"):
                P.dma(rt[:], router.rearrange("(k p) e -> p k e", p=128))

            def route(o, n):
                for t0 in range(0, n, 128):
                    ti = (o + t0) // 128
                    pl = nps()
                    for kc in range(8):
                        P.mm(pl[:, 0:NE], hf[:, kc, t0:t0 + 128], rt[:, kc, :], start=(kc == 0), stop=(kc == 7))
                    P.copy(lg[:], pl[:, 0:NE])
                    P.reduce(m1[:], lg[:], ALU.max)
                    P.ts(l2[:], lg[:], m1[:, 0:1], -1e30, op0=ALU.is_equal, op1=ALU.mult)
                    P.tt(l2[:], l2[:], lg[:], ALU.add)
                    P.reduce(m2[:], l2[:], ALU.max)
                    P.ts(nm1[:], m1[:], -1.0, None, op0=ALU.mult)
                    P.act(ex[:], lg[:], AF.Exp, bias=nm1[:, 0:1])
                    P.act(den[:], m2[:], AF.Exp, bias=nm1[:, 0:1])
                    P.ts(den[:], den[:], 1.0, None, op0=ALU.add)
                    P.recip(den[:], den[:])
                    P.ts(l2[:], lg[:], m2[:, 0:1], None, op0=ALU.is_ge)
                    P.tt(l2[:], l2[:], ex[:], ALU.mult)
                    P.ts(gates[:, ti, :], l2[:], den[:, 0:1], None, op0=ALU.mult)

            norm(A2, 24, want_f32=route)
        else:
            norm(A2, 24)

        P.barrier()
        ph.close()
        ph = ExitStack()
        sb2 = lambda n, s_, d: ph.enter_context(nc.sbuf_tensor(n, s_, d))
        maxhalf = max(sum(n for _, n, _ in h) for h in cfg.halves)
        actT = sb2("actT", [128, NF, maxhalf], BF16)
        w1b = [sb2("w1b%d" % i, [128, 8, 128], BF16) for i in range(3)]
        w3b = [sb2("w3b%d" % i, [128, 8, 128], BF16) for i in range(3)]
        w2b = [sb2("w2b%d" % i, [128, NF, 128], BF16) for i in range(2)]
        if moe:
            gcol = sb2("gcol", [128, 128], F32)
            gateB = [sb2("gateB%d" % i, [128, 512], F32) for i in range(3)]
        nexp = NE if moe else 1
        wi = 0
        w2i = 0
        gi = 0
        for half in cfg.halves:
            hb = half[0][0]
            for e in range(nexp):
                for fc in range(NF):
                    a = w1b[wi % 3]
                    b = w3b[wi % 3]
                    wi += 1
                    P.dma(a[:], w1[e, :, fc * 128:(fc + 1) * 128].rearrange("(k p) m -> p k m", p=128), q="gpsimd")
                    P.dma(b[:], w3[e, :, fc * 128:(fc + 1) * 128].rearrange("(k p) m -> p k m", p=128), q="gpsimd")
                    for (o, n, col) in half:
                        p1 = nps()
                        for kc in range(8):
                            P.mm(p1[:, 0:n], a[:, kc, :], hT[:, kc, o:o + n], start=(kc == 0), stop=(kc == 7))
                        p3 = nps()
                        for kc in range(8):
                            P.mm(p3[:, 0:n], b[:, kc, :], hT[:, kc, o:o + n], start=(kc == 0), stop=(kc == 7))
                        sg = sig[gi % 2]
                        gi += 1
                        P.act(sg[:, 0:n], p1[:, 0:n], AF.Silu)
                        P.tt(actT[:, fc, o - hb:o - hb + n], sg[:, 0:n], p3[:, 0:n], ALU.mult)
                gB = {}
                if moe:
                    for (o, n, col) in half:
                        gb = gateB[gi % 3]
                        gi += 1
                        for t0 in range(0, n, 128):
                            ti = (o + t0) // 128
                            P.copy(gcol[:], gates[:, ti, e:e + 1].to_broadcast([128, 128]))
                            pgm = nps()
                            P.mm(pgm[:, 0:128], gcol[:], ident[:])
                            P.copy(gb[:, t0:t0 + 128], pgm[:, 0:128], eng="scalar")
                        gB[o] = gb
                for dc in range(8):
                    c = w2b[w2i % 2]
                    w2i += 1
                    P.dma(c[:], w2[e, :, dc * 128:(dc + 1) * 128].rearrange("(k p) m -> p k m", p=128), q="gpsimd")
                    for (o, n, col) in half:
                        py = nps()
                        for fc in range(NF):
                            P.mm(py[:, 0:n], c[:, fc, :], actT[:, fc, o - hb:o - hb + n], start=(fc == 0), stop=(fc == NF - 1))
                        if moe:
                            t = tmpf[dc % 2]
                            P.tt(t[:, 0:n], py[:, 0:n], gB[o][:, 0:n], ALU.mult)
                            P.stt(xs[:, dc, o:o + n], t[:, 0:n], modT[:, 40 + dc, col:col + 1], xs[:, dc, o:o + n], ALU.mult, ALU.add)
                        else:
                            P.stt(xs[:, dc, o:o + n], py[:, 0:n], modT[:, 40 + dc, col:col + 1], xs[:, dc, o:o + n], ALU.mult, ALU.add)
        for kc in range(8):
            P.dma(x_o[kc * 128:(kc + 1) * 128, :], xs[:, kc, :], is_out=True)
        P.barrier()
        ph.close()
        P.barrier()
        P.finish()
    return nc


DBG = int(os.environ.get('SEQ_DBG', '47'))

GN_EPS = 64e-5
NAT_KH_MAX = 8
NAT_KW = 16
POOL_WINDOWS = (2, 4, 8, 16)
OFF_RWKV = 512
OFF_Q = 2272
OFF_K = 2784
OFF_V = 3296
OFF_GATE = 3808
FM_BLOCKS = [("r", 128), ("k", 128), ("v", 128), ("wlo", 64), ("alo", 64), ("glo", 96), ("q", 128), ("kn", 128)]
NFM = sum(n for _, n in FM_BLOCKS)


def nat_geometry(T, GW=64):
    rows = T // GW
    kh = min(NAT_KH_MAX, rows)
    ntl = rows // 2
    rs = lambda r: min(max(r - kh // 2, 0), rows - kh)
    cs = lambda c: min(max(c - NAT_KW // 2, 0), GW - NAT_KW)
    qi = np.arange(128)
    ki = np.arange(128)
    uniq = {}
    tiles = []
    plan = []
    for m in range(ntl):
        r = 2 * m + qi // GW
        c = qi % GW
        rsv = np.array([rs(x) for x in r])
        csv = np.array([cs(x) for x in c])
        lo = min(rsv) // 2
        hi = (max(rsv) + kh - 1) // 2
        lst = []
        for kt in range(lo, hi + 1):
            kr = 2 * kt + ki // GW
            kc = ki % GW
            valid = ((kr[:, None] >= rsv[None, :]) & (kr[:, None] < rsv[None, :] + kh) &
                     (kc[:, None] >= csv[None, :]) & (kc[:, None] < csv[None, :] + NAT_KW))
            if not valid.any():
                continue
            ro = (kr[:, None] - r[None, :] + NAT_KH_MAX - 1) * valid
            co = (kc[:, None] - c[None, :] + NAT_KW - 1) * valid
            key = (valid.tobytes(), ro.tobytes(), co.tobytes())
            if key not in uniq:
                uniq[key] = len(tiles)
                tiles.append((valid, ro, co))
            lst.append((kt, uniq[key]))
        plan.append(lst)
    return plan, tiles


def pool_bands(W):
    Tt = 512
    t = np.arange(Tt)
    lo = np.clip(t - W // 2, 0, Tt)
    hi = np.clip(t - W // 2 + W, 0, Tt)
    M = np.zeros((Tt, Tt), np.float32)
    for j in range(Tt):
        M[lo[j]:hi[j], j] = 1.0 / float(hi[j] - lo[j])
    M -= np.eye(Tt, dtype=np.float32)
    b = np.stack([M[0:128, 128:256], M[128:256, 128:256], M[256:384, 128:256], M[0:128, 0:128], M[384:512, 384:512]])
    return np.ascontiguousarray(b)


def build_seq(cfg, stop=99):
    D, T, CTX, TS = cfg.D, cfg.T, cfg.CTX, cfg.TS
    NTL = T // 128
    NTC = CTX // 128
    NTS = TS // 128
    plan, tiles = nat_geometry(T, cfg.GW)
    NT = len(tiles)
    nc = bass.Bass("TRN2", target_bir_lowering=False)
    dr = lambda n, s, d, k="ExternalInput": nc.dram_tensor(n, s, d, kind=k).ap()
    hTs = dr("hTs", [D, TS], BF16)
    w_fm = dr("w_fm", [D, NFM], F32)
    w_tm = dr("w_tm", [D, 256], F32)
    pch_d = dr("pch", [128, 18], F32)
    plo_d = dr("plo", [64, 4], F32)
    pgl_d = dr("pgl", [96, 2], F32)
    dw2_d = dr("dw2", [64, 128], F32)
    aw2_d = dr("aw2", [64, 128], F32)
    gg2_d = dr("gg2", [96, 128], F32)
    poolw_d = dr("poolw", [128, 128], F32)
    bands_d = dr("bands", [5, 128, 128], F32)
    nbias_d = dr("nbias", [NT, 2, 128, 128], F32)
    nmask_d = dr("nmask", [NT, 128, 128], F32)
    ident_d = dr("ident", [128, 128], F32)
    bones_d = dr("bones", [128, 128], F32)
    hsel_d = dr("hsel", [2, 128], F32)
    opad_d = dr("opad", [2, 128, 128], F32)
    brT_o = dr("brTo", [3, 128, TS], BF16, "ExternalOutput")
    di = lambda n, s, d: nc.dram_tensor(n, s, d).ap()
    zlat = di("zlat", [6, 128, T + 2], F32)
    zctx = di("zctx", [6, 128, CTX + 2], F32)
    xtm = [[di("xtm_%d_%d" % (d, i), [TS, 128], F32) for i in range(3)] for d in range(2)]
    xkk = di("xtm_kk", [TS, 128], F32)
    xr = di("xtm_r", [TS, 128], F32)
    bon_d = di("bon_d", [128, TS], F32)
    gate_d = di("gate_d", [128, TS], F32)

    seqs = [("lat", 0, T, zlat), ("ctx", T, CTX, zctx)]

    with ExitStack() as es:
        es.enter_context(nc.allow_non_contiguous_dma("scratch layouts"))
        sb = lambda n, s, d: es.enter_context(nc.sbuf_tensor(n, s, d))
        ps = lambda n, s, d: es.enter_context(nc.psum_tensor(n, s, d))
        sems = mk_sems(nc, es)
        P = Prog(nc, sems)
        PS = [ps("ps%d" % i, [128, 512], F32) for i in range(8)]
        psi = [0]

        def nps(k=8):
            psi[0] = (psi[0] + 1) % k
            return PS[psi[0]]

        ident = sb("ident_s", [128, 128], F32)
        bones = sb("bones_s", [128, 128], F32)
        hsel = sb("hsel_s", [2, 128], F32)
        pch = sb("pch_s", [128, 18], F32)
        pder = sb("pder", [128, 8], F32)
        plo = sb("plo_s", [64, 4], F32)
        plod = sb("plod", [64, 2], F32)
        pgl = sb("pgl_s", [96, 2], F32)
        pgld = sb("pgld", [96, 1], F32)
        dw2 = sb("dw2_s", [64, 128], F32)
        aw2 = sb("aw2_s", [64, 128], F32)
        gg2 = sb("gg2_s", [96, 128], F32)
        zero = sb("zero_s", [128, 8], F32)
        P.dma(ident[:], ident_d)
        P.dma(bones[:], bones_d)
        P.dma(hsel[:], hsel_d)
        P.dma(pch[:], pch_d)
        P.dma(plo[:], plo_d)
        P.dma(pgl[:], pgl_d)
        P.dma(dw2[:], dw2_d)
        P.dma(aw2[:], aw2_d)
        P.dma(gg2[:], gg2_d)
        P.memset(zero[:], 0.0)
        for i in range(3):
            P.tt(pder[:, i:i + 1], pch[:, 2 * i:2 * i + 1], pch[:, 2 * i + 1:2 * i + 2], ALU.add)
            P.ts(pder[:, i:i + 1], pder[:, i:i + 1], -1.0, 1.0, op0=ALU.mult, op1=ALU.add)
        P.ts(pder[:, 3:4], pch[:, 11:12], -1.0, 1.0, op0=ALU.mult, op1=ALU.add)
        P.ts(pder[:, 4:5], pch[:, 16:17], 0.125, None, op0=ALU.mult)
        for i in range(2):
            P.tt(plod[:, i:i + 1], plo[:, 2 * i:2 * i + 1], plo[:, 2 * i + 1:2 * i + 2], ALU.add)
            P.ts(plod[:, i:i + 1], plod[:, i:i + 1], -1.0, 1.0, op0=ALU.mult, op1=ALU.add)
        P.tt(pgld[:, 0:1], pgl[:, 0:1], pgl[:, 1:2], ALU.add)
        P.ts(pgld[:, 0:1], pgld[:, 0:1], -1.0, 1.0, op0=ALU.mult, op1=ALU.add)
        for (_, _, L, zz) in seqs:
            for rb in range(6):
                P.dma(Sub(zz[rb, :, 0:1], ("h", rb, 0)), zero[:, 0:1])
                P.dma(Sub(zz[rb, :, L + 1:L + 2], ("h", rb, 1)), zero[:, 0:1])

        if stop <= 0:
            P.barrier()
            P.finish()
            return nc
        ph = ExitStack()
        sb2 = lambda n, s, d: ph.enter_context(nc.sbuf_tensor(n, s, d))
        wfm = sb2("wfm", [128, 8, NFM], BF16)
        wtm = sb2("wtm", [128, 8, 256], BF16)
        P.dma(wfm[:], w_fm.rearrange("(k p) m -> p k m", p=128), q="gpsimd")
        P.dma(wtm[:], w_tm.rearrange("(k p) m -> p k m", p=128), q="gpsimd")
        QN = sb2("QN", [128, TS], BF16)
        KN = sb2("KN", [128, TS], BF16)
        VP = sb2("VP", [128, NTS, 2, 128], BF16)
        zp = sb2("zp", [128, NTS, 128], F32)
        hb = [sb2("hb%d" % i, [128, 8, 512], BF16) for i in range(2)]
        stg = [sb2("stg%d" % i, [128, 512], F32) for i in range(3)]
        sqn = [sb2("sqn%d" % i, [128, 512], F32) for i in range(2)]
        rsn = [sb2("rsn%d" % i, [128, 512], F32) for i in range(2)]
        P.memset(VP[:], 0.0, eng="gpsimd")
        if stop <= 0.5:
            P.barrier()
            ph.close()
            P.barrier()
            P.finish()
            return nc
        bi = 0
        si = 0
        for (sname, s0, L, zz) in seqs:
            for o in range(0, L, 512):
                n = min(512, L - o)
                h = hb[bi % 2]
                bi += 1
                P.dma(h[:, :, 0:n], hTs[:, s0 + o:s0 + o + n].rearrange("(k p) n -> p k n", p=128))
                c0 = 0
                for rbi, (nm, M) in enumerate(FM_BLOCKS):
                    pt = nps()
                    for kc in range(8):
                        P.mm(pt[0:M, 0:n], wfm[:, kc, c0:c0 + M], h[:, kc, 0:n], start=(kc == 0), stop=(kc == 7))
                    c0 += M
                    if rbi < 6:
                        st = stg[si % 3]
                        si += 1
                        P.copy(st[0:M, 0:n], pt[0:M, 0:n], eng=("scalar" if rbi % 2 else "vector"))
                        if DBG & 2:
                            P.dma(Sub(zz[rbi, 0:M, 1 + o:1 + o + n], ("b", rbi, o)), st[0:M, 0:n])
                    elif DBG & 4:
                        s_ = sqn[rbi % 2]
                        P.act(s_[:, 0:n], pt[:, 0:n], AF.Square)
                        p2 = nps()
                        P.mm(p2[:, 0:n], bones[:], s_[:, 0:n])
                        r_ = rsn[rbi % 2]
                        P.ts(r_[:, 0:n], p2[:, 0:n], 1.0 / 64.0, 1e-6, op0=ALU.mult, op1=ALU.add)
                        P.recip(r_[:, 0:n], r_[:, 0:n])
                        P.act(r_[:, 0:n], r_[:, 0:n], AF.Sqrt)
                        P.tt(r_[:, 0:n], r_[:, 0:n], pt[:, 0:n], ALU.mult)
                        if nm == "q":
                            P.ts(QN[:, s0 + o:s0 + o + n], r_[:, 0:n], pder[:, 4:5], None, op0=ALU.mult)
                        else:
                            P.ts(KN[:, s0 + o:s0 + o + n], r_[:, 0:n], pch[:, 17:18], None, op0=ALU.mult)
                for t0 in (range(0, n, 128) if DBG & 8 else []):
                    ti = (s0 + o + t0) // 128
                    pt = nps()
                    for kc in range(8):
                        P.mm(pt[:, 0:256], h[:, kc, t0:t0 + 128], wtm[:, kc, :], start=(kc == 0), stop=(kc == 7))
                    P.copy(zp[:, ti, :], pt[:, 0:128])
                    if DBG & 16:
                        P.copy(VP[:, ti, 0, 0:64], pt[:, 128:192], eng="scalar")
                        P.copy(VP[:, ti, 1, 64:128], pt[:, 192:256], eng="scalar")
                    if DBG & 32:
                        P.copy(VP[:, ti, 0, 0:64], pt[:, 128:192], eng="vector")
                        P.copy(VP[:, ti, 1, 64:128], pt[:, 192:256], eng="vector")

        if stop <= 1:
            P.barrier()
            ph.close()
            P.barrier()
            P.finish()
            return nc
        bands = sb2("bands_s", [128, 5, 128], F32)
        poolw = sb2("poolw_s", [128, 128], F32)
        for i in range(5):
            P.dma(bands[:, i, :], bands_d[i])
        P.dma(poolw[:], poolw_d)
        pst = [sb2("pst%d" % i, [128, 512], F32) for i in range(2)]
        pob = [sb2("pob%d" % i, [128, 512], BF16) for i in range(2)]
        bi = 0
        for (sname, s0, L, zz) in seqs:
            nt = L // 128
            tb = s0 // 128
            for o in range(0, nt, 4):
                m = min(4, nt - o)
                pt = nps()
                for j in range(m):
                    i = o + j
                    terms = []
                    if i > 0:
                        terms.append((i - 1, 0))
                    terms.append((i, 3 if i == 0 else (4 if i == nt - 1 else 1)))
                    if i < nt - 1:
                        terms.append((i + 1, 2))
                    for q_, (ti, bidx) in enumerate(terms):
                        P.mm(pt[:, j * 128:(j + 1) * 128], zp[:, tb + ti, :], bands[:, bidx, :], start=(q_ == 0), stop=(q_ == len(terms) - 1))
                st = pst[bi % 2]
                ob = pob[bi % 2]
                bi += 1
                P.copy(st[:, 0:m * 128], pt[:, 0:m * 128])
                p2 = nps()
                P.mm(p2[:, 0:m * 128], poolw[:], st[:, 0:m * 128])
                P.ts(ob[:, 0:m * 128], p2[:, 0:m * 128], pch[:, 15:16], None, op0=ALU.mult)
                P.dma(brT_o[0, :, s0 + o * 128:s0 + (o + m) * 128], ob[:, 0:m * 128], is_out=True)

        if stop <= 2:
            P.barrier()
            ph.close()
            P.barrier()
            P.finish()
            return nc
        nbm = sb2("nbm", [128, NT, 2, 128], F32)
        nmk = sb2("nmk", [128, NT, 128], F32)
        opad = sb2("opad_s", [128, 2, 128], BF16)
        for i in range(NT):
            P.dma(nmk[:, i, :], nmask_d[i])
            for hh in range(2):
                P.dma(nbm[:, i, hh, :], nbias_d[i, hh])
        opf = sb2("opf", [128, 2, 128], F32)
        for hh in range(2):
            P.dma(opf[:, hh, :], opad_d[hh])
        P.copy(opad[:], opf[:])
        P.ts(nmk[:], nmk[:], 1.0, 30000.0, op0=ALU.subtract, op1=ALU.mult)
        for hh in range(2):
            P.tt(nbm[:, :, hh, :], nbm[:, :, hh, :], nmk[:], ALU.add)
        sct = [sb2("sct%d" % i, [128, 128], F32) for i in range(3)]
        ptb = [sb2("ptb%d" % i, [128, 128], BF16) for i in range(4)]
        nrc = sb2("nrc", [128, 128], F32)
        nob = [sb2("nob%d" % i, [128, 512], BF16) for i in range(2)]
        ctx_tiles = [NTL + i for i in range(NTC)]
        qtiles = [(m, [(kt, tid) for kt, tid in plan[m]] + [(c, None) for c in ctx_tiles]) for m in range(NTL)]
        qtiles += [(NTL + i, [(c, None) for c in ctx_tiles]) for i in range(NTC)]
        ci = 0
        po = PS[6]
        pd = PS[7]
        for qi_, (m, klist) in enumerate(qtiles):
            tot = 2 * len(klist)
            cnt = 0
            for hh in range(2):
                hs = slice(hh * 64, (hh + 1) * 64)
                for (kt, tid) in klist:
                    pt = nps(6)
                    P.mm(pt[:, 0:128], KN[hs, kt * 128:(kt + 1) * 128], QN[hs, m * 128:(m + 1) * 128])
                    pb = ptb[ci % 4]
                    if tid is not None:
                        sc_ = sct[ci % 3]
                        P.tt(sc_[:], pt[:, 0:128], nbm[:, tid, hh, :], ALU.add)
                        P.act(pb[:], sc_[:], AF.Exp)
                    else:
                        P.act(pb[:], pt[:, 0:128], AF.Exp)
                    ci += 1
                    P.mm(po[:, 0:128], VP[:, kt, hh, :], pb[:], start=(cnt == 0), stop=(cnt == tot - 1))
                    P.mm(pd[:, 0:128], opad[:, hh, :], pb[:], start=(cnt == 0), stop=(cnt == tot - 1))
                    cnt += 1
            ob = nob[(qi_ // 4) % 2]
            j = qi_ % 4
            P.recip(nrc[:], pd[:, 0:128])
            P.tt(ob[:, j * 128:(j + 1) * 128], po[:, 0:128], nrc[:], ALU.mult)
            if j == 3 or qi_ == len(qtiles) - 1:
                q0 = (qi_ // 4) * 4
                P.dma(brT_o[2, :, q0 * 128:(qi_ + 1) * 128], ob[:, 0:(j + 1) * 128], is_out=True)
        if stop <= 3:
            P.barrier()
            ph.close()
            P.barrier()
            P.finish()
            return nc
        P.barrier()
        ph.close()

        ph = ExitStack()
        sb2 = lambda n, s, d: ph.enter_context(nc.sbuf_tensor(n, s, d))
        vS = sb2("vS", [128, TS], F32)
        Od = [sb2("O%d" % d, [128, TS], F32) for d in range(2)]
        ph4 = ExitStack()
        sb1 = sb2
        sb2 = lambda n, s, d: ph4.enter_context(nc.sbuf_tensor(n, s, d))
        zin = {nm: sb2("zin_" + nm, [M, 514], F32) for nm, M in FM_BLOCKS[:6]}
        zs = {nm: sb2("zs_" + nm, [M, 512], F32) for nm, M in FM_BLOCKS[:6] if nm != "v"}
        tw = sb2("tw", [64, 512], F32)
        ed = [sb2("e%d" % d, [128, 512], F32) for d in range(2)]
        ad = [sb2("a%d" % d, [128, 512], F32) for d in range(2)]
        wd = [sb2("w%d" % d, [128, 512], F32) for d in range(2)]
        kx = sb2("kx", [128, 512], F32)
        ksq = sb2("ksq", [128, 512], F32)
        rn = sb2("rn", [128, 512], F32)
        kk = sb2("kk", [128, 512], F32)
        nkka = [sb2("nkka%d" % d, [128, 512], F32) for d in range(2)]
        kd = [sb2("kd%d" % d, [128, 512], F32) for d in range(2)]
        tq = sb2("tq", [128, 512], F32)
        bonb = sb2("bonb", [128, 512], F32)
        sgl = sb2("sgl", [96, 512], F32)
        gtb = sb2("gtb", [128, 512], F32)
        tst = [sb2("tst%d" % i, [128, 128], F32) for i in range(4)]
        mus = {"r": (0, 1, 0), "k": (2, 3, 1), "v": (4, 5, 2)}
        ti_ = 0
        for (sname, s0, L, zz) in seqs:
            for o in range(0, L, 512):
                n = min(512, L - o)
                g0 = s0 + o
                for rbi, (nm, M) in enumerate(FM_BLOCKS[:6]):
                    P.dma(zin[nm][:, 0:n + 2], Sub(zz[rbi, 0:M, o:o + n + 2], None))
                    z = zin[nm]
                    if nm in mus:
                        a_, b_, c_ = mus[nm]
                        mp, mn, c0_ = pch[:, a_:a_ + 1], pch[:, b_:b_ + 1], pder[:, c_:c_ + 1]
                    elif nm == "wlo":
                        mp, mn, c0_ = plo[:, 0:1], plo[:, 1:2], plod[:, 0:1]
                    elif nm == "alo":
                        mp, mn, c0_ = plo[:, 2:3], plo[:, 3:4], plod[:, 1:2]
                    else:
                        mp, mn, c0_ = pgl[:, 0:1], pgl[:, 1:2], pgld[:, 0:1]
                    out = vS[:, g0:g0 + n] if nm == "v" else zs[nm][:, 0:n]
                    P.ts(out, z[:, 1:n + 1], c0_, None, op0=ALU.mult)
                    P.stt(out, z[:, 0:n], mp, out, ALU.mult, ALU.add)
                    P.stt(out, z[:, 2:n + 2], mn, out, ALU.mult, ALU.add)
                r_ = zs["r"]
                k_ = zs["k"]
                P.ts(kx[:, 0:n], k_[:, 0:n], pch[:, 10:11], None, op0=ALU.mult)
                P.act(ksq[:, 0:n], kx[:, 0:n], AF.Square)
                p2 = nps()
                P.mm(p2[:, 0:n], bones[:], ksq[:, 0:n])
                P.ts(rn[:, 0:n], p2[:, 0:n], 1e-24, None, op0=ALU.max)
                P.recip(rn[:, 0:n], rn[:, 0:n])
                P.act(rn[:, 0:n], rn[:, 0:n], AF.Sqrt)
                P.tt(kk[:, 0:n], kx[:, 0:n], rn[:, 0:n], ALU.mult)
                P.act(tw[:, 0:n], zs["wlo"][:, 0:n], AF.Tanh)
                for d in range(2):
                    ds_ = slice(d * 32, (d + 1) * 32)
                    pw = nps()
                    P.mm(pw[:, 0:n], dw2[ds_, :], tw[ds_, 0:n])
                    P.act(ed[d][:, 0:n], pw[:, 0:n], AF.Sigmoid, bias=pch[:, 6 + d:7 + d])
                    P.act(wd[d][:, 0:n], ed[d][:, 0:n], AF.Exp, scale=-0.6065306597126334)
                    pa = nps()
                    P.mm(pa[:, 0:n], aw2[ds_, :], zs["alo"][ds_, 0:n])
                    P.act(ad[d][:, 0:n], pa[:, 0:n], AF.Sigmoid, bias=pch[:, 8 + d:9 + d])
                    P.stt(nkka[d][:, 0:n], ad[d][:, 0:n], -1.0, kk[:, 0:n], ALU.mult, ALU.mult)
                    P.ts(tq[:, 0:n], ad[d][:, 0:n], pch[:, 11:12], pder[:, 3:4], op0=ALU.mult, op1=ALU.add)
                    P.tt(kd[d][:, 0:n], k_[:, 0:n], tq[:, 0:n], ALU.mult)
                P.tt(tq[:, 0:n], kd[0][:, 0:n], kd[1][:, 0:n], ALU.add)
                P.stt(tq[:, 0:n], tq[:, 0:n], pch[:, 12:13], r_[:, 0:n], ALU.mult, ALU.mult)
                pbn = nps()
                P.mm(pbn[:, 0:n], bones[:], tq[:, 0:n])
                P.tt(bonb[:, 0:n], pbn[:, 0:n], vS[:, g0:g0 + n], ALU.mult)
                P.dma(Sub(bon_d[:, g0:g0 + n], g0), bonb[:, 0:n])
                P.act(sgl[:, 0:n], zs["glo"][:, 0:n], AF.Sigmoid)
                pgt = nps()
                P.mm(pgt[:, 0:n], gg2[:], sgl[:, 0:n])
                P.copy(gtb[:, 0:n], pgt[:, 0:n], eng="scalar")
                P.dma(Sub(gate_d[:, g0:g0 + n], g0), gtb[:, 0:n])
                srcs = [(kk, xkk), (r_, xr)] + [(wd[d], xtm[d][0]) for d in range(2)] + [(nkka[d], xtm[d][1]) for d in range(2)] + [(kd[d], xtm[d][2]) for d in range(2)]
                for (src, dst) in srcs:
                    for t0 in range(0, n, 128):
                        ptr = nps()
                        P.tr(ptr[:, 0:128], src[:, t0:t0 + 128], ident[:])
                        st = tst[ti_ % 4]
                        P.copy(st[:], ptr[:, 0:128], eng=("scalar" if ti_ % 2 else "vector"))
                        ti_ += 1
                        P.dma(Sub(dst[g0 + t0:g0 + t0 + 128, :], (g0 + t0) // 32), st[:])
        if stop <= 4:
            P.barrier()
            ph4.close()
            ph.close()
            P.barrier()
            P.finish()
            return nc
        P.barrier()
        ph4.close()
        ph4 = ExitStack()
        sb2 = lambda n, s, d: ph4.enter_context(nc.sbuf_tensor(n, s, d))
        TB = 32
        S = [sb2("S%d" % i, [128, 64], F32) for i in range(3)]
        junk = sb2("junk", [128, 64], F32)
        sa = sb2("sa", [128, 1], F32)
        xf = [sb2("xf%d" % j, [2, TB, 5, 64], F32) for j in range(2)]
        for d in range(2):
            ops_d = [xkk, xtm[d][0], xtm[d][1], xtm[d][2], xr]
            P.memset(S[0][:], 0.0)
            order = [seqs[1], seqs[0]]
            bidx = 0
            gidx = 0
            for (sname, s0, L, zz) in order:
                blocks = list(range(0, L, TB))
                if d == 1:
                    blocks = blocks[::-1]
                for o in blocks:
                    g0 = s0 + o
                    buf = xf[bidx % 2]
                    bidx += 1
                    for i in range(5):
                        P.dma(buf[:, :, i, :], Sub(ops_d[i][g0:g0 + TB, :].rearrange("t (h j) -> h t j", h=2), None))
                    toks = list(range(TB))
                    if d == 1:
                        toks = toks[::-1]
                    for tq_ in toks:
                        t = g0 + tq_
                        bank = PS[gidx % 8]
                        gidx += 1
                        P.mm(bank[:, 0:320], hsel[:], buf[:, tq_, :, :].rearrange("h i j -> h (i j)"))
                        B = [bank[:, i * 64:(i + 1) * 64] for i in range(5)]
                        P.stt(junk[:], S[0][:], 1.0, B[0], ALU.mult, ALU.mult, accum_out=sa[:, 0:1])
                        P.tt(S[1][:], S[0][:], B[1], ALU.mult)
                        P.stt(S[2][:], B[2], sa[:, 0:1], S[1][:], ALU.mult, ALU.add)
                        P.stt(S[0][:], B[3], vS[:, t:t + 1], S[2][:], ALU.mult, ALU.add)
                        P.stt(junk[:], S[0][:], 1.0, B[4], ALU.mult, ALU.mult, accum_out=Od[d][:, t:t + 1])
        if stop <= 5:
            P.barrier()
            ph4.close()
            ph.close()
            P.barrier()
            P.finish()
            return nc
        P.barrier()
        ph4.close()
        ph4 = ExitStack()
        sb2 = lambda n, s, d: ph4.enter_context(nc.sbuf_tensor(n, s, d))
        ksq = sb2("ksq2", [128, 512], F32)
        rn = sb2("rn2", [128, 512], F32)
        bonb = sb2("bonb2", [128, 512], F32)
        gtb = sb2("gtb2", [128, 512], F32)
        wk = sb2("wk", [128, 512], F32)
        cen = sb2("cen", [128, 512], F32)
        rob = [sb2("rob%d" % i, [128, 512], BF16) for i in range(2)]
        bi = 0
        for o in range(0, TS, 512):
            n = min(512, TS - o)
            P.tt(wk[:, 0:n], Od[0][:, o:o + n], Od[1][:, o:o + n], ALU.add)
            pm = nps()
            P.mm(pm[:, 0:n], bones[:], wk[:, 0:n])
            P.stt(cen[:, 0:n], pm[:, 0:n], -1.0 / 64.0, wk[:, 0:n], ALU.mult, ALU.add)
            P.act(ksq[:, 0:n], cen[:, 0:n], AF.Square)
            pv = nps()
            P.mm(pv[:, 0:n], bones[:], ksq[:, 0:n])
            P.ts(rn[:, 0:n], pv[:, 0:n], 1.0 / 64.0, GN_EPS, op0=ALU.mult, op1=ALU.add)
            P.recip(rn[:, 0:n], rn[:, 0:n])
            P.act(rn[:, 0:n], rn[:, 0:n], AF.Sqrt)
            P.tt(cen[:, 0:n], cen[:, 0:n], rn[:, 0:n], ALU.mult)
            P.ts(cen[:, 0:n], cen[:, 0:n], pch[:, 13:14], pch[:, 14:15], op0=ALU.mult, op1=ALU.add)
            P.dma(bonb[:, 0:n], Sub(bon_d[:, o:o + n], None))
            P.dma(gtb[:, 0:n], Sub(gate_d[:, o:o + n], None))
            P.tt(cen[:, 0:n], cen[:, 0:n], bonb[:, 0:n], ALU.add)
            ob = rob[bi % 2]
            bi += 1
            P.tt(ob[:, 0:n], cen[:, 0:n], gtb[:, 0:n], ALU.mult)
            P.dma(brT_o[1, :, o:o + n], ob[:, 0:n], is_out=True)
        P.barrier()
        ph4.close()
        ph.close()
        P.barrier()
        P.finish()
    return nc


def seq_inputs(cfg, W, li, g, hTs):
    gs = slice(g * 128, (g + 1) * 128)
    w_in = W["w_in"][li]
    zr0 = OFF_RWKV
    cols_fm = np.concatenate([
        np.arange(zr0 + g * 128, zr0 + g * 128 + 128), np.arange(zr0 + 512 + g * 128, zr0 + 512 + g * 128 + 128),
        np.arange(zr0 + 1024 + g * 128, zr0 + 1024 + g * 128 + 128), np.arange(zr0 + 1536, zr0 + 1760),
        np.arange(OFF_Q + g * 128, OFF_Q + g * 128 + 128), np.arange(OFF_K + g * 128, OFF_K + g * 128 + 128)])
    cols_tm = np.concatenate([np.arange(g * 128, g * 128 + 128), np.arange(OFF_V + g * 128, OFF_V + g * 128 + 128)])
    mu = W["shift_mu"][li]
    pch = np.zeros((128, 18), np.float32)
    pch[:, 0] = mu[0, gs]; pch[:, 1] = mu[1, gs]
    pch[:, 2] = mu[0, 512 + g * 128:512 + g * 128 + 128]; pch[:, 3] = mu[1, 512 + g * 128:512 + g * 128 + 128]
    pch[:, 4] = mu[0, 1024 + g * 128:1024 + g * 128 + 128]; pch[:, 5] = mu[1, 1024 + g * 128:1024 + g * 128 + 128]
    pch[:, 6] = W["decay_w0"][li][0, gs]; pch[:, 7] = W["decay_w0"][li][1, gs]
    pch[:, 8] = W["iclr_a0"][li][0, gs]; pch[:, 9] = W["iclr_a0"][li][1, gs]
    pch[:, 10] = W["key_kk"][li][gs]; pch[:, 11] = W["key_ka"][li][gs]
    pch[:, 12] = W["bonus_rk"][li].reshape(-1)[gs]
    pch[:, 13] = W["gn_g"][li][gs]; pch[:, 14] = W["gn_b"][li][gs]
    pch[:, 15] = W["pool_scale"][li][gs]
    pch[:, 16] = np.tile(W["nat_qn_g"][li], 2); pch[:, 17] = np.tile(W["nat_kn_g"][li], 2)
    plo = np.stack([mu[0, 1536:1600], mu[1, 1536:1600], mu[0, 1600:1664], mu[1, 1600:1664]], axis=1)
    pgl = np.stack([mu[0, 1664:1760], mu[1, 1664:1760]], axis=1)
    dw2 = W["decay_w2"][li][:, :, gs].reshape(64, 128)
    aw2 = W["iclr_a2"][li][:, :, gs].reshape(64, 128)
    gg2 = W["gate_g2"][li][:, gs]
    plan, tiles = nat_geometry(cfg.T, cfg.GW)
    rpb = W["nat_rpb"][li]
    nbias = np.stack([np.stack([rpb[2 * g + hh][ro, co] * valid for hh in range(2)]) for (valid, ro, co) in tiles]).astype(np.float32)
    nmask = np.stack([valid.astype(np.float32) for (valid, ro, co) in tiles])
    bones = np.kron(np.eye(2, dtype=np.float32), np.ones((64, 64), np.float32))
    hsel = np.kron(np.eye(2, dtype=np.float32), np.ones((1, 64), np.float32))
    opad = np.zeros((2, 128, 128), np.float32)
    opad[0, :, 0:64] = 1.0
    opad[1, :, 64:128] = 1.0
    f = lambda a: np.ascontiguousarray(a, dtype=np.float32)
    return dict(hTs=hTs, w_fm=f(w_in[:, cols_fm]), w_tm=f(w_in[:, cols_tm]), pch=pch, plo=f(plo), pgl=f(pgl), dw2=f(dw2), aw2=f(aw2),
                gg2=f(gg2), poolw=f(W["pool_w"][li][g]), bands=pool_bands(POOL_WINDOWS[g]), nbias=nbias, nmask=nmask,
                ident=np.eye(128, dtype=np.float32), bones=bones, hsel=hsel, opad=opad)


import ml_dtypes

_BF = ml_dtypes.bfloat16
_PROGS = {}


def _prog(key, fn):
    if key not in _PROGS:
        _PROGS[key] = fn()
    return _PROGS[key]


def _run(nc, in_maps):
    res = run_bass_kernel_spmd(nc, in_maps, core_ids=list(range(8)))
    return res.results


def kernel(**inputs):
    W = {k: np.asarray(v) for k, v in inputs.items()}
    x = W["x"]
    ctx = W["ctx"]
    Bsz, T, D = x.shape
    CTX = ctx.shape[1]
    DFF = W["ffn_w1"].shape[2]
    NE = W["router"].shape[2]
    depth = W["w_mod"].shape[0]
    cfg = Cfg(T=T, CTX=CTX, DFF=DFF, NE=NE)
    TL, CL, NTOK, TS = cfg.TL, cfg.CL, cfg.NTOK, cfg.TS
    f32 = lambda a: np.ascontiguousarray(a, dtype=np.float32)
    ones = np.ones((128, 128), np.float32)
    ident = np.eye(128, dtype=np.float32)
    cores = [(b, j) for b in range(2) for j in range(4)]
    xT = []
    for (b, j) in cores:
        a = np.zeros((D, NTOK), np.float32)
        a[:, :TL] = x[b, j * TL:(j + 1) * TL].T
        a[:, TL:TL + CL] = ctx[b, j * CL:(j + 1) * CL].T
        xT.append(a)
    cvecs = [f32(np.stack([W["c"][b], W["c_ctx"]], axis=1)) for b in range(2)]
    ncA = _prog(("A", T, CTX), lambda: build_tok(cfg, "A"))
    ncS = _prog(("S", T, CTX), lambda: build_seq(cfg))
    for li in range(depth):
        moe = (li % 2 == 1)
        jj = li // 2
        wm = W["w_mod"][li]
        bm = W["b_mod"][li]
        wmA = f32(wm[:, :2 * D])
        bmA = f32(bm[:2 * D])
        gmix = f32(W["norm_mix_g"][li])
        mapsA = [dict(xT=xT[c], cvec=cvecs[b], w_mod=wmA, b_mod=bmA, g_mix=gmix, ones=ones, ident=ident) for c, (b, j) in enumerate(cores)]
        rA = _run(ncA, mapsA)
        hTs = []
        for b in range(2):
            a = np.zeros((D, TS), _BF)
            for j in range(4):
                h = rA[b * 4 + j]["hT"]
                a[:, j * TL:(j + 1) * TL] = h[:, :TL]
                a[:, T + j * CL:T + (j + 1) * CL] = h[:, TL:TL + CL]
            hTs.append(a)
        mapsS = [seq_inputs(cfg, W, li, g, hTs[b]) for (b, g) in cores]
        rS = _run(ncS, mapsS)
        brs = []
        for (b, j) in cores:
            a = np.zeros((3, 512, NTOK), _BF)
            for g in range(4):
                o = rS[b * 4 + g]["brTo"]
                a[:, g * 128:(g + 1) * 128, :TL] = o[:, :, j * TL:(j + 1) * TL]
                a[:, g * 128:(g + 1) * 128, TL:TL + CL] = o[:, :, T + j * CL:T + (j + 1) * CL]
            brs.append(a)
        ncB = _prog(("B", T, CTX, DFF, moe), lambda: build_tok(cfg, "B", moe=moe))
        common = dict(w_mod=f32(wm), b_mod=f32(bm), g_mix=gmix, g_ffn=f32(W["norm_ffn_g"][li]),
                      w_gate=f32(W["w_in"][li][:, OFF_GATE:]), w_branch=f32(W["w_branch"][li]), w_out=f32(W["w_out"][li]),
                      ones=ones, ident=ident)
        if moe:
            common.update(router=f32(W["router"][jj]), w1=f32(W["moe_w1"][jj]), w3=f32(W["moe_w3"][jj]), w2=f32(W["moe_w2"][jj]))
        else:
            common.update(w1=f32(W["ffn_w1"][jj][None]), w3=f32(W["ffn_w3"][jj][None]), w2=f32(W["ffn_w2"][jj][None]))
        mapsB = [dict(xT=xT[c], cvec=cvecs[b], brT=brs[c], **common) for c, (b, j) in enumerate(cores)]
        rB = _run(ncB, mapsB)
        xT = [f32(rB[c]["xo"]) for c in range(8)]
    out = np.zeros((Bsz, T, D), np.float32)
    for c, (b, j) in enumerate(cores):
        out[b, j * TL:(j + 1) * TL] = xT[c][:, :TL].T
    return out
```

```python
import os
from contextlib import ExitStack
import numpy as np
import concourse.bass as bass
import concourse.mybir as mybir
from concourse.bass_utils import run_bass_kernel_spmd


F32 = mybir.dt.float32
BF16 = mybir.dt.bfloat16
AF = mybir.ActivationFunctionType
ALU = mybir.AluOpType
AX = mybir.AxisListType

ENGS = ("tensor", "vector", "scalar", "gpsimd", "sync")
NDMA_SEM = 40
NDMA_HW = 32


class Sub:
    def __init__(self, ap, sub):
        self.ap = ap
        self.sub = sub


def _ak(x):
    if isinstance(x, Sub):
        return x.ap, (x.ap.name, x.sub)
    return x, (x.name, None)


class Prog:
    def __init__(self, nc, sems=None):
        self.nc = nc
        self.sems = sems
        self.ops = {e: [] for e in ENGS}
        self.cnt = {e: 0 for e in ENGS}
        self.st = {}
        self.waited = {e: {} for e in ENGS}
        self.ndma = 0
        self.ndma_sw = 0
        self.dma_cnt = [0] * NDMA_SEM
        self.out_tokens = []
        self.n_instr = 0

    def _deps(self, eng, reads, writes):
        raw = {}
        oth = {}

        def add(dst, tok):
            s, v = tok
            if dst.get(s, -1) < v:
                dst[s] = v

        for key in reads:
            name, sub = key
            d = self.st.setdefault(name, {})
            if sub not in d:
                d[sub] = {"w": None, "r": {}}
            subs = list(d.keys()) if sub is None else ([sub] + ([None] if None in d else []))
            for s in subs:
                if d[s]["w"] is not None:
                    add(raw, d[s]["w"])
        for key in writes:
            name, sub = key
            d = self.st.setdefault(name, {})
            if sub not in d:
                d[sub] = {"w": None, "r": {}}
            subs = list(d.keys()) if sub is None else ([sub] + ([None] if None in d else []))
            for s in subs:
                if d[s]["w"] is not None:
                    add(oth, d[s]["w"])
                for rs, rv in d[s]["r"].items():
                    add(oth, (rs, rv))
        return raw, oth

    def _commit(self, tok, reads, writes):
        for key in reads:
            name, sub = key
            e = self.st[name][sub]
            s, v = tok
            if e["r"].get(s, -1) < v:
                e["r"][s] = v
        for key in writes:
            name, sub = key
            d = self.st[name]
            if sub is None:
                for s in d:
                    d[s] = {"w": tok, "r": {}}
            else:
                d[sub] = {"w": tok, "r": {}}

    def _emit_waits(self, eng, raw, oth):
        own = "E_" + eng
        need = {}
        for s, v in raw.items():
            need[s] = max(need.get(s, -1), v)
        for s, v in oth.items():
            if s == own and eng == "tensor":
                continue
            need[s] = max(need.get(s, -1), v)
        wl = []
        for s, v in need.items():
            if self.waited[eng].get(s, -1) >= v:
                continue
            self.waited[eng][s] = v
            wl.append((s, v))
        return wl

    def op(self, eng, fn, reads, writes, is_dma=False, is_out=False):
        rk = [_ak(x)[1] for x in reads]
        wk = [_ak(x)[1] for x in writes]
        raw, oth = self._deps(eng, rk, wk)
        if is_dma:
            if eng == "gpsimd":
                i = NDMA_HW + (self.ndma_sw % (NDMA_SEM - NDMA_HW))
                self.ndma_sw += 1
            else:
                i = self.ndma % NDMA_HW
                self.ndma += 1
            prev = self.dma_cnt[i]
            self.dma_cnt[i] += 16
            tok = ("D_%d" % i, self.dma_cnt[i])
            if prev > 0:
                oth[tok[0]] = max(oth.get(tok[0], -1), prev)
        else:
            self.cnt[eng] += 1
            tok = ("E_" + eng, self.cnt[eng])
        wl = self._emit_waits(eng, raw, oth)
        eobj = getattr(self.nc, eng)
        for s_, v_ in wl:
            eobj.wait_ge(self.sems[s_], v_)
        ins = fn(eobj)
        ins.then_inc(self.sems[tok[0]], 16 if is_dma else 1)
        self._commit(tok, rk, wk)
        if is_out:
            self.out_tokens.append(tok)
        self.n_instr += 1
        return tok

    def dma(self, out, in_, q="sync", is_out=False, **kw):
        o, _ = _ak(out)
        i, _ = _ak(in_)
        return self.op(q, lambda e: e.dma_start(out=o, in_=i, **kw), [in_], [out], is_dma=True, is_out=is_out)

    def mm(self, out, lhsT, rhs, start=True, stop=True, **kw):
        o, _ = _ak(out)
        a, _ = _ak(lhsT)
        b, _ = _ak(rhs)
        rd = [lhsT, rhs] + ([] if start else [out])
        return self.op("tensor", lambda e: e.matmul(o, a, b, start=start, stop=stop, **kw), rd, [out])

    def tr(self, out, in_, ident):
        o, _ = _ak(out)
        a, _ = _ak(in_)
        b, _ = _ak(ident)
        return self.op("tensor", lambda e: e.transpose(o, a, b), [in_, ident], [out])

    def act(self, out, in_, func, bias=None, scale=None, accum_out=None, eng="scalar"):
        o, _ = _ak(out)
        a, _ = _ak(in_)
        rd = [in_]
        wr = [out]
        kw = {}
        if bias is not None:
            if isinstance(bias, (int, float)):
                kw["bias"] = float(bias)
            else:
                kw["bias"] = _ak(bias)[0]
                rd.append(bias)
        if scale is not None:
            if isinstance(scale, (int, float)):
                kw["scale"] = float(scale)
            else:
                kw["scale"] = _ak(scale)[0]
                rd.append(scale)
        if accum_out is not None:
            kw["accum_out"] = _ak(accum_out)[0]
            wr.append(accum_out)
        return self.op(eng, lambda e: e.activation(o, a, func, **kw), rd, wr)

    def tt(self, out, a, b, op, eng="vector"):
        o, _ = _ak(out)
        x, _ = _ak(a)
        y, _ = _ak(b)
        return self.op(eng, lambda e: e.tensor_tensor(o, x, y, op), [a, b], [out])

    def ts(self, out, a, s1, s2=None, op0=ALU.mult, op1=None, eng="vector", accum_out=None):
        o, _ = _ak(out)
        x, _ = _ak(a)
        rd = [a]
        wr = [out]

        def sc(s):
            if s is None or isinstance(s, (int, float)):
                return None if s is None else float(s)
            rd.append(s)
            return _ak(s)[0]

        v1 = sc(s1)
        v2 = sc(s2)
        kw = {}
        if op1 is not None:
            kw["op1"] = op1
        if accum_out is not None:
            kw["accum_out"] = _ak(accum_out)[0]
            wr.append(accum_out)
        return self.op(eng, lambda e: e.tensor_scalar(o, x, v1, v2, op0, **kw), rd, wr)

    def stt(self, out, in0, scalar, in1, op0, op1, eng="vector", accum_out=None):
        o, _ = _ak(out)
        x, _ = _ak(in0)
        y, _ = _ak(in1)
        rd = [in0, in1]
        wr = [out]
        if isinstance(scalar, (int, float)):
            sv = float(scalar)
        else:
            sv = _ak(scalar)[0]
            rd.append(scalar)
        kw = {}
        if accum_out is not None:
            kw["accum_out"] = _ak(accum_out)[0]
            wr.append(accum_out)
        return self.op(eng, lambda e: e.scalar_tensor_tensor(o, x, sv, y, op0, op1, **kw), rd, wr)

    def copy(self, out, in_, eng="vector"):
        o, _ = _ak(out)
        a, _ = _ak(in_)
        if eng == "scalar":
            return self.op(eng, lambda e: e.copy(o, a), [in_], [out])
        return self.op(eng, lambda e: e.tensor_copy(o, a), [in_], [out])

    def memset(self, out, val, eng="vector"):
        o, _ = _ak(out)
        return self.op(eng, lambda e: e.memset(o, val), [], [out])

    def reduce(self, out, in_, op, axis=AX.X, eng="vector"):
        o, _ = _ak(out)
        a, _ = _ak(in_)
        return self.op(eng, lambda e: e.tensor_reduce(o, a, axis, op), [in_], [out])

    def recip(self, out, in_):
        o, _ = _ak(out)
        a, _ = _ak(in_)
        return self.op("vector", lambda e: e.reciprocal(o, a), [in_], [out])

    def barrier(self):
        toks = {}
        for e in ENGS:
            if self.cnt[e] > 0:
                toks["E_" + e] = self.cnt[e]
        for i in range(NDMA_SEM):
            if self.dma_cnt[i] > 0:
                toks["D_%d" % i] = self.dma_cnt[i]
        for e in ENGS:
            eobj = getattr(self.nc, e)
            for s_, v_ in toks.items():
                if s_ == "E_" + e:
                    continue
                if self.waited[e].get(s_, -1) >= v_:
                    continue
                self.waited[e][s_] = v_
                eobj.wait_ge(self.sems[s_], v_)

    def finish(self):
        fin = {}
        for s_, v_ in self.out_tokens:
            fin[s_] = max(fin.get(s_, -1), v_)
        for s_, v_ in fin.items():
            self.nc.sync.wait_ge(self.sems[s_], v_)

    def finalize_old(self, block, sems):
        fin = {}
        for s, v in self.out_tokens:
            fin[s] = max(fin.get(s, -1), v)
        ops = self.ops

        def run(eng_name, eng):
            for wl, fn, tok, is_dma in ops[eng_name]:
                for s, v in wl:
                    eng.wait_ge(sems[s], v)
                ins = fn(eng)
                ins.then_inc(sems[tok[0]], 16 if is_dma else 1)
            if eng_name == "sync":
                for s, v in fin.items():
                    eng.wait_ge(sems[s], v)

        @block.tensor
        def _(e):
            run("tensor", e)

        @block.vector
        def _(e):
            run("vector", e)

        @block.scalar
        def _(e):
            run("scalar", e)

        @block.gpsimd
        def _(e):
            run("gpsimd", e)

        @block.sync
        def _(e):
            run("sync", e)


RMS_EPS = 1e-6


class Cfg:
    def __init__(self, T=8192, CTX=256, DFF=2816, NE=8, GW=64):
        self.D = 1024
        self.T = T
        self.CTX = CTX
        self.DFF = DFF
        self.NE = NE
        self.GW = GW
        self.TL = T // 4
        self.CL = CTX // 4
        self.NTOK = self.TL + 128
        self.TS = T + CTX
        blocks = []
        o = 0
        while o < self.TL:
            n = min(512, self.TL - o)
            blocks.append((o, n, 0))
            o += n
        blocks.append((self.TL, 128, 1))
        self.blocks = blocks
        h = len(blocks) // 2
        self.halves = [blocks[:h], blocks[h:]]


def mk_sems(nc, es):
    sems = {}
    for e in ENGS:
        sems["E_" + e] = es.enter_context(nc.semaphore("s_" + e))
    for i in range(NDMA_SEM):
        sems["D_%d" % i] = es.enter_context(nc.semaphore("d_%d" % i))
    return sems


def build_tok(cfg, mode, moe=False):
    D, NTOK, DFF, NE = cfg.D, cfg.NTOK, cfg.DFF, cfg.NE
    NF = DFF // 128
    nc = bass.Bass("TRN2", target_bir_lowering=False)
    dr = lambda n, s, d, k="ExternalInput": nc.dram_tensor(n, s, d, kind=k).ap()
    xT = dr("xT", [D, NTOK], F32)
    cvec = dr("cvec", [D, 2], F32)
    nmod = 2 * D if mode == "A" else 6 * D
    w_mod = dr("w_mod", [D, nmod], F32)
    b_mod = dr("b_mod", [nmod], F32)
    g_mix = dr("g_mix", [D], F32)
    ones_d = dr("ones", [128, 128], F32)
    ident_d = dr("ident", [128, 128], F32)
    if mode == "A":
        hT_o = dr("hT", [D, NTOK], BF16, "ExternalOutput")
    else:
        g_ffn = dr("g_ffn", [D], F32)
        brT = dr("brT", [3, 512, NTOK], BF16)
        w_gate = dr("w_gate", [D, 3 * D], F32)
        w_branch = dr("w_branch", [3, 512, D], F32)
        w_out = dr("w_out", [D, D], F32)
        if moe:
            router = dr("router", [D, NE], F32)
            w1 = dr("w1", [NE, D, DFF], F32)
            w3 = dr("w3", [NE, D, DFF], F32)
            w2 = dr("w2", [NE, DFF, D], F32)
        else:
            w1 = dr("w1", [1, D, DFF], F32)
            w3 = dr("w3", [1, D, DFF], F32)
            w2 = dr("w2", [1, DFF, D], F32)
        x_o = dr("xo", [D, NTOK], F32, "ExternalOutput")

    with ExitStack() as es:
        sb = lambda n, s, d: es.enter_context(nc.sbuf_tensor(n, s, d))
        ps = lambda n, s, d: es.enter_context(nc.psum_tensor(n, s, d))
        xs = sb("xs", [128, 8, NTOK], F32)
        hT = sb("hTs", [128, 8, NTOK], BF16)
        cv = sb("cv", [128, 8, 2], F32)
        siluc = sb("siluc", [128, 8, 2], BF16)
        nch = nmod // 128
        modT = sb("modT", [128, nch, 2], F32)
        bmod = sb("bmod", [128, nch], F32)
        gmix = sb("gmix", [128, 8], F32)
        A1 = sb("A1", [128, 8, 2], F32)
        ones = sb("ones_s", [128, 128], F32)
        ident = sb("ident_s", [128, 128], F32)
        sq = [sb("sq%d" % i, [128, 512], F32) for i in range(2)]
        rstd = sb("rstd", [128, 512], F32)
        tmpf = [sb("tmpf%d" % i, [128, 512], F32) for i in range(2)]
        PS = [ps("ps%d" % i, [128, 512], F32) for i in range(7)]
        if mode == "B":
            gffn = sb("gffn", [128, 8], F32)
            A2 = sb("A2", [128, 8, 2], F32)
            sig = [sb("sig%d" % i, [128, 512], F32) for i in range(2)]
            if moe:
                ntile = NTOK // 128
                gates = sb("gates", [128, ntile, NE], F32)
        sems = mk_sems(nc, es)
        P = Prog(nc, sems)
        psi = [0]

        def nps():
            psi[0] = (psi[0] + 1) % len(PS)
            return PS[psi[0]]

        P.dma(ones[:], ones_d)
        P.dma(ident[:], ident_d)
        for kc in range(8):
            P.dma(xs[:, kc, :], xT[kc * 128:(kc + 1) * 128, :], q=("sync" if kc % 2 == 0 else "gpsimd"))
        with nc.allow_non_contiguous_dma("small param loads"):
            P.dma(cv[:], cvec.rearrange("(c p) n -> p c n", p=128))
            P.dma(bmod[:], b_mod.rearrange("(c p) -> p c", p=128))
            P.dma(gmix[:], g_mix.rearrange("(c p) -> p c", p=128))
            if mode == "B":
                P.dma(gffn[:], g_ffn.rearrange("(c p) -> p c", p=128))
        P.act(siluc[:], cv[:], AF.Silu)
        wi = 0
        ph0 = ExitStack()
        wm = [ph0.enter_context(nc.sbuf_tensor("wm%d" % i, [128, 8, 512], BF16)) for i in range(2)]
        for g0 in range(0, nch, 4):
            w = wm[wi % 2]
            wi += 1
            P.dma(w[:], w_mod[:, g0 * 128:(g0 + 4) * 128].rearrange("(k p) m -> p k m", p=128), q="gpsimd")
            for j in range(4):
                mc = g0 + j
                pt = nps()
                for kc in range(8):
                    P.mm(pt[:, 0:2], w[:, kc, j * 128:(j + 1) * 128], siluc[:, kc, :], start=(kc == 0), stop=(kc == 7))
                P.ts(modT[:, mc, :], pt[:, 0:2], bmod[:, mc:mc + 1], None, op0=ALU.add)
        P.barrier()
        ph0.close()
        def mkA(Aout, gvec, sc_base):
            for kc in range(8):
                P.ts(Aout[:, kc, :], modT[:, sc_base + kc, :], 1.0, gvec[:, kc:kc + 1], op0=ALU.add, op1=ALU.mult)

        mkA(A1, gmix, 8)
        if mode == "B":
            mkA(A2, gffn, 32)

        def norm(Acoef, sh_base, want_f32=None):
            for (o, n, col) in cfg.blocks:
                pt = nps()
                for kc in range(8):
                    s = sq[kc % 2]
                    P.act(s[:, 0:n], xs[:, kc, o:o + n], AF.Square)
                    P.mm(pt[:, 0:n], ones[:], s[:, 0:n], start=(kc == 0), stop=(kc == 7))
                P.ts(rstd[:, 0:n], pt[:, 0:n], 1.0 / 1024.0, RMS_EPS, op0=ALU.mult, op1=ALU.add)
                P.recip(rstd[:, 0:n], rstd[:, 0:n])
                P.act(rstd[:, 0:n], rstd[:, 0:n], AF.Sqrt)
                for kc in range(8):
                    t = tmpf[kc % 2]
                    P.tt(t[:, 0:n], xs[:, kc, o:o + n], rstd[:, 0:n], ALU.mult)
                    P.act(hT[:, kc, o:o + n], t[:, 0:n], AF.Identity, bias=modT[:, sh_base + kc, col:col + 1], scale=Acoef[:, kc, col:col + 1])
                    if want_f32 is not None:
                        P.act(hf[:, kc, 0:n], t[:, 0:n], AF.Identity, bias=modT[:, sh_base + kc, col:col + 1], scale=Acoef[:, kc, col:col + 1])
                if want_f32 is not None:
                    want_f32(o, n)

        norm(A1, 0)
        if mode == "A":
            for kc in range(8):
                P.dma(hT_o[kc * 128:(kc + 1) * 128, :], hT[:, kc, :], is_out=True)
            P.barrier()
            P.finish()
            return nc

        P.barrier()
        with ExitStack() as ph:
            sb2 = lambda n, s_, d: ph.enter_context(nc.sbuf_tensor(n, s_, d))
            br = sb2("br", [128, 3, 4, 512], BF16)
            mT = sb2("mT", [128, 8, 512], BF16)
            macc = sb2("macc", [128, 512], F32)
            wg = [sb2("wg%d" % i, [128, 8, 128], BF16) for i in range(3)]
            wb = [sb2("wb%d" % i, [128, 4, 128], BF16) for i in range(3)]
            wi = 0
            for (o, n, col) in cfg.blocks:
                for n_ in range(3):
                    for g in range(4):
                        P.dma(br[:, n_, g, 0:n], brT[n_, g * 128:(g + 1) * 128, o:o + n], q="sync")
                for dc in range(8):
                    for n_ in range(3):
                        wgt = wg[wi % 3]
                        wbt = wb[wi % 3]
                        wi += 1
                        c0 = n_ * D + dc * 128
                        P.dma(wgt[:], w_gate[:, c0:c0 + 128].rearrange("(k p) m -> p k m", p=128), q="gpsimd")
                        P.dma(wbt[:], w_branch[n_, :, dc * 128:(dc + 1) * 128].rearrange("(k p) m -> p k m", p=128), q="gpsimd")
                        pg = nps()
                        for kc in range(8):
                            P.mm(pg[:, 0:n], wgt[:, kc, :], hT[:, kc, o:o + n], start=(kc == 0), stop=(kc == 7))
                        pp = nps()
                        for g in range(4):
                            P.mm(pp[:, 0:n], wbt[:, g, :], br[:, n_, g, 0:n], start=(g == 0), stop=(g == 3))
                        sg = sig[wi % 2]
                        P.act(sg[:, 0:n], pg[:, 0:n], AF.Sigmoid)
                        if n_ == 0:
                            P.tt(macc[:, 0:n], sg[:, 0:n], pp[:, 0:n], ALU.mult)
                        else:
                            P.tt(sg[:, 0:n], sg[:, 0:n], pp[:, 0:n], ALU.mult)
                            P.tt(macc[:, 0:n], macc[:, 0:n], sg[:, 0:n], ALU.add, eng="gpsimd")
                    P.copy(mT[:, dc, 0:n], macc[:, 0:n], eng="scalar")
                for dc in range(8):
                    wgt = wg[wi % 3]
                    wi += 1
                    P.dma(wgt[:], w_out[:, dc * 128:(dc + 1) * 128].rearrange("(k p) m -> p k m", p=128), q="gpsimd")
                    py = nps()
                    for kc in range(8):
                        P.mm(py[:, 0:n], wgt[:, kc, :], mT[:, kc, 0:n], start=(kc == 0), stop=(kc == 7))
                    P.stt(xs[:, dc, o:o + n], py[:, 0:n], modT[:, 16 + dc, col:col + 1], xs[:, dc, o:o + n], ALU.mult, ALU.add)
            P.barrier()

        ph = ExitStack()
        sb2 = lambda n, s_, d: ph.enter_context(nc.sbuf_tensor(n, s_, d))
        if moe:
            hf = sb2("hf", [128, 8, 512], F32)
            rt = sb2("rt", [128, 8, NE], F32)
            lg = sb2("lg", [128, NE], F32)
            l2 = sb2("l2", [128, NE], F32)
            m1 = sb2("m1", [128, 1], F32)
            m2 = sb2("m2", [128, 1], F32)
            nm1 = sb2("nm1", [128, 1], F32)
            den = sb2("den", [128, 1], F32)
            ex = sb2("ex", [128, NE], F32)
            with nc.allow_non_contiguous_dma("router"):
                P.dma(rt[:], router.rearrange("(k p) e -> p k e", p=128))

            def route(o, n):
                for t0 in range(0, n, 128):
                    ti = (o + t0) // 128
                    pl = nps()
                    for kc in range(8):
                        P.mm(pl[:, 0:NE], hf[:, kc, t0:t0 + 128], rt[:, kc, :], start=(kc == 0), stop=(kc == 7))
                    P.copy(lg[:], pl[:, 0:NE])
                    P.reduce(m1[:], lg[:], ALU.max)
                    P.ts(l2[:], lg[:], m1[:, 0:1], -1e30, op0=ALU.is_equal, op1=ALU.mult)
                    P.tt(l2[:], l2[:], lg[:], ALU.add)
                    P.reduce(m2[:], l2[:], ALU.max)
                    P.ts(nm1[:], m1[:], -1.0, None, op0=ALU.mult)
                    P.act(ex[:], lg[:], AF.Exp, bias=nm1[:, 0:1])
                    P.act(den[:], m2[:], AF.Exp, bias=nm1[:, 0:1])
                    P.ts(den[:], den[:], 1.0, None, op0=ALU.add)
                    P.recip(den[:], den[:])
                    P.ts(l2[:], lg[:], m2[:, 0:1], None, op0=ALU.is_ge)
                    P.tt(l2[:], l2[:], ex[:], ALU.mult)
                    P.ts(gates[:, ti, :], l2[:], den[:, 0:1], None, op0=ALU.mult)

            norm(A2, 24, want_f32=route)
        else:
            norm(A2, 24)

        P.barrier()
        ph.close()
        ph = ExitStack()
        sb2 = lambda n, s_, d: ph.enter_context(nc.sbuf_tensor(n, s_, d))
        maxhalf = max(sum(n for _, n, _ in h) for h in cfg.halves)
        actT = sb2("actT", [128, NF, maxhalf], BF16)
        w1b = [sb2("w1b%d" % i, [128, 8, 128], BF16) for i in range(3)]
        w3b = [sb2("w3b%d" % i, [128, 8, 128], BF16) for i in range(3)]
        w2b = [sb2("w2b%d" % i, [128, NF, 128], BF16) for i in range(2)]
        if moe:
            gcol = sb2("gcol", [128, 128], F32)
            gateB = [sb2("gateB%d" % i, [128, 512], F32) for i in range(3)]
        nexp = NE if moe else 1
        wi = 0
        w2i = 0
        gi = 0
        for half in cfg.halves:
            hb = half[0][0]
            for e in range(nexp):
                for fc in range(NF):
                    a = w1b[wi % 3]
                    b = w3b[wi % 3]
                    wi += 1
                    P.dma(a[:], w1[e, :, fc * 128:(fc + 1) * 128].rearrange("(k p) m -> p k m", p=128), q="gpsimd")
                    P.dma(b[:], w3[e, :, fc * 128:(fc + 1) * 128].rearrange("(k p) m -> p k m", p=128), q="gpsimd")
                    for (o, n, col) in half:
                        p1 = nps()
                        for kc in range(8):
                            P.mm(p1[:, 0:n], a[:, kc, :], hT[:, kc, o:o + n], start=(kc == 0), stop=(kc == 7))
                        p3 = nps()
                        for kc in range(8):
                            P.mm(p3[:, 0:n], b[:, kc, :], hT[:, kc, o:o + n], start=(kc == 0), stop=(kc == 7))
                        sg = sig[gi % 2]
                        gi += 1
                        P.act(sg[:, 0:n], p1[:, 0:n], AF.Silu)
                        P.tt(actT[:, fc, o - hb:o - hb + n], sg[:, 0:n], p3[:, 0:n], ALU.mult)
                gB = {}
                if moe:
                    for (o, n, col) in half:
                        gb = gateB[gi % 3]
                        gi += 1
                        for t0 in range(0, n, 128):
                            ti = (o + t0) // 128
                            P.copy(gcol[:], gates[:, ti, e:e + 1].to_broadcast([128, 128]))
                            pgm = nps()
                            P.mm(pgm[:, 0:128], gcol[:], ident[:])
                            P.copy(gb[:, t0:t0 + 128], pgm[:, 0:128], eng="scalar")
                        gB[o] = gb
                for dc in range(8):
                    c = w2b[w2i % 2]
                    w2i += 1
                    P.dma(c[:], w2[e, :, dc * 128:(dc + 1) * 128].rearrange("(k p) m -> p k m", p=128), q="gpsimd")
                    for (o, n, col) in half:
                        py = nps()
                        for fc in range(NF):
                            P.mm(py[:, 0:n], c[:, fc, :], actT[:, fc, o - hb:o - hb + n], start=(fc == 0), stop=(fc == NF - 1))
                        if moe:
                            t = tmpf[dc % 2]
                            P.tt(t[:, 0:n], py[:, 0:n], gB[o][:, 0:n], ALU.mult)
                            P.stt(xs[:, dc, o:o + n], t[:, 0:n], modT[:, 40 + dc, col:col + 1], xs[:, dc, o:o + n], ALU.mult, ALU.add)
                        else:
                            P.stt(xs[:, dc, o:o + n], py[:, 0:n], modT[:, 40 + dc, col:col + 1], xs[:, dc, o:o + n], ALU.mult, ALU.add)
        for kc in range(8):
            P.dma(x_o[kc * 128:(kc + 1) * 128, :], xs[:, kc, :], is_out=True)
        P.barrier()
        ph.close()
        P.barrier()
        P.finish()
    return nc


DBG = int(os.environ.get('SEQ_DBG', '47'))

GN_EPS = 64e-5
NAT_KH_MAX = 8
NAT_KW = 16
POOL_WINDOWS = (2, 4, 8, 16)
OFF_RWKV = 512
OFF_Q = 2272
OFF_K = 2784
OFF_V = 3296
OFF_GATE = 3808
FM_BLOCKS = [("r", 128), ("k", 128), ("v", 128), ("wlo", 64), ("alo", 64), ("glo", 96), ("q", 128), ("kn", 128)]
NFM = sum(n for _, n in FM_BLOCKS)


def nat_geometry(T, GW=64):
    rows = T // GW
    kh = min(NAT_KH_MAX, rows)
    ntl = rows // 2
    rs = lambda r: min(max(r - kh // 2, 0), rows - kh)
    cs = lambda c: min(max(c - NAT_KW // 2, 0), GW - NAT_KW)
    qi = np.arange(128)
    ki = np.arange(128)
    uniq = {}
    tiles = []
    plan = []
    for m in range(ntl):
        r = 2 * m + qi // GW
        c = qi % GW
        rsv = np.array([rs(x) for x in r])
        csv = np.array([cs(x) for x in c])
        lo = min(rsv) // 2
        hi = (max(rsv) + kh - 1) // 2
        lst = []
        for kt in range(lo, hi + 1):
            kr = 2 * kt + ki // GW
            kc = ki % GW
            valid = ((kr[:, None] >= rsv[None, :]) & (kr[:, None] < rsv[None, :] + kh) &
                     (kc[:, None] >= csv[None, :]) & (kc[:, None] < csv[None, :] + NAT_KW))
            if not valid.any():
                continue
            ro = (kr[:, None] - r[None, :] + NAT_KH_MAX - 1) * valid
            co = (kc[:, None] - c[None, :] + NAT_KW - 1) * valid
            key = (valid.tobytes(), ro.tobytes(), co.tobytes())
            if key not in uniq:
                uniq[key] = len(tiles)
                tiles.append((valid, ro, co))
            lst.append((kt, uniq[key]))
        plan.append(lst)
    return plan, tiles


def pool_bands(W):
    Tt = 512
    t = np.arange(Tt)
    lo = np.clip(t - W // 2, 0, Tt)
    hi = np.clip(t - W // 2 + W, 0, Tt)
    M = np.zeros((Tt, Tt), np.float32)
    for j in range(Tt):
        M[lo[j]:hi[j], j] = 1.0 / float(hi[j] - lo[j])
    M -= np.eye(Tt, dtype=np.float32)
    b = np.stack([M[0:128, 128:256], M[128:256, 128:256], M[256:384, 128:256], M[0:128, 0:128], M[384:512, 384:512]])
    return np.ascontiguousarray(b)


def build_seq(cfg, stop=99):
    D, T, CTX, TS = cfg.D, cfg.T, cfg.CTX, cfg.TS
    NTL = T // 128
    NTC = CTX // 128
    NTS = TS // 128
    plan, tiles = nat_geometry(T, cfg.GW)
    NT = len(tiles)
    nc = bass.Bass("TRN2", target_bir_lowering=False)
    dr = lambda n, s, d, k="ExternalInput": nc.dram_tensor(n, s, d, kind=k).ap()
    hTs = dr("hTs", [D, TS], BF16)
    w_fm = dr("w_fm", [D, NFM], F32)
    w_tm = dr("w_tm", [D, 256], F32)
    pch_d = dr("pch", [128, 18], F32)
    plo_d = dr("plo", [64, 4], F32)
    pgl_d = dr("pgl", [96, 2], F32)
    dw2_d = dr("dw2", [64, 128], F32)
    aw2_d = dr("aw2", [64, 128], F32)
    gg2_d = dr("gg2", [96, 128], F32)
    poolw_d = dr("poolw", [128, 128], F32)
    bands_d = dr("bands", [5, 128, 128], F32)
    nbias_d = dr("nbias", [NT, 2, 128, 128], F32)
    nmask_d = dr("nmask", [NT, 128, 128], F32)
    ident_d = dr("ident", [128, 128], F32)
    bones_d = dr("bones", [128, 128], F32)
    hsel_d = dr("hsel", [2, 128], F32)
    opad_d = dr("opad", [2, 128, 128], F32)
    brT_o = dr("brTo", [3, 128, TS], BF16, "ExternalOutput")
    di = lambda n, s, d: nc.dram_tensor(n, s, d).ap()
    zlat = di("zlat", [6, 128, T + 2], F32)
    zctx = di("zctx", [6, 128, CTX + 2], F32)
    xtm = [[di("xtm_%d_%d" % (d, i), [TS, 128], F32) for i in range(3)] for d in range(2)]
    xkk = di("xtm_kk", [TS, 128], F32)
    xr = di("xtm_r", [TS, 128], F32)
    bon_d = di("bon_d", [128, TS], F32)
    gate_d = di("gate_d", [128, TS], F32)

    seqs = [("lat", 0, T, zlat), ("ctx", T, CTX, zctx)]

    with ExitStack() as es:
        es.enter_context(nc.allow_non_contiguous_dma("scratch layouts"))
        sb = lambda n, s, d: es.enter_context(nc.sbuf_tensor(n, s, d))
        ps = lambda n, s, d: es.enter_context(nc.psum_tensor(n, s, d))
        sems = mk_sems(nc, es)
        P = Prog(nc, sems)
        PS = [ps("ps%d" % i, [128, 512], F32) for i in range(8)]
        psi = [0]

        def nps(k=8):
            psi[0] = (psi[0] + 1) % k
            return PS[psi[0]]

        ident = sb("ident_s", [128, 128], F32)
        bones = sb("bones_s", [128, 128], F32)
        hsel = sb("hsel_s", [2, 128], F32)
        pch = sb("pch_s", [128, 18], F32)
        pder = sb("pder", [128, 8], F32)
        plo = sb("plo_s", [64, 4], F32)
        plod = sb("plod", [64, 2], F32)
        pgl = sb("pgl_s", [96, 2], F32)
        pgld = sb("pgld", [96, 1], F32)
        dw2 = sb("dw2_s", [64, 128], F32)
        aw2 = sb("aw2_s", [64, 128], F32)
        gg2 = sb("gg2_s", [96, 128], F32)
        zero = sb("zero_s", [128, 8], F32)
        P.dma(ident[:], ident_d)
        P.dma(bones[:], bones_d)
        P.dma(hsel[:], hsel_d)
        P.dma(pch[:], pch_d)
        P.dma(plo[:], plo_d)
        P.dma(pgl[:], pgl_d)
        P.dma(dw2[:], dw2_d)
        P.dma(aw2[:], aw2_d)
        P.dma(gg2[:], gg2_d)
        P.memset(zero[:], 0.0)
        for i in range(3):
            P.tt(pder[:, i:i + 1], pch[:, 2 * i:2 * i + 1], pch[:, 2 * i + 1:2 * i + 2], ALU.add)
            P.ts(pder[:, i:i + 1], pder[:, i:i + 1], -1.0, 1.0, op0=ALU.mult, op1=ALU.add)
        P.ts(pder[:, 3:4], pch[:, 11:12], -1.0, 1.0, op0=ALU.mult, op1=ALU.add)
        P.ts(pder[:, 4:5], pch[:, 16:17], 0.125, None, op0=ALU.mult)
        for i in range(2):
            P.tt(plod[:, i:i + 1], plo[:, 2 * i:2 * i + 1], plo[:, 2 * i + 1:2 * i + 2], ALU.add)
            P.ts(plod[:, i:i + 1], plod[:, i:i + 1], -1.0, 1.0, op0=ALU.mult, op1=ALU.add)
        P.tt(pgld[:, 0:1], pgl[:, 0:1], pgl[:, 1:2], ALU.add)
        P.ts(pgld[:, 0:1], pgld[:, 0:1], -1.0, 1.0, op0=ALU.mult, op1=ALU.add)
        for (_, _, L, zz) in seqs:
            for rb in range(6):
                P.dma(Sub(zz[rb, :, 0:1], ("h", rb, 0)), zero[:, 0:1])
                P.dma(Sub(zz[rb, :, L + 1:L + 2], ("h", rb, 1)), zero[:, 0:1])

        if stop <= 0:
            P.barrier()
            P.finish()
            return nc
        ph = ExitStack()
        sb2 = lambda n, s, d: ph.enter_context(nc.sbuf_tensor(n, s, d))
        wfm = sb2("wfm", [128, 8, NFM], BF16)
        wtm = sb2("wtm", [128, 8, 256], BF16)
        P.dma(wfm[:], w_fm.rearrange("(k p) m -> p k m", p=128), q="gpsimd")
        P.dma(wtm[:], w_tm.rearrange("(k p) m -> p k m", p=128), q="gpsimd")
        QN = sb2("QN", [128, TS], BF16)
        KN = sb2("KN", [128, TS], BF16)
        VP = sb2("VP", [128, NTS, 2, 128], BF16)
        zp = sb2("zp", [128, NTS, 128], F32)
        hb = [sb2("hb%d" % i, [128, 8, 512], BF16) for i in range(2)]
        stg = [sb2("stg%d" % i, [128, 512], F32) for i in range(3)]
        sqn = [sb2("sqn%d" % i, [128, 512], F32) for i in range(2)]
        rsn = [sb2("rsn%d" % i, [128, 512], F32) for i in range(2)]
        P.memset(VP[:], 0.0, eng="gpsimd")
        if stop <= 0.5:
            P.barrier()
            ph.close()
            P.barrier()
            P.finish()
            return nc
        bi = 0
        si = 0
        for (sname, s0, L, zz) in seqs:
            for o in range(0, L, 512):
                n = min(512, L - o)
                h = hb[bi % 2]
                bi += 1
                P.dma(h[:, :, 0:n], hTs[:, s0 + o:s0 + o + n].rearrange("(k p) n -> p k n", p=128))
                c0 = 0
                for rbi, (nm, M) in enumerate(FM_BLOCKS):
                    pt = nps()
                    for kc in range(8):
                        P.mm(pt[0:M, 0:n], wfm[:, kc, c0:c0 + M], h[:, kc, 0:n], start=(kc == 0), stop=(kc == 7))
                    c0 += M
                    if rbi < 6:
                        st = stg[si % 3]
                        si += 1
                        P.copy(st[0:M, 0:n], pt[0:M, 0:n], eng=("scalar" if rbi % 2 else "vector"))
                        if DBG & 2:
                            P.dma(Sub(zz[rbi, 0:M, 1 + o:1 + o + n], ("b", rbi, o)), st[0:M, 0:n])
                    elif DBG & 4:
                        s_ = sqn[rbi % 2]
                        P.act(s_[:, 0:n], pt[:, 0:n], AF.Square)
                        p2 = nps()
                        P.mm(p2[:, 0:n], bones[:], s_[:, 0:n])
                        r_ = rsn[rbi % 2]
                        P.ts(r_[:, 0:n], p2[:, 0:n], 1.0 / 64.0, 1e-6, op0=ALU.mult, op1=ALU.add)
                        P.recip(r_[:, 0:n], r_[:, 0:n])
                        P.act(r_[:, 0:n], r_[:, 0:n], AF.Sqrt)
                        P.tt(r_[:, 0:n], r_[:, 0:n], pt[:, 0:n], ALU.mult)
                        if nm == "q":
                            P.ts(QN[:, s0 + o:s0 + o + n], r_[:, 0:n], pder[:, 4:5], None, op0=ALU.mult)
                        else:
                            P.ts(KN[:, s0 + o:s0 + o + n], r_[:, 0:n], pch[:, 17:18], None, op0=ALU.mult)
                for t0 in (range(0, n, 128) if DBG & 8 else []):
                    ti = (s0 + o + t0) // 128
                    pt = nps()
                    for kc in range(8):
                        P.mm(pt[:, 0:256], h[:, kc, t0:t0 + 128], wtm[:, kc, :], start=(kc == 0), stop=(kc == 7))
                    P.copy(zp[:, ti, :], pt[:, 0:128])
                    if DBG & 16:
                        P.copy(VP[:, ti, 0, 0:64], pt[:, 128:192], eng="scalar")
                        P.copy(VP[:, ti, 1, 64:128], pt[:, 192:256], eng="scalar")
                    if DBG & 32:
                        P.copy(VP[:, ti, 0, 0:64], pt[:, 128:192], eng="vector")
                        P.copy(VP[:, ti, 1, 64:128], pt[:, 192:256], eng="vector")

        if stop <= 1:
            P.barrier()
            ph.close()
            P.barrier()
            P.finish()
            return nc
        bands = sb2("bands_s", [128, 5, 128], F32)
        poolw = sb2("poolw_s", [128, 128], F32)
        for i in range(5):
            P.dma(bands[:, i, :], bands_d[i])
        P.dma(poolw[:], poolw_d)
        pst = [sb2("pst%d" % i, [128, 512], F32) for i in range(2)]
        pob = [sb2("pob%d" % i, [128, 512], BF16) for i in range(2)]
        bi = 0
        for (sname, s0, L, zz) in seqs:
            nt = L // 128
            tb = s0 // 128
            for o in range(0, nt, 4):
                m = min(4, nt - o)
                pt = nps()
                for j in range(m):
                    i = o + j
                    terms = []
                    if i > 0:
                        terms.append((i - 1, 0))
                    terms.append((i, 3 if i == 0 else (4 if i == nt - 1 else 1)))
                    if i < nt - 1:
                        terms.append((i + 1, 2))
                    for q_, (ti, bidx) in enumerate(terms):
                        P.mm(pt[:, j * 128:(j + 1) * 128], zp[:, tb + ti, :], bands[:, bidx, :], start=(q_ == 0), stop=(q_ == len(terms) - 1))
                st = pst[bi % 2]
                ob = pob[bi % 2]
                bi += 1
                P.copy(st[:, 0:m * 128], pt[:, 0:m * 128])
                p2 = nps()
                P.mm(p2[:, 0:m * 128], poolw[:], st[:, 0:m * 128])
                P.ts(ob[:, 0:m * 128], p2[:, 0:m * 128], pch[:, 15:16], None, op0=ALU.mult)
                P.dma(brT_o[0, :, s0 + o * 128:s0 + (o + m) * 128], ob[:, 0:m * 128], is_out=True)

        if stop <= 2:
            P.barrier()
            ph.close()
            P.barrier()
            P.finish()
            return nc
        nbm = sb2("nbm", [128, NT, 2, 128], F32)
        nmk = sb2("nmk", [128, NT, 128], F32)
        opad = sb2("opad_s", [128, 2, 128], BF16)
        for i in range(NT):
            P.dma(nmk[:, i, :], nmask_d[i])
            for hh in range(2):
                P.dma(nbm[:, i, hh, :], nbias_d[i, hh])
        opf = sb2("opf", [128, 2, 128], F32)
        for hh in range(2):
            P.dma(opf[:, hh, :], opad_d[hh])
        P.copy(opad[:], opf[:])
        P.ts(nmk[:], nmk[:], 1.0, 30000.0, op0=ALU.subtract, op1=ALU.mult)
        for hh in range(2):
            P.tt(nbm[:, :, hh, :], nbm[:, :, hh, :], nmk[:], ALU.add)
        sct = [sb2("sct%d" % i, [128, 128], F32) for i in range(3)]
        ptb = [sb2("ptb%d" % i, [128, 128], BF16) for i in range(4)]
        nrc = sb2("nrc", [128, 128], F32)
        nob = [sb2("nob%d" % i, [128, 512], BF16) for i in range(2)]
        ctx_tiles = [NTL + i for i in range(NTC)]
        qtiles = [(m, [(kt, tid) for kt, tid in plan[m]] + [(c, None) for c in ctx_tiles]) for m in range(NTL)]
        qtiles += [(NTL + i, [(c, None) for c in ctx_tiles]) for i in range(NTC)]
        ci = 0
        po = PS[6]
        pd = PS[7]
        for qi_, (m, klist) in enumerate(qtiles):
            tot = 2 * len(klist)
            cnt = 0
            for hh in range(2):
                hs = slice(hh * 64, (hh + 1) * 64)
                for (kt, tid) in klist:
                    pt = nps(6)
                    P.mm(pt[:, 0:128], KN[hs, kt * 128:(kt + 1) * 128], QN[hs, m * 128:(m + 1) * 128])
                    pb = ptb[ci % 4]
                    if tid is not None:
                        sc_ = sct[ci % 3]
                        P.tt(sc_[:], pt[:, 0:128], nbm[:, tid, hh, :], ALU.add)
                        P.act(pb[:], sc_[:], AF.Exp)
                    else:
                        P.act(pb[:], pt[:, 0:128], AF.Exp)
                    ci += 1
                    P.mm(po[:, 0:128], VP[:, kt, hh, :], pb[:], start=(cnt == 0), stop=(cnt == tot - 1))
                    P.mm(pd[:, 0:128], opad[:, hh, :], pb[:], start=(cnt == 0), stop=(cnt == tot - 1))
                    cnt += 1
            ob = nob[(qi_ // 4) % 2]
            j = qi_ % 4
            P.recip(nrc[:], pd[:, 0:128])
            P.tt(ob[:, j * 128:(j + 1) * 128], po[:, 0:128], nrc[:], ALU.mult)
            if j == 3 or qi_ == len(qtiles) - 1:
                q0 = (qi_ // 4) * 4
                P.dma(brT_o[2, :, q0 * 128:(qi_ + 1) * 128], ob[:, 0:(j + 1) * 128], is_out=True)
        if stop <= 3:
            P.barrier()
            ph.close()
            P.barrier()
            P.finish()
            return nc
        P.barrier()
        ph.close()

        ph = ExitStack()
        sb2 = lambda n, s, d: ph.enter_context(nc.sbuf_tensor(n, s, d))
        vS = sb2("vS", [128, TS], F32)
        Od = [sb2("O%d" % d, [128, TS], F32) for d in range(2)]
        ph4 = ExitStack()
        sb1 = sb2
        sb2 = lambda n, s, d: ph4.enter_context(nc.sbuf_tensor(n, s, d))
        zin = {nm: sb2("zin_" + nm, [M, 514], F32) for nm, M in FM_BLOCKS[:6]}
        zs = {nm: sb2("zs_" + nm, [M, 512], F32) for nm, M in FM_BLOCKS[:6] if nm != "v"}
        tw = sb2("tw", [64, 512], F32)
        ed = [sb2("e%d" % d, [128, 512], F32) for d in range(2)]
        ad = [sb2("a%d" % d, [128, 512], F32) for d in range(2)]
        wd = [sb2("w%d" % d, [128, 512], F32) for d in range(2)]
        kx = sb2("kx", [128, 512], F32)
        ksq = sb2("ksq", [128, 512], F32)
        rn = sb2("rn", [128, 512], F32)
        kk = sb2("kk", [128, 512], F32)
        nkka = [sb2("nkka%d" % d, [128, 512], F32) for d in range(2)]
        kd = [sb2("kd%d" % d, [128, 512], F32) for d in range(2)]
        tq = sb2("tq", [128, 512], F32)
        bonb = sb2("bonb", [128, 512], F32)
        sgl = sb2("sgl", [96, 512], F32)
        gtb = sb2("gtb", [128, 512], F32)
        tst = [sb2("tst%d" % i, [128, 128], F32) for i in range(4)]
        mus = {"r": (0, 1, 0), "k": (2, 3, 1), "v": (4, 5, 2)}
        ti_ = 0
        for (sname, s0, L, zz) in seqs:
            for o in range(0, L, 512):
                n = min(512, L - o)
                g0 = s0 + o
                for rbi, (nm, M) in enumerate(FM_BLOCKS[:6]):
                    P.dma(zin[nm][:, 0:n + 2], Sub(zz[rbi, 0:M, o:o + n + 2], None))
                    z = zin[nm]
                    if nm in mus:
                        a_, b_, c_ = mus[nm]
                        mp, mn, c0_ = pch[:, a_:a_ + 1], pch[:, b_:b_ + 1], pder[:, c_:c_ + 1]
                    elif nm == "wlo":
                        mp, mn, c0_ = plo[:, 0:1], plo[:, 1:2], plod[:, 0:1]
                    elif nm == "alo":
                        mp, mn, c0_ = plo[:, 2:3], plo[:, 3:4], plod[:, 1:2]
                    else:
                        mp, mn, c0_ = pgl[:, 0:1], pgl[:, 1:2], pgld[:, 0:1]
                    out = vS[:, g0:g0 + n] if nm == "v" else zs[nm][:, 0:n]
                    P.ts(out, z[:, 1:n + 1], c0_, None, op0=ALU.mult)
                    P.stt(out, z[:, 0:n], mp, out, ALU.mult, ALU.add)
                    P.stt(out, z[:, 2:n + 2], mn, out, ALU.mult, ALU.add)
                r_ = zs["r"]
                k_ = zs["k"]
                P.ts(kx[:, 0:n], k_[:, 0:n], pch[:, 10:11], None, op0=ALU.mult)
                P.act(ksq[:, 0:n], kx[:, 0:n], AF.Square)
                p2 = nps()
                P.mm(p2[:, 0:n], bones[:], ksq[:, 0:n])
                P.ts(rn[:, 0:n], p2[:, 0:n], 1e-24, None, op0=ALU.max)
                P.recip(rn[:, 0:n], rn[:, 0:n])
                P.act(rn[:, 0:n], rn[:, 0:n], AF.Sqrt)
                P.tt(kk[:, 0:n], kx[:, 0:n], rn[:, 0:n], ALU.mult)
                P.act(tw[:, 0:n], zs["wlo"][:, 0:n], AF.Tanh)
                for d in range(2):
                    ds_ = slice(d * 32, (d + 1) * 32)
                    pw = nps()
                    P.mm(pw[:, 0:n], dw2[ds_, :], tw[ds_, 0:n])
                    P.act(ed[d][:, 0:n], pw[:, 0:n], AF.Sigmoid, bias=pch[:, 6 + d:7 + d])
                    P.act(wd[d][:, 0:n], ed[d][:, 0:n], AF.Exp, scale=-0.6065306597126334)
                    pa = nps()
                    P.mm(pa[:, 0:n], aw2[ds_, :], zs["alo"][ds_, 0:n])
                    P.act(ad[d][:, 0:n], pa[:, 0:n], AF.Sigmoid, bias=pch[:, 8 + d:9 + d])
                    P.stt(nkka[d][:, 0:n], ad[d][:, 0:n], -1.0, kk[:, 0:n], ALU.mult, ALU.mult)
                    P.ts(tq[:, 0:n], ad[d][:, 0:n], pch[:, 11:12], pder[:, 3:4], op0=ALU.mult, op1=ALU.add)
                    P.tt(kd[d][:, 0:n], k_[:, 0:n], tq[:, 0:n], ALU.mult)
                P.tt(tq[:, 0:n], kd[0][:, 0:n], kd[1][:, 0:n], ALU.add)
                P.stt(tq[:, 0:n], tq[:, 0:n], pch[:, 12:13], r_[:, 0:n], ALU.mult, ALU.mult)
                pbn = nps()
                P.mm(pbn[:, 0:n], bones[:], tq[:, 0:n])
                P.tt(bonb[:, 0:n], pbn[:, 0:n], vS[:, g0:g0 + n], ALU.mult)
                P.dma(Sub(bon_d[:, g0:g0 + n], g0), bonb[:, 0:n])
                P.act(sgl[:, 0:n], zs["glo"][:, 0:n], AF.Sigmoid)
                pgt = nps()
                P.mm(pgt[:, 0:n], gg2[:], sgl[:, 0:n])
                P.copy(gtb[:, 0:n], pgt[:, 0:n], eng="scalar")
                P.dma(Sub(gate_d[:, g0:g0 + n], g0), gtb[:, 0:n])
                srcs = [(kk, xkk), (r_, xr)] + [(wd[d], xtm[d][0]) for d in range(2)] + [(nkka[d], xtm[d][1]) for d in range(2)] + [(kd[d], xtm[d][2]) for d in range(2)]
                for (src, dst) in srcs:
                    for t0 in range(0, n, 128):
                        ptr = nps()
                        P.tr(ptr[:, 0:128], src[:, t0:t0 + 128], ident[:])
                        st = tst[ti_ % 4]
                        P.copy(st[:], ptr[:, 0:128], eng=("scalar" if ti_ % 2 else "vector"))
                        ti_ += 1
                        P.dma(Sub(dst[g0 + t0:g0 + t0 + 128, :], (g0 + t0) // 32), st[:])
        if stop <= 4:
            P.barrier()
            ph4.close()
            ph.close()
            P.barrier()
            P.finish()
            return nc
        P.barrier()
        ph4.close()
        ph4 = ExitStack()
        sb2 = lambda n, s, d: ph4.enter_context(nc.sbuf_tensor(n, s, d))
        TB = 16
        Sd = [[sb2("S%d_%d" % (d, i), [128, 64], F32) for i in range(3)] for d in range(2)]
        junkd = [sb2("junk%d" % d, [128, 64], F32) for d in range(2)]
        sad = [sb2("sa%d" % d, [128, 1], F32) for d in range(2)]
        xfd = [[sb2("xf%d_%d" % (d, j), [2, TB, 5, 64], F32) for j in range(2)] for d in range(2)]

        def dir_stream(d):
            ops_d = [xkk, xtm[d][0], xtm[d][1], xtm[d][2], xr]
            S = Sd[d]
            junk = junkd[d]
            sa = sad[d]
            P.memset(S[0][:], 0.0)
            order = [seqs[1], seqs[0]]
            bidx = 0
            gidx = 0
            for (sname, s0, L, zz) in order:
                blocks = list(range(0, L, TB))
                if d == 1:
                    blocks = blocks[::-1]
                for o in blocks:
                    g0 = s0 + o
                    buf = xfd[d][bidx % 2]
                    bidx += 1
                    first = True
                    toks = list(range(TB))
                    if d == 1:
                        toks = toks[::-1]
                    for tq_ in toks:
                        t = g0 + tq_
                        bank = PS[d * 4 + gidx % 4]
                        gidx += 1
                        B = [bank[:, i * 64:(i + 1) * 64] for i in range(5)]

                        def f0(buf=buf, g0=g0, first=first, bank=bank, tq_=tq_):
                            if first:
                                for i in range(5):
                                    P.dma(buf[:, :, i, :], Sub(ops_d[i][g0:g0 + TB, :].rearrange("t (h j) -> h t j", h=2), None))
                            P.mm(bank[:, 0:320], hsel[:], buf[:, tq_, :, :].rearrange("h i j -> h (i j)"))

                        thunks = [
                            f0,
                            lambda B=B: P.stt(junk[:], S[0][:], 1.0, B[0], ALU.mult, ALU.mult, accum_out=sa[:, 0:1]),
                            lambda B=B: P.tt(S[1][:], S[0][:], B[1], ALU.mult),
                            lambda B=B: P.stt(S[2][:], B[2], sa[:, 0:1], S[1][:], ALU.mult, ALU.add),
                            lambda B=B, t=t: P.stt(S[0][:], B[3], vS[:, t:t + 1], S[2][:], ALU.mult, ALU.add),
                            lambda B=B, t=t: P.stt(junk[:], S[0][:], 1.0, B[4], ALU.mult, ALU.mult, accum_out=Od[d][:, t:t + 1]),
                        ]
                        first = False
                        yield thunks

        for stF, stB in zip(dir_stream(0), dir_stream(1)):
            for i in range(6):
                stF[i]()
                stB[i]()
        if stop <= 5:
            P.barrier()
            ph4.close()
            ph.close()
            P.barrier()
            P.finish()
            return nc
        P.barrier()
        ph4.close()
        ph4 = ExitStack()
        sb2 = lambda n, s, d: ph4.enter_context(nc.sbuf_tensor(n, s, d))
        ksq = sb2("ksq2", [128, 512], F32)
        rn = sb2("rn2", [128, 512], F32)
        bonb = sb2("bonb2", [128, 512], F32)
        gtb = sb2("gtb2", [128, 512], F32)
        wk = sb2("wk", [128, 512], F32)
        cen = sb2("cen", [128, 512], F32)
        rob = [sb2("rob%d" % i, [128, 512], BF16) for i in range(2)]
        bi = 0
        for o in range(0, TS, 512):
            n = min(512, TS - o)
            P.tt(wk[:, 0:n], Od[0][:, o:o + n], Od[1][:, o:o + n], ALU.add)
            pm = nps()
            P.mm(pm[:, 0:n], bones[:], wk[:, 0:n])
            P.stt(cen[:, 0:n], pm[:, 0:n], -1.0 / 64.0, wk[:, 0:n], ALU.mult, ALU.add)
            P.act(ksq[:, 0:n], cen[:, 0:n], AF.Square)
            pv = nps()
            P.mm(pv[:, 0:n], bones[:], ksq[:, 0:n])
            P.ts(rn[:, 0:n], pv[:, 0:n], 1.0 / 64.0, GN_EPS, op0=ALU.mult, op1=ALU.add)
            P.recip(rn[:, 0:n], rn[:, 0:n])
            P.act(rn[:, 0:n], rn[:, 0:n], AF.Sqrt)
            P.tt(cen[:, 0:n], cen[:, 0:n], rn[:, 0:n], ALU.mult)
            P.ts(cen[:, 0:n], cen[:, 0:n], pch[:, 13:14], pch[:, 14:15], op0=ALU.mult, op1=ALU.add)
            P.dma(bonb[:, 0:n], Sub(bon_d[:, o:o + n], None))
            P.dma(gtb[:, 0:n], Sub(gate_d[:, o:o + n], None))
            P.tt(cen[:, 0:n], cen[:, 0:n], bonb[:, 0:n], ALU.add)
            ob = rob[bi % 2]
            bi += 1
            P.tt(ob[:, 0:n], cen[:, 0:n], gtb[:, 0:n], ALU.mult)
            P.dma(brT_o[1, :, o:o + n], ob[:, 0:n], is_out=True)
        P.barrier()
        ph4.close()
        ph.close()
        P.barrier()
        P.finish()
    return nc


def seq_inputs(cfg, W, li, g, hTs):
    gs = slice(g * 128, (g + 1) * 128)
    w_in = W["w_in"][li]
    zr0 = OFF_RWKV
    cols_fm = np.concatenate([
        np.arange(zr0 + g * 128, zr0 + g * 128 + 128), np.arange(zr0 + 512 + g * 128, zr0 + 512 + g * 128 + 128),
        np.arange(zr0 + 1024 + g * 128, zr0 + 1024 + g * 128 + 128), np.arange(zr0 + 1536, zr0 + 1760),
        np.arange(OFF_Q + g * 128, OFF_Q + g * 128 + 128), np.arange(OFF_K + g * 128, OFF_K + g * 128 + 128)])
    cols_tm = np.concatenate([np.arange(g * 128, g * 128 + 128), np.arange(OFF_V + g * 128, OFF_V + g * 128 + 128)])
    mu = W["shift_mu"][li]
    pch = np.zeros((128, 18), np.float32)
    pch[:, 0] = mu[0, gs]; pch[:, 1] = mu[1, gs]
    pch[:, 2] = mu[0, 512 + g * 128:512 + g * 128 + 128]; pch[:, 3] = mu[1, 512 + g * 128:512 + g * 128 + 128]
    pch[:, 4] = mu[0, 1024 + g * 128:1024 + g * 128 + 128]; pch[:, 5] = mu[1, 1024 + g * 128:1024 + g * 128 + 128]
    pch[:, 6] = W["decay_w0"][li][0, gs]; pch[:, 7] = W["decay_w0"][li][1, gs]
    pch[:, 8] = W["iclr_a0"][li][0, gs]; pch[:, 9] = W["iclr_a0"][li][1, gs]
    pch[:, 10] = W["key_kk"][li][gs]; pch[:, 11] = W["key_ka"][li][gs]
    pch[:, 12] = W["bonus_rk"][li].reshape(-1)[gs]
    pch[:, 13] = W["gn_g"][li][gs]; pch[:, 14] = W["gn_b"][li][gs]
    pch[:, 15] = W["pool_scale"][li][gs]
    pch[:, 16] = np.tile(W["nat_qn_g"][li], 2); pch[:, 17] = np.tile(W["nat_kn_g"][li], 2)
    plo = np.stack([mu[0, 1536:1600], mu[1, 1536:1600], mu[0, 1600:1664], mu[1, 1600:1664]], axis=1)
    pgl = np.stack([mu[0, 1664:1760], mu[1, 1664:1760]], axis=1)
    dw2 = W["decay_w2"][li][:, :, gs].reshape(64, 128)
    aw2 = W["iclr_a2"][li][:, :, gs].reshape(64, 128)
    gg2 = W["gate_g2"][li][:, gs]
    plan, tiles = nat_geometry(cfg.T, cfg.GW)
    rpb = W["nat_rpb"][li]
    nbias = np.stack([np.stack([rpb[2 * g + hh][ro, co] * valid for hh in range(2)]) for (valid, ro, co) in tiles]).astype(np.float32)
    nmask = np.stack([valid.astype(np.float32) for (valid, ro, co) in tiles])
    bones = np.kron(np.eye(2, dtype=np.float32), np.ones((64, 64), np.float32))
    hsel = np.kron(np.eye(2, dtype=np.float32), np.ones((1, 64), np.float32))
    opad = np.zeros((2, 128, 128), np.float32)
    opad[0, :, 0:64] = 1.0
    opad[1, :, 64:128] = 1.0
    f = lambda a: np.ascontiguousarray(a, dtype=np.float32)
    return dict(hTs=hTs, w_fm=f(w_in[:, cols_fm]), w_tm=f(w_in[:, cols_tm]), pch=pch, plo=f(plo), pgl=f(pgl), dw2=f(dw2), aw2=f(aw2),
                gg2=f(gg2), poolw=f(W["pool_w"][li][g]), bands=pool_bands(POOL_WINDOWS[g]), nbias=nbias, nmask=nmask,
                ident=np.eye(128, dtype=np.float32), bones=bones, hsel=hsel, opad=opad)


import ml_dtypes

_BF = ml_dtypes.bfloat16
_PROGS = {}


def _prog(key, fn):
    if key not in _PROGS:
        _PROGS[key] = fn()
    return _PROGS[key]


def _run(nc, in_maps):
    res = run_bass_kernel_spmd(nc, in_maps, core_ids=list(range(8)))
    return res.results


def kernel(**inputs):
    W = {k: np.asarray(v) for k, v in inputs.items()}
    x = W["x"]
    ctx = W["ctx"]
    Bsz, T, D = x.shape
    CTX = ctx.shape[1]
    DFF = W["ffn_w1"].shape[2]
    NE = W["router"].shape[2]
    depth = W["w_mod"].shape[0]
    cfg = Cfg(T=T, CTX=CTX, DFF=DFF, NE=NE)
    TL, CL, NTOK, TS = cfg.TL, cfg.CL, cfg.NTOK, cfg.TS
    f32 = lambda a: np.ascontiguousarray(a, dtype=np.float32)
    ones = np.ones((128, 128), np.float32)
    ident = np.eye(128, dtype=np.float32)
    cores = [(b, j) for b in range(2) for j in range(4)]
    xT = []
    for (b, j) in cores:
        a = np.zeros((D, NTOK), np.float32)
        a[:, :TL] = x[b, j * TL:(j + 1) * TL].T
        a[:, TL:TL + CL] = ctx[b, j * CL:(j + 1) * CL].T
        xT.append(a)
    cvecs = [f32(np.stack([W["c"][b], W["c_ctx"]], axis=1)) for b in range(2)]
    ncA = _prog(("A", T, CTX), lambda: build_tok(cfg, "A"))
    ncS = _prog(("S", T, CTX), lambda: build_seq(cfg))
    for li in range(depth):
        moe = (li % 2 == 1)
        jj = li // 2
        wm = W["w_mod"][li]
        bm = W["b_mod"][li]
        wmA = f32(wm[:, :2 * D])
        bmA = f32(bm[:2 * D])
        gmix = f32(W["norm_mix_g"][li])
        mapsA = [dict(xT=xT[c], cvec=cvecs[b], w_mod=wmA, b_mod=bmA, g_mix=gmix, ones=ones, ident=ident) for c, (b, j) in enumerate(cores)]
        rA = _run(ncA, mapsA)
        hTs = []
        for b in range(2):
            a = np.zeros((D, TS), _BF)
            for j in range(4):
                h = rA[b * 4 + j]["hT"]
                a[:, j * TL:(j + 1) * TL] = h[:, :TL]
                a[:, T + j * CL:T + (j + 1) * CL] = h[:, TL:TL + CL]
            hTs.append(a)
        mapsS = [seq_inputs(cfg, W, li, g, hTs[b]) for (b, g) in cores]
        rS = _run(ncS, mapsS)
        brs = []
        for (b, j) in cores:
            a = np.zeros((3, 512, NTOK), _BF)
            for g in range(4):
                o = rS[b * 4 + g]["brTo"]
                a[:, g * 128:(g + 1) * 128, :TL] = o[:, :, j * TL:(j + 1) * TL]
                a[:, g * 128:(g + 1) * 128, TL:TL + CL] = o[:, :, T + j * CL:T + (j + 1) * CL]
            brs.append(a)
        ncB = _prog(("B", T, CTX, DFF, moe), lambda: build_tok(cfg, "B", moe=moe))
        common = dict(w_mod=f32(wm), b_mod=f32(bm), g_mix=gmix, g_ffn=f32(W["norm_ffn_g"][li]),
                      w_gate=f32(W["w_in"][li][:, OFF_GATE:]), w_branch=f32(W["w_branch"][li]), w_out=f32(W["w_out"][li]),
                      ones=ones, ident=ident)
        if moe:
            common.update(router=f32(W["router"][jj]), w1=f32(W["moe_w1"][jj]), w3=f32(W["moe_w3"][jj]), w2=f32(W["moe_w2"][jj]))
        else:
            common.update(w1=f32(W["ffn_w1"][jj][None]), w3=f32(W["ffn_w3"][jj][None]), w2=f32(W["ffn_w2"][jj][None]))
        mapsB = [dict(xT=xT[c], cvec=cvecs[b], brT=brs[c], **common) for c, (b, j) in enumerate(cores)]
        rB = _run(ncB, mapsB)
        xT = [f32(rB[c]["xo"]) for c in range(8)]
    out = np.zeros((Bsz, T, D), np.float32)
    for c, (b, j) in enumerate(cores):
        out[b, j * TL:(j + 1) * TL] = xT[c][:, :TL].T
    return out
```

```python
import os
from contextlib import ExitStack
import numpy as np
import concourse.bass as bass
import concourse.mybir as mybir
from concourse.bass_utils import run_bass_kernel_spmd


F32 = mybir.dt.float32
BF16 = mybir.dt.bfloat16
AF = mybir.ActivationFunctionType
ALU = mybir.AluOpType
AX = mybir.AxisListType

ENGS = ("tensor", "vector", "scalar", "gpsimd", "sync")
NDMA_SEM = 40
NDMA_HW = 32


class Sub:
    def __init__(self, ap, sub):
        self.ap = ap
        self.sub = sub


def _ak(x):
    if isinstance(x, Sub):
        return x.ap, (x.ap.name, x.sub)
    return x, (x.name, None)


class Prog:
    def __init__(self, nc, sems=None):
        self.nc = nc
        self.sems = sems
        self.ops = {e: [] for e in ENGS}
        self.cnt = {e: 0 for e in ENGS}
        self.st = {}
        self.waited = {e: {} for e in ENGS}
        self.ndma = 0
        self.ndma_sw = 0
        self.dma_cnt = [0] * NDMA_SEM
        self.out_tokens = []
        self.n_instr = 0

    def _deps(self, eng, reads, writes):
        raw = {}
        oth = {}

        def add(dst, tok):
            s, v = tok
            if dst.get(s, -1) < v:
                dst[s] = v

        for key in reads:
            name, sub = key
            d = self.st.setdefault(name, {})
            if sub not in d:
                d[sub] = {"w": None, "r": {}}
            subs = list(d.keys()) if sub is None else ([sub] + ([None] if None in d else []))
            for s in subs:
                if d[s]["w"] is not None:
                    add(raw, d[s]["w"])
        for key in writes:
            name, sub = key
            d = self.st.setdefault(name, {})
            if sub not in d:
                d[sub] = {"w": None, "r": {}}
            subs = list(d.keys()) if sub is None else ([sub] + ([None] if None in d else []))
            for s in subs:
                if d[s]["w"] is not None:
                    add(oth, d[s]["w"])
                for rs, rv in d[s]["r"].items():
                    add(oth, (rs, rv))
        return raw, oth

    def _commit(self, tok, reads, writes):
        for key in reads:
            name, sub = key
            e = self.st[name][sub]
            s, v = tok
            if e["r"].get(s, -1) < v:
                e["r"][s] = v
        for key in writes:
            name, sub = key
            d = self.st[name]
            if sub is None:
                for s in d:
                    d[s] = {"w": tok, "r": {}}
            else:
                d[sub] = {"w": tok, "r": {}}

    def _emit_waits(self, eng, raw, oth):
        own = "E_" + eng
        need = {}
        for s, v in raw.items():
            need[s] = max(need.get(s, -1), v)
        for s, v in oth.items():
            if s == own and eng == "tensor":
                continue
            need[s] = max(need.get(s, -1), v)
        wl = []
        for s, v in need.items():
            if self.waited[eng].get(s, -1) >= v:
                continue
            self.waited[eng][s] = v
            wl.append((s, v))
        return wl

    def op(self, eng, fn, reads, writes, is_dma=False, is_out=False):
        rk = [_ak(x)[1] for x in reads]
        wk = [_ak(x)[1] for x in writes]
        raw, oth = self._deps(eng, rk, wk)
        if is_dma:
            if eng == "gpsimd":
                i = NDMA_HW + (self.ndma_sw % (NDMA_SEM - NDMA_HW))
                self.ndma_sw += 1
            else:
                i = self.ndma % NDMA_HW
                self.ndma += 1
            prev = self.dma_cnt[i]
            self.dma_cnt[i] += 16
            tok = ("D_%d" % i, self.dma_cnt[i])
            if prev > 0:
                oth[tok[0]] = max(oth.get(tok[0], -1), prev)
        else:
            self.cnt[eng] += 1
            tok = ("E_" + eng, self.cnt[eng])
        wl = self._emit_waits(eng, raw, oth)
        eobj = getattr(self.nc, eng)
        for s_, v_ in wl:
            eobj.wait_ge(self.sems[s_], v_)
        ins = fn(eobj)
        ins.then_inc(self.sems[tok[0]], 16 if is_dma else 1)
        self._commit(tok, rk, wk)
        if is_out:
            self.out_tokens.append(tok)
        self.n_instr += 1
        return tok

    def dma(self, out, in_, q="sync", is_out=False, **kw):
        o, _ = _ak(out)
        i, _ = _ak(in_)
        return self.op(q, lambda e: e.dma_start(out=o, in_=i, **kw), [in_], [out], is_dma=True, is_out=is_out)

    def mm(self, out, lhsT, rhs, start=True, stop=True, **kw):
        o, _ = _ak(out)
        a, _ = _ak(lhsT)
        b, _ = _ak(rhs)
        rd = [lhsT, rhs] + ([] if start else [out])
        return self.op("tensor", lambda e: e.matmul(o, a, b, start=start, stop=stop, **kw), rd, [out])

    def tr(self, out, in_, ident):
        o, _ = _ak(out)
        a, _ = _ak(in_)
        b, _ = _ak(ident)
        return self.op("tensor", lambda e: e.transpose(o, a, b), [in_, ident], [out])

    def act(self, out, in_, func, bias=None, scale=None, accum_out=None, eng="scalar"):
        o, _ = _ak(out)
        a, _ = _ak(in_)
        rd = [in_]
        wr = [out]
        kw = {}
        if bias is not None:
            if isinstance(bias, (int, float)):
                kw["bias"] = float(bias)
            else:
                kw["bias"] = _ak(bias)[0]
                rd.append(bias)
        if scale is not None:
            if isinstance(scale, (int, float)):
                kw["scale"] = float(scale)
            else:
                kw["scale"] = _ak(scale)[0]
                rd.append(scale)
        if accum_out is not None:
            kw["accum_out"] = _ak(accum_out)[0]
            wr.append(accum_out)
        return self.op(eng, lambda e: e.activation(o, a, func, **kw), rd, wr)

    def tt(self, out, a, b, op, eng="vector"):
        o, _ = _ak(out)
        x, _ = _ak(a)
        y, _ = _ak(b)
        return self.op(eng, lambda e: e.tensor_tensor(o, x, y, op), [a, b], [out])

    def ts(self, out, a, s1, s2=None, op0=ALU.mult, op1=None, eng="vector", accum_out=None):
        o, _ = _ak(out)
        x, _ = _ak(a)
        rd = [a]
        wr = [out]

        def sc(s):
            if s is None or isinstance(s, (int, float)):
                return None if s is None else float(s)
            rd.append(s)
            return _ak(s)[0]

        v1 = sc(s1)
        v2 = sc(s2)
        kw = {}
        if op1 is not None:
            kw["op1"] = op1
        if accum_out is not None:
            kw["accum_out"] = _ak(accum_out)[0]
            wr.append(accum_out)
        return self.op(eng, lambda e: e.tensor_scalar(o, x, v1, v2, op0, **kw), rd, wr)

    def stt(self, out, in0, scalar, in1, op0, op1, eng="vector", accum_out=None):
        o, _ = _ak(out)
        x, _ = _ak(in0)
        y, _ = _ak(in1)
        rd = [in0, in1]
        wr = [out]
        if isinstance(scalar, (int, float)):
            sv = float(scalar)
        else:
            sv = _ak(scalar)[0]
            rd.append(scalar)
        kw = {}
        if accum_out is not None:
            kw["accum_out"] = _ak(accum_out)[0]
            wr.append(accum_out)
        return self.op(eng, lambda e: e.scalar_tensor_tensor(o, x, sv, y, op0, op1, **kw), rd, wr)

    def copy(self, out, in_, eng="vector"):
        o, _ = _ak(out)
        a, _ = _ak(in_)
        if eng == "scalar":
            return self.op(eng, lambda e: e.copy(o, a), [in_], [out])
        return self.op(eng, lambda e: e.tensor_copy(o, a), [in_], [out])

    def memset(self, out, val, eng="vector"):
        o, _ = _ak(out)
        return self.op(eng, lambda e: e.memset(o, val), [], [out])

    def reduce(self, out, in_, op, axis=AX.X, eng="vector"):
        o, _ = _ak(out)
        a, _ = _ak(in_)
        return self.op(eng, lambda e: e.tensor_reduce(o, a, axis, op), [in_], [out])

    def recip(self, out, in_):
        o, _ = _ak(out)
        a, _ = _ak(in_)
        return self.op("vector", lambda e: e.reciprocal(o, a), [in_], [out])

    def barrier(self):
        toks = {}
        for e in ENGS:
            if self.cnt[e] > 0:
                toks["E_" + e] = self.cnt[e]
        for i in range(NDMA_SEM):
            if self.dma_cnt[i] > 0:
                toks["D_%d" % i] = self.dma_cnt[i]
        for e in ENGS:
            eobj = getattr(self.nc, e)
            for s_, v_ in toks.items():
                if s_ == "E_" + e:
                    continue
                if self.waited[e].get(s_, -1) >= v_:
                    continue
                self.waited[e][s_] = v_
                eobj.wait_ge(self.sems[s_], v_)

    def finish(self):
        fin = {}
        for s_, v_ in self.out_tokens:
            fin[s_] = max(fin.get(s_, -1), v_)
        for s_, v_ in fin.items():
            self.nc.sync.wait_ge(self.sems[s_], v_)

    def finalize_old(self, block, sems):
        fin = {}
        for s, v in self.out_tokens:
            fin[s] = max(fin.get(s, -1), v)
        ops = self.ops

        def run(eng_name, eng):
            for wl, fn, tok, is_dma in ops[eng_name]:
                for s, v in wl:
                    eng.wait_ge(sems[s], v)
                ins = fn(eng)
                ins.then_inc(sems[tok[0]], 16 if is_dma else 1)
            if eng_name == "sync":
                for s, v in fin.items():
                    eng.wait_ge(sems[s], v)

        @block.tensor
        def _(e):
            run("tensor", e)

        @block.vector
        def _(e):
            run("vector", e)

        @block.scalar
        def _(e):
            run("scalar", e)

        @block.gpsimd
        def _(e):
            run("gpsimd", e)

        @block.sync
        def _(e):
            run("sync", e)


RMS_EPS = 1e-6


class Cfg:
    def __init__(self, T=8192, CTX=256, DFF=2816, NE=8, GW=64):
        self.D = 1024
        self.T = T
        self.CTX = CTX
        self.DFF = DFF
        self.NE = NE
        self.GW = GW
        self.TL = T // 4
        self.CL = CTX // 4
        self.NTOK = self.TL + 128
        self.TS = T + CTX
        blocks = []
        o = 0
        while o < self.TL:
            n = min(512, self.TL - o)
            blocks.append((o, n, 0))
            o += n
        blocks.append((self.TL, 128, 1))
        self.blocks = blocks
        h = len(blocks) // 2
        self.halves = [blocks[:h], blocks[h:]]


def mk_sems(nc, es):
    sems = {}
    for e in ENGS:
        sems["E_" + e] = es.enter_context(nc.semaphore("s_" + e))
    for i in range(NDMA_SEM):
        sems["D_%d" % i] = es.enter_context(nc.semaphore("d_%d" % i))
    return sems


def build_tok(cfg, mode, moe=False):
    D, NTOK, DFF, NE = cfg.D, cfg.NTOK, cfg.DFF, cfg.NE
    NF = DFF // 128
    nc = bass.Bass("TRN2", target_bir_lowering=False)
    dr = lambda n, s, d, k="ExternalInput": nc.dram_tensor(n, s, d, kind=k).ap()
    xT = dr("xT", [D, NTOK], F32)
    cvec = dr("cvec", [D, 2], F32)
    nmod = 2 * D if mode == "A" else 6 * D
    w_mod = dr("w_mod", [D, nmod], F32)
    b_mod = dr("b_mod", [nmod], F32)
    g_mix = dr("g_mix", [D], F32)
    ones_d = dr("ones", [128, 128], F32)
    ident_d = dr("ident", [128, 128], F32)
    if mode == "A":
        hT_o = dr("hT", [D, NTOK], BF16, "ExternalOutput")
    else:
        g_ffn = dr("g_ffn", [D], F32)
        brT = dr("brT", [3, 512, NTOK], BF16)
        w_gate = dr("w_gate", [D, 3 * D], F32)
        w_branch = dr("w_branch", [3, 512, D], F32)
        w_out = dr("w_out", [D, D], F32)
        if moe:
            router = dr("router", [D, NE], F32)
            w1 = dr("w1", [NE, D, DFF], F32)
            w3 = dr("w3", [NE, D, DFF], F32)
            w2 = dr("w2", [NE, DFF, D], F32)
        else:
            w1 = dr("w1", [1, D, DFF], F32)
            w3 = dr("w3", [1, D, DFF], F32)
            w2 = dr("w2", [1, DFF, D], F32)
        x_o = dr("xo", [D, NTOK], F32, "ExternalOutput")

    with ExitStack() as es:
        sb = lambda n, s, d: es.enter_context(nc.sbuf_tensor(n, s, d))
        ps = lambda n, s, d: es.enter_context(nc.psum_tensor(n, s, d))
        xs = sb("xs", [128, 8, NTOK], F32)
        hT = sb("hTs", [128, 8, NTOK], BF16)
        cv = sb("cv", [128, 8, 2], F32)
        siluc = sb("siluc", [128, 8, 2], BF16)
        nch = nmod // 128
        modT = sb("modT", [128, nch, 2], F32)
        bmod = sb("bmod", [128, nch], F32)
        gmix = sb("gmix", [128, 8], F32)
        A1 = sb("A1", [128, 8, 2], F32)
        ones = sb("ones_s", [128, 128], F32)
        ident = sb("ident_s", [128, 128], F32)
        sq = [sb("sq%d" % i, [128, 512], F32) for i in range(2)]
        rstd = sb("rstd", [128, 512], F32)
        tmpf = [sb("tmpf%d" % i, [128, 512], F32) for i in range(2)]
        PS = [ps("ps%d" % i, [128, 512], F32) for i in range(7)]
        if mode == "B":
            gffn = sb("gffn", [128, 8], F32)
            A2 = sb("A2", [128, 8, 2], F32)
            sig = [sb("sig%d" % i, [128, 512], F32) for i in range(2)]
            if moe:
                ntile = NTOK // 128
                gates = sb("gates", [128, ntile, NE], F32)
        sems = mk_sems(nc, es)
        P = Prog(nc, sems)
        psi = [0]

        def nps():
            psi[0] = (psi[0] + 1) % len(PS)
            return PS[psi[0]]

        P.dma(ones[:], ones_d)
        P.dma(ident[:], ident_d)
        for kc in range(8):
            P.dma(xs[:, kc, :], xT[kc * 128:(kc + 1) * 128, :], q=("sync" if kc % 2 == 0 else "gpsimd"))
        with nc.allow_non_contiguous_dma("small param loads"):
            P.dma(cv[:], cvec.rearrange("(c p) n -> p c n", p=128))
            P.dma(bmod[:], b_mod.rearrange("(c p) -> p c", p=128))
            P.dma(gmix[:], g_mix.rearrange("(c p) -> p c", p=128))
            if mode == "B":
                P.dma(gffn[:], g_ffn.rearrange("(c p) -> p c", p=128))
        P.act(siluc[:], cv[:], AF.Silu)
        wi = 0
        ph0 = ExitStack()
        wm = [ph0.enter_context(nc.sbuf_tensor("wm%d" % i, [128, 8, 512], BF16)) for i in range(2)]
        for g0 in range(0, nch, 4):
            w = wm[wi % 2]
            wi += 1
            P.dma(w[:], w_mod[:, g0 * 128:(g0 + 4) * 128].rearrange("(k p) m -> p k m", p=128), q="gpsimd")
            for j in range(4):
                mc = g0 + j
                pt = nps()
                for kc in range(8):
                    P.mm(pt[:, 0:2], w[:, kc, j * 128:(j + 1) * 128], siluc[:, kc, :], start=(kc == 0), stop=(kc == 7))
                P.ts(modT[:, mc, :], pt[:, 0:2], bmod[:, mc:mc + 1], None, op0=ALU.add)
        P.barrier()
        ph0.close()
        def mkA(Aout, gvec, sc_base):
            for kc in range(8):
                P.ts(Aout[:, kc, :], modT[:, sc_base + kc, :], 1.0, gvec[:, kc:kc + 1], op0=ALU.add, op1=ALU.mult)

        mkA(A1, gmix, 8)
        if mode == "B":
            mkA(A2, gffn, 32)

        def norm(Acoef, sh_base, want_f32=None):
            for (o, n, col) in cfg.blocks:
                pt = nps()
                for kc in range(8):
                    s = sq[kc % 2]
                    P.act(s[:, 0:n], xs[:, kc, o:o + n], AF.Square)
                    P.mm(pt[:, 0:n], ones[:], s[:, 0:n], start=(kc == 0), stop=(kc == 7))
                P.ts(rstd[:, 0:n], pt[:, 0:n], 1.0 / 1024.0, RMS_EPS, op0=ALU.mult, op1=ALU.add)
                P.recip(rstd[:, 0:n], rstd[:, 0:n])
                P.act(rstd[:, 0:n], rstd[:, 0:n], AF.Sqrt)
                for kc in range(8):
                    t = tmpf[kc % 2]
                    P.tt(t[:, 0:n], xs[:, kc, o:o + n], rstd[:, 0:n], ALU.mult)
                    P.act(hT[:, kc, o:o + n], t[:, 0:n], AF.Identity, bias=modT[:, sh_base + kc, col:col + 1], scale=Acoef[:, kc, col:col + 1])
                    if want_f32 is not None:
                        P.act(hf[:, kc, 0:n], t[:, 0:n], AF.Identity, bias=modT[:, sh_base + kc, col:col + 1], scale=Acoef[:, kc, col:col + 1])
                if want_f32 is not None:
                    want_f32(o, n)

        norm(A1, 0)
        if mode == "A":
            for kc in range(8):
                P.dma(hT_o[kc * 128:(kc + 1) * 128, :], hT[:, kc, :], is_out=True)
            P.barrier()
            P.finish()
            return nc

        P.barrier()
        with ExitStack() as ph:
            sb2 = lambda n, s_, d: ph.enter_context(nc.sbuf_tensor(n, s_, d))
            br = sb2("br", [128, 3, 4, 512], BF16)
            mT = sb2("mT", [128, 8, 512], BF16)
            macc = sb2("macc", [128, 512], F32)
            wg = [sb2("wg%d" % i, [128, 8, 128], BF16) for i in range(3)]
            wb = [sb2("wb%d" % i, [128, 4, 128], BF16) for i in range(3)]
            wi = 0
            for (o, n, col) in cfg.blocks:
                for n_ in range(3):
                    for g in range(4):
                        P.dma(br[:, n_, g, 0:n], brT[n_, g * 128:(g + 1) * 128, o:o + n], q="sync")
                for dc in range(8):
                    for n_ in range(3):
                        wgt = wg[wi % 3]
                        wbt = wb[wi % 3]
                        wi += 1
                        c0 = n_ * D + dc * 128
                        P.dma(wgt[:], w_gate[:, c0:c0 + 128].rearrange("(k p) m -> p k m", p=128), q="gpsimd")
                        P.dma(wbt[:], w_branch[n_, :, dc * 128:(dc + 1) * 128].rearrange("(k p) m -> p k m", p=128), q="gpsimd")
                        pg = nps()
                        for kc in range(8):
                            P.mm(pg[:, 0:n], wgt[:, kc, :], hT[:, kc, o:o + n], start=(kc == 0), stop=(kc == 7))
                        pp = nps()
                        for g in range(4):
                            P.mm(pp[:, 0:n], wbt[:, g, :], br[:, n_, g, 0:n], start=(g == 0), stop=(g == 3))
                        sg = sig[wi % 2]
                        P.act(sg[:, 0:n], pg[:, 0:n], AF.Sigmoid)
                        if n_ == 0:
                            P.tt(macc[:, 0:n], sg[:, 0:n], pp[:, 0:n], ALU.mult)
                        else:
                            P.tt(sg[:, 0:n], sg[:, 0:n], pp[:, 0:n], ALU.mult)
                            P.tt(macc[:, 0:n], macc[:, 0:n], sg[:, 0:n], ALU.add, eng="gpsimd")
                    P.copy(mT[:, dc, 0:n], macc[:, 0:n], eng="scalar")
                for dc in range(8):
                    wgt = wg[wi % 3]
                    wi += 1
                    P.dma(wgt[:], w_out[:, dc * 128:(dc + 1) * 128].rearrange("(k p) m -> p k m", p=128), q="gpsimd")
                    py = nps()
                    for kc in range(8):
                        P.mm(py[:, 0:n], wgt[:, kc, :], mT[:, kc, 0:n], start=(kc == 0), stop=(kc == 7))
                    P.stt(xs[:, dc, o:o + n], py[:, 0:n], modT[:, 16 + dc, col:col + 1], xs[:, dc, o:o + n], ALU.mult, ALU.add)
            P.barrier()

        ph = ExitStack()
        sb2 = lambda n, s_, d: ph.enter_context(nc.sbuf_tensor(n, s_, d))
        if moe:
            hf = sb2("hf", [128, 8, 512], F32)
            rt = sb2("rt", [128, 8, NE], F32)
            lg = sb2("lg", [128, NE], F32)
            l2 = sb2("l2", [128, NE], F32)
            m1 = sb2("m1", [128, 1], F32)
            m2 = sb2("m2", [128, 1], F32)
            nm1 = sb2("nm1", [128, 1], F32)
            den = sb2("den", [128, 1], F32)
            ex = sb2("ex", [128, NE], F32)
            with nc.allow_non_contiguous_dma("router"):
                P.dma(rt[:], router.rearrange("(k p) e -> p k e", p=128))

            def route(o, n):
                for t0 in range(0, n, 128):
                    ti = (o + t0) // 128
                    pl = nps()
                    for kc in range(8):
                        P.mm(pl[:, 0:NE], hf[:, kc, t0:t0 + 128], rt[:, kc, :], start=(kc == 0), stop=(kc == 7))
                    P.copy(lg[:], pl[:, 0:NE])
                    P.reduce(m1[:], lg[:], ALU.max)
                    P.ts(l2[:], lg[:], m1[:, 0:1], -1e30, op0=ALU.is_equal, op1=ALU.mult)
                    P.tt(l2[:], l2[:], lg[:], ALU.add)
                    P.reduce(m2[:], l2[:], ALU.max)
                    P.ts(nm1[:], m1[:], -1.0, None, op0=ALU.mult)
                    P.act(ex[:], lg[:], AF.Exp, bias=nm1[:, 0:1])
                    P.act(den[:], m2[:], AF.Exp, bias=nm1[:, 0:1])
                    P.ts(den[:], den[:], 1.0, None, op0=ALU.add)
                    P.recip(den[:], den[:])
                    P.ts(l2[:], lg[:], m2[:, 0:1], None, op0=ALU.is_ge)
                    P.tt(l2[:], l2[:], ex[:], ALU.mult)
                    P.ts(gates[:, ti, :], l2[:], den[:, 0:1], None, op0=ALU.mult)

            norm(A2, 24, want_f32=route)
        else:
            norm(A2, 24)

        P.barrier()
        ph.close()
        ph = ExitStack()
        sb2 = lambda n, s_, d: ph.enter_context(nc.sbuf_tensor(n, s_, d))
        maxhalf = max(sum(n for _, n, _ in h) for h in cfg.halves)
        actT = sb2("actT", [128, NF, maxhalf], BF16)
        w1b = [sb2("w1b%d" % i, [128, 8, 128], BF16) for i in range(3)]
        w3b = [sb2("w3b%d" % i, [128, 8, 128], BF16) for i in range(3)]
        w2b = [sb2("w2b%d" % i, [128, NF, 128], BF16) for i in range(2)]
        if moe:
            gcol = sb2("gcol", [128, 128], F32)
            gateB = [sb2("gateB%d" % i, [128, 512], F32) for i in range(3)]
        nexp = NE if moe else 1
        wi = 0
        w2i = 0
        gi = 0
        for half in cfg.halves:
            hb = half[0][0]
            for e in range(nexp):
                for fc in range(NF):
                    a = w1b[wi % 3]
                    b = w3b[wi % 3]
                    wi += 1
                    P.dma(a[:], w1[e, :, fc * 128:(fc + 1) * 128].rearrange("(k p) m -> p k m", p=128), q="gpsimd")
                    P.dma(b[:], w3[e, :, fc * 128:(fc + 1) * 128].rearrange("(k p) m -> p k m", p=128), q="gpsimd")
                    for (o, n, col) in half:
                        p1 = nps()
                        for kc in range(8):
                            P.mm(p1[:, 0:n], a[:, kc, :], hT[:, kc, o:o + n], start=(kc == 0), stop=(kc == 7))
                        p3 = nps()
                        for kc in range(8):
                            P.mm(p3[:, 0:n], b[:, kc, :], hT[:, kc, o:o + n], start=(kc == 0), stop=(kc == 7))
                        sg = sig[gi % 2]
                        gi += 1
                        P.act(sg[:, 0:n], p1[:, 0:n], AF.Silu)
                        P.tt(actT[:, fc, o - hb:o - hb + n], sg[:, 0:n], p3[:, 0:n], ALU.mult)
                gB = {}
                if moe:
                    for (o, n, col) in half:
                        gb = gateB[gi % 3]
                        gi += 1
                        for t0 in range(0, n, 128):
                            ti = (o + t0) // 128
                            P.copy(gcol[:], gates[:, ti, e:e + 1].to_broadcast([128, 128]))
                            pgm = nps()
                            P.mm(pgm[:, 0:128], gcol[:], ident[:])
                            P.copy(gb[:, t0:t0 + 128], pgm[:, 0:128], eng="scalar")
                        gB[o] = gb
                for dc in range(8):
                    c = w2b[w2i % 2]
                    w2i += 1
                    P.dma(c[:], w2[e, :, dc * 128:(dc + 1) * 128].rearrange("(k p) m -> p k m", p=128), q="gpsimd")
                    for (o, n, col) in half:
                        py = nps()
                        for fc in range(NF):
                            P.mm(py[:, 0:n], c[:, fc, :], actT[:, fc, o - hb:o - hb + n], start=(fc == 0), stop=(fc == NF - 1))
                        if moe:
                            t = tmpf[dc % 2]
                            P.tt(t[:, 0:n], py[:, 0:n], gB[o][:, 0:n], ALU.mult)
                            P.stt(xs[:, dc, o:o + n], t[:, 0:n], modT[:, 40 + dc, col:col + 1], xs[:, dc, o:o + n], ALU.mult, ALU.add)
                        else:
                            P.stt(xs[:, dc, o:o + n], py[:, 0:n], modT[:, 40 + dc, col:col + 1], xs[:, dc, o:o + n], ALU.mult, ALU.add)
        for kc in range(8):
            P.dma(x_o[kc * 128:(kc + 1) * 128, :], xs[:, kc, :], is_out=True)
        P.barrier()
        ph.close()
        P.barrier()
        P.finish()
    return nc


DBG = int(os.environ.get('SEQ_DBG', '47'))
CHD = int(os.environ.get('CH_DBG', '9'))

GN_EPS = 64e-5
NAT_KH_MAX = 8
NAT_KW = 16
POOL_WINDOWS = (2, 4, 8, 16)
OFF_RWKV = 512
OFF_Q = 2272
OFF_K = 2784
OFF_V = 3296
OFF_GATE = 3808
FM_BLOCKS = [("r", 128), ("k", 128), ("v", 128), ("wlo", 64), ("alo", 64), ("glo", 96), ("q", 128), ("kn", 128)]
NFM = sum(n for _, n in FM_BLOCKS)


def nat_geometry(T, GW=64):
    rows = T // GW
    kh = min(NAT_KH_MAX, rows)
    ntl = rows // 2
    rs = lambda r: min(max(r - kh // 2, 0), rows - kh)
    cs = lambda c: min(max(c - NAT_KW // 2, 0), GW - NAT_KW)
    qi = np.arange(128)
    ki = np.arange(128)
    uniq = {}
    tiles = []
    plan = []
    for m in range(ntl):
        r = 2 * m + qi // GW
        c = qi % GW
        rsv = np.array([rs(x) for x in r])
        csv = np.array([cs(x) for x in c])
        lo = min(rsv) // 2
        hi = (max(rsv) + kh - 1) // 2
        lst = []
        for kt in range(lo, hi + 1):
            kr = 2 * kt + ki // GW
            kc = ki % GW
            valid = ((kr[:, None] >= rsv[None, :]) & (kr[:, None] < rsv[None, :] + kh) &
                     (kc[:, None] >= csv[None, :]) & (kc[:, None] < csv[None, :] + NAT_KW))
            if not valid.any():
                continue
            ro = (kr[:, None] - r[None, :] + NAT_KH_MAX - 1) * valid
            co = (kc[:, None] - c[None, :] + NAT_KW - 1) * valid
            key = (valid.tobytes(), ro.tobytes(), co.tobytes())
            if key not in uniq:
                uniq[key] = len(tiles)
                tiles.append((valid, ro, co))
            lst.append((kt, uniq[key]))
        plan.append(lst)
    return plan, tiles


def pool_bands(W):
    Tt = 512
    t = np.arange(Tt)
    lo = np.clip(t - W // 2, 0, Tt)
    hi = np.clip(t - W // 2 + W, 0, Tt)
    M = np.zeros((Tt, Tt), np.float32)
    for j in range(Tt):
        M[lo[j]:hi[j], j] = 1.0 / float(hi[j] - lo[j])
    M -= np.eye(Tt, dtype=np.float32)
    b = np.stack([M[0:128, 128:256], M[128:256, 128:256], M[256:384, 128:256], M[0:128, 0:128], M[384:512, 384:512]])
    return np.ascontiguousarray(b)


def build_seq(cfg, stop=99):
    D, T, CTX, TS = cfg.D, cfg.T, cfg.CTX, cfg.TS
    NTL = T // 128
    NTC = CTX // 128
    NTS = TS // 128
    plan, tiles = nat_geometry(T, cfg.GW)
    NT = len(tiles)
    nc = bass.Bass("TRN2", target_bir_lowering=False)
    dr = lambda n, s, d, k="ExternalInput": nc.dram_tensor(n, s, d, kind=k).ap()
    hTs = dr("hTs", [D, TS], BF16)
    w_fm = dr("w_fm", [D, NFM], F32)
    w_tm = dr("w_tm", [D, 256], F32)
    pch_d = dr("pch", [128, 18], F32)
    plo_d = dr("plo", [64, 4], F32)
    pgl_d = dr("pgl", [96, 2], F32)
    dw2_d = dr("dw2", [64, 128], F32)
    aw2_d = dr("aw2", [64, 128], F32)
    gg2_d = dr("gg2", [96, 128], F32)
    poolw_d = dr("poolw", [128, 128], F32)
    bands_d = dr("bands", [5, 128, 128], F32)
    nbias_d = dr("nbias", [NT, 2, 128, 128], F32)
    nmask_d = dr("nmask", [NT, 128, 128], F32)
    ident_d = dr("ident", [128, 128], F32)
    bones_d = dr("bones", [128, 128], F32)
    hsel_d = dr("hsel", [2, 128], F32)
    opad_d = dr("opad", [2, 128, 128], F32)
    cmask_d = dr("cmask", [2, 3, 128, 128], F32)
    brT_o = dr("brTo", [3, 128, TS], BF16, "ExternalOutput")
    di = lambda n, s, d: nc.dram_tensor(n, s, d).ap()
    zlat = di("zlat", [6, 128, T + 2], F32)
    zctx = di("zctx", [6, 128, CTX + 2], F32)
    xtm = [[di("xtm_%d_%d" % (d, i), [TS, 128], F32) for i in range(3)] for d in range(2)]
    xkk = di("xtm_kk", [TS, 128], F32)
    xr = di("xtm_r", [TS, 128], F32)
    bon_d = di("bon_d", [128, TS], F32)
    gate_d = di("gate_d", [128, TS], F32)

    seqs = [("lat", 0, T, zlat), ("ctx", T, CTX, zctx)]

    with ExitStack() as es:
        es.enter_context(nc.allow_non_contiguous_dma("scratch layouts"))
        sb = lambda n, s, d: es.enter_context(nc.sbuf_tensor(n, s, d))
        ps = lambda n, s, d: es.enter_context(nc.psum_tensor(n, s, d))
        sems = mk_sems(nc, es)
        P = Prog(nc, sems)
        PS = [ps("ps%d" % i, [128, 512], F32) for i in range(8)]
        psi = [0]

        def nps(k=8):
            psi[0] = (psi[0] + 1) % k
            return PS[psi[0]]

        ident = sb("ident_s", [128, 128], F32)
        bones = sb("bones_s", [128, 128], F32)
        hsel = sb("hsel_s", [2, 128], F32)
        pch = sb("pch_s", [128, 18], F32)
        pder = sb("pder", [128, 8], F32)
        plo = sb("plo_s", [64, 4], F32)
        plod = sb("plod", [64, 2], F32)
        pgl = sb("pgl_s", [96, 2], F32)
        pgld = sb("pgld", [96, 1], F32)
        dw2 = sb("dw2_s", [64, 128], F32)
        aw2 = sb("aw2_s", [64, 128], F32)
        gg2 = sb("gg2_s", [96, 128], F32)
        zero = sb("zero_s", [128, 8], F32)
        P.dma(ident[:], ident_d)
        P.dma(bones[:], bones_d)
        P.dma(hsel[:], hsel_d)
        P.dma(pch[:], pch_d)
        P.dma(plo[:], plo_d)
        P.dma(pgl[:], pgl_d)
        P.dma(dw2[:], dw2_d)
        P.dma(aw2[:], aw2_d)
        P.dma(gg2[:], gg2_d)
        P.memset(zero[:], 0.0)
        for i in range(3):
            P.tt(pder[:, i:i + 1], pch[:, 2 * i:2 * i + 1], pch[:, 2 * i + 1:2 * i + 2], ALU.add)
            P.ts(pder[:, i:i + 1], pder[:, i:i + 1], -1.0, 1.0, op0=ALU.mult, op1=ALU.add)
        P.ts(pder[:, 3:4], pch[:, 11:12], -1.0, 1.0, op0=ALU.mult, op1=ALU.add)
        P.ts(pder[:, 4:5], pch[:, 16:17], 0.125, None, op0=ALU.mult)
        for i in range(2):
            P.tt(plod[:, i:i + 1], plo[:, 2 * i:2 * i + 1], plo[:, 2 * i + 1:2 * i + 2], ALU.add)
            P.ts(plod[:, i:i + 1], plod[:, i:i + 1], -1.0, 1.0, op0=ALU.mult, op1=ALU.add)
        P.tt(pgld[:, 0:1], pgl[:, 0:1], pgl[:, 1:2], ALU.add)
        P.ts(pgld[:, 0:1], pgld[:, 0:1], -1.0, 1.0, op0=ALU.mult, op1=ALU.add)
        for (_, _, L, zz) in seqs:
            for rb in range(6):
                P.dma(Sub(zz[rb, :, 0:1], ("h", rb, 0)), zero[:, 0:1])
                P.dma(Sub(zz[rb, :, L + 1:L + 2], ("h", rb, 1)), zero[:, 0:1])

        if stop <= 0:
            P.barrier()
            P.finish()
            return nc
        ph = ExitStack()
        sb2 = lambda n, s, d: ph.enter_context(nc.sbuf_tensor(n, s, d))
        wfm = sb2("wfm", [128, 8, NFM], BF16)
        wtm = sb2("wtm", [128, 8, 256], BF16)
        P.dma(wfm[:], w_fm.rearrange("(k p) m -> p k m", p=128), q="gpsimd")
        P.dma(wtm[:], w_tm.rearrange("(k p) m -> p k m", p=128), q="gpsimd")
        QN = sb2("QN", [128, TS], BF16)
        KN = sb2("KN", [128, TS], BF16)
        VP = sb2("VP", [128, NTS, 2, 128], BF16)
        zp = sb2("zp", [128, NTS, 128], F32)
        hb = [sb2("hb%d" % i, [128, 8, 512], BF16) for i in range(2)]
        stg = [sb2("stg%d" % i, [128, 512], F32) for i in range(3)]
        sqn = [sb2("sqn%d" % i, [128, 512], F32) for i in range(2)]
        rsn = [sb2("rsn%d" % i, [128, 512], F32) for i in range(2)]
        P.memset(VP[:], 0.0, eng="gpsimd")
        if stop <= 0.5:
            P.barrier()
            ph.close()
            P.barrier()
            P.finish()
            return nc
        bi = 0
        si = 0
        for (sname, s0, L, zz) in seqs:
            for o in range(0, L, 512):
                n = min(512, L - o)
                h = hb[bi % 2]
                bi += 1
                P.dma(h[:, :, 0:n], hTs[:, s0 + o:s0 + o + n].rearrange("(k p) n -> p k n", p=128))
                c0 = 0
                for rbi, (nm, M) in enumerate(FM_BLOCKS):
                    pt = nps()
                    for kc in range(8):
                        P.mm(pt[0:M, 0:n], wfm[:, kc, c0:c0 + M], h[:, kc, 0:n], start=(kc == 0), stop=(kc == 7))
                    c0 += M
                    if rbi < 6:
                        st = stg[si % 3]
                        si += 1
                        P.copy(st[0:M, 0:n], pt[0:M, 0:n], eng=("scalar" if rbi % 2 else "vector"))
                        if DBG & 2:
                            P.dma(Sub(zz[rbi, 0:M, 1 + o:1 + o + n], ("b", rbi, o)), st[0:M, 0:n])
                    elif DBG & 4:
                        s_ = sqn[rbi % 2]
                        P.act(s_[:, 0:n], pt[:, 0:n], AF.Square)
                        p2 = nps()
                        P.mm(p2[:, 0:n], bones[:], s_[:, 0:n])
                        r_ = rsn[rbi % 2]
                        P.ts(r_[:, 0:n], p2[:, 0:n], 1.0 / 64.0, 1e-6, op0=ALU.mult, op1=ALU.add)
                        P.recip(r_[:, 0:n], r_[:, 0:n])
                        P.act(r_[:, 0:n], r_[:, 0:n], AF.Sqrt)
                        P.tt(r_[:, 0:n], r_[:, 0:n], pt[:, 0:n], ALU.mult)
                        if nm == "q":
                            P.ts(QN[:, s0 + o:s0 + o + n], r_[:, 0:n], pder[:, 4:5], None, op0=ALU.mult)
                        else:
                            P.ts(KN[:, s0 + o:s0 + o + n], r_[:, 0:n], pch[:, 17:18], None, op0=ALU.mult)
                for t0 in (range(0, n, 128) if DBG & 8 else []):
                    ti = (s0 + o + t0) // 128
                    pt = nps()
                    for kc in range(8):
                        P.mm(pt[:, 0:256], h[:, kc, t0:t0 + 128], wtm[:, kc, :], start=(kc == 0), stop=(kc == 7))
                    P.copy(zp[:, ti, :], pt[:, 0:128])
                    if DBG & 16:
                        P.copy(VP[:, ti, 0, 0:64], pt[:, 128:192], eng="scalar")
                        P.copy(VP[:, ti, 1, 64:128], pt[:, 192:256], eng="scalar")
                    if DBG & 32:
                        P.copy(VP[:, ti, 0, 0:64], pt[:, 128:192], eng="vector")
                        P.copy(VP[:, ti, 1, 64:128], pt[:, 192:256], eng="vector")

        if stop <= 1:
            P.barrier()
            ph.close()
            P.barrier()
            P.finish()
            return nc
        bands = sb2("bands_s", [128, 5, 128], F32)
        poolw = sb2("poolw_s", [128, 128], F32)
        for i in range(5):
            P.dma(bands[:, i, :], bands_d[i])
        P.dma(poolw[:], poolw_d)
        pst = [sb2("pst%d" % i, [128, 512], F32) for i in range(2)]
        pob = [sb2("pob%d" % i, [128, 512], BF16) for i in range(2)]
        bi = 0
        for (sname, s0, L, zz) in seqs:
            nt = L // 128
            tb = s0 // 128
            for o in range(0, nt, 4):
                m = min(4, nt - o)
                pt = nps()
                for j in range(m):
                    i = o + j
                    terms = []
                    if i > 0:
                        terms.append((i - 1, 0))
                    terms.append((i, 3 if i == 0 else (4 if i == nt - 1 else 1)))
                    if i < nt - 1:
                        terms.append((i + 1, 2))
                    for q_, (ti, bidx) in enumerate(terms):
                        P.mm(pt[:, j * 128:(j + 1) * 128], zp[:, tb + ti, :], bands[:, bidx, :], start=(q_ == 0), stop=(q_ == len(terms) - 1))
                st = pst[bi % 2]
                ob = pob[bi % 2]
                bi += 1
                P.copy(st[:, 0:m * 128], pt[:, 0:m * 128])
                p2 = nps()
                P.mm(p2[:, 0:m * 128], poolw[:], st[:, 0:m * 128])
                P.ts(ob[:, 0:m * 128], p2[:, 0:m * 128], pch[:, 15:16], None, op0=ALU.mult)
                P.dma(brT_o[0, :, s0 + o * 128:s0 + (o + m) * 128], ob[:, 0:m * 128], is_out=True)

        if stop <= 2:
            P.barrier()
            ph.close()
            P.barrier()
            P.finish()
            return nc
        nbm = sb2("nbm", [128, NT, 2, 128], F32)
        nmk = sb2("nmk", [128, NT, 128], F32)
        opad = sb2("opad_s", [128, 2, 128], BF16)
        for i in range(NT):
            P.dma(nmk[:, i, :], nmask_d[i])
            for hh in range(2):
                P.dma(nbm[:, i, hh, :], nbias_d[i, hh])
        opf = sb2("opf", [128, 2, 128], F32)
        for hh in range(2):
            P.dma(opf[:, hh, :], opad_d[hh])
        P.copy(opad[:], opf[:])
        P.ts(nmk[:], nmk[:], 1.0, 30000.0, op0=ALU.subtract, op1=ALU.mult)
        for hh in range(2):
            P.tt(nbm[:, :, hh, :], nbm[:, :, hh, :], nmk[:], ALU.add)
        sct = [sb2("sct%d" % i, [128, 128], F32) for i in range(3)]
        ptb = [sb2("ptb%d" % i, [128, 128], BF16) for i in range(4)]
        nrc = sb2("nrc", [128, 128], F32)
        nob = [sb2("nob%d" % i, [128, 512], BF16) for i in range(2)]
        ctx_tiles = [NTL + i for i in range(NTC)]
        qtiles = [(m, [(kt, tid) for kt, tid in plan[m]] + [(c, None) for c in ctx_tiles]) for m in range(NTL)]
        qtiles += [(NTL + i, [(c, None) for c in ctx_tiles]) for i in range(NTC)]
        ci = 0
        po = PS[6]
        pd = PS[7]
        for qi_, (m, klist) in enumerate(qtiles):
            tot = 2 * len(klist)
            cnt = 0
            for hh in range(2):
                hs = slice(hh * 64, (hh + 1) * 64)
                for (kt, tid) in klist:
                    pt = nps(6)
                    P.mm(pt[:, 0:128], KN[hs, kt * 128:(kt + 1) * 128], QN[hs, m * 128:(m + 1) * 128])
                    pb = ptb[ci % 4]
                    if tid is not None:
                        sc_ = sct[ci % 3]
                        P.tt(sc_[:], pt[:, 0:128], nbm[:, tid, hh, :], ALU.add)
                        P.act(pb[:], sc_[:], AF.Exp)
                    else:
                        P.act(pb[:], pt[:, 0:128], AF.Exp)
                    ci += 1
                    P.mm(po[:, 0:128], VP[:, kt, hh, :], pb[:], start=(cnt == 0), stop=(cnt == tot - 1))
                    P.mm(pd[:, 0:128], opad[:, hh, :], pb[:], start=(cnt == 0), stop=(cnt == tot - 1))
                    cnt += 1
            ob = nob[(qi_ // 4) % 2]
            j = qi_ % 4
            P.recip(nrc[:], pd[:, 0:128])
            P.tt(ob[:, j * 128:(j + 1) * 128], po[:, 0:128], nrc[:], ALU.mult)
            if j == 3 or qi_ == len(qtiles) - 1:
                q0 = (qi_ // 4) * 4
                P.dma(brT_o[2, :, q0 * 128:(qi_ + 1) * 128], ob[:, 0:(j + 1) * 128], is_out=True)
        if stop <= 3:
            P.barrier()
            ph.close()
            P.barrier()
            P.finish()
            return nc
        P.barrier()
        ph.close()

        ph = ExitStack()
        sb2 = lambda n, s, d: ph.enter_context(nc.sbuf_tensor(n, s, d))
        vS = sb2("vS", [128, TS], F32)
        Od = [sb2("O%d" % d, [128, TS], F32) for d in range(2)]
        ph4 = ExitStack()
        sb2 = lambda n, s, d: ph4.enter_context(nc.sbuf_tensor(n, s, d))
        NB = 256
        NCH = NB // 64
        cm = sb2("cm", [128, 2, 3, 128], F32)
        for d in range(2):
            for i in range(3):
                P.dma(cm[:, d, i, :], cmask_d[d, i])
        onesb = sb2("onesb", [128, 64], F32)
        P.memset(onesb[:], 1.0)
        zin = {nm: sb2("zin_" + nm, [M, NB + 2], F32) for nm, M in FM_BLOCKS[:6]}
        zs = {nm: sb2("zs_" + nm, [M, NB], F32) for nm, M in FM_BLOCKS[:6] if nm != "v"}
        tw = sb2("tw", [64, NB], F32)
        ed = [sb2("e%d" % d, [128, NB], F32) for d in range(2)]
        ad = [sb2("a%d" % d, [128, NB], F32) for d in range(2)]
        lw = [sb2("lw%d" % d, [128, NB], F32) for d in range(2)]
        kx = sb2("kx", [128, NB], F32)
        ksq = sb2("ksq", [128, NB], F32)
        rn = sb2("rn", [128, NB], F32)
        kk = sb2("kk", [128, NB], F32)
        nkka = [sb2("nkka%d" % d, [128, NB], F32) for d in range(2)]
        kd = [sb2("kd%d" % d, [128, NB], F32) for d in range(2)]
        tq = sb2("tq", [128, NB], F32)
        bonb = sb2("bonb", [128, NB], F32)
        sgl = sb2("sgl", [96, NB], F32)
        gtb = sb2("gtb", [128, NB], F32)
        Lc = [sb2("Lc%d" % d, [128, NB], F32) for d in range(2)]
        Lt = sb2("Lt", [128, NB], F32)
        E1 = sb2("E1", [128, NB], F32)
        Eex = sb2("Eex", [128, NB], F32)
        Ein = sb2("Ein", [128, NB], F32)
        Een = sb2("Een", [128, NB], F32)
        Lend = [sb2("Lend%d" % d, [128, NCH], F32) for d in range(2)]
        gC = [sb2("gC%d" % d, [128, NCH], F32) for d in range(2)]
        QR = [sb2("QR%d" % d, [128, NCH, 192], F32) for d in range(2)]
        nAp = [sb2("nAp%d" % d, [128, NCH, 2, 64], F32) for d in range(2)]
        Ktp = [sb2("Ktp%d" % d, [128, NCH, 2, 64], F32) for d in range(2)]
        Aep = [sb2("Aep%d" % d, [128, NCH, 2, 64], F32) for d in range(2)]
        Kep = [sb2("Kep%d" % d, [128, NCH, 2, 64], F32) for d in range(2)]
        Vp = [sb2("Vp%d" % d, [128, NCH, 2, 64], F32) for d in range(2)]
        for d in range(2):
            P.memset(QR[d][:], 0.0, eng="gpsimd")
            for t_ in (nAp, Ktp, Aep, Kep, Vp):
                P.memset(t_[d][:], 0.0, eng="gpsimd")
        bdn = ["XT", "X", "XTb", "Xb", "AkT", "Pm", "AeT", "KeT", "Vbd", "dg"]
        BD = [[{nm: sb2("%s_%d_%d" % (nm, d, p), [128, 128], F32) for nm in bdn} for p in range(2)] for d in range(2)]
        cpn = ["Brna", "Brk", "Vc"]
        CP = [[{nm: sb2("%s_%d_%d" % (nm, d, p), [128, 64], F32) for nm in cpn} for p in range(2)] for d in range(2)]
        Ubd = [sb2("Ubd%d" % d, [128, 128], F32) for d in range(2)]
        Hbd = [sb2("Hbd%d" % d, [128, 128], F32) for d in range(2)]
        Hc = [sb2("Hc%d" % d, [128, 64], F32) for d in range(2)]
        Uc = [sb2("Uc%d" % d, [128, 64], F32) for d in range(2)]
        RHc = [sb2("RHc%d" % d, [128, 64], F32) for d in range(2)]
        for d in range(2):
            P.memset(Ubd[d][:], 0.0)
            P.memset(Hbd[d][:], 0.0)
            P.memset(Hc[d][:], 0.0)
        mus = {"r": (0, 1, 0), "k": (2, 3, 1), "v": (4, 5, 2)}
        v3 = lambda ap, n: ap.rearrange("p (c t) -> p c t", t=64)

        def prep_block(s0, o, n, zz, dsel):
            g0 = s0 + o
            nch = n // 64
            for rbi, (nm, M) in enumerate(FM_BLOCKS[:6]):
                P.dma(zin[nm][:, 0:n + 2], Sub(zz[rbi, 0:M, o:o + n + 2], None))
                z = zin[nm]
                if nm in mus:
                    a_, b_, c_ = mus[nm]
                    mp, mn, c0_ = pch[:, a_:a_ + 1], pch[:, b_:b_ + 1], pder[:, c_:c_ + 1]
                elif nm == "wlo":
                    mp, mn, c0_ = plo[:, 0:1], plo[:, 1:2], plod[:, 0:1]
                elif nm == "alo":
                    mp, mn, c0_ = plo[:, 2:3], plo[:, 3:4], plod[:, 1:2]
                else:
                    mp, mn, c0_ = pgl[:, 0:1], pgl[:, 1:2], pgld[:, 0:1]
                out = vS[:, g0:g0 + n] if nm == "v" else zs[nm][:, 0:n]
                P.ts(out, z[:, 1:n + 1], c0_, None, op0=ALU.mult)
                P.stt(out, z[:, 0:n], mp, out, ALU.mult, ALU.add)
                P.stt(out, z[:, 2:n + 2], mn, out, ALU.mult, ALU.add)
            r_ = zs["r"]
            k_ = zs["k"]
            P.ts(kx[:, 0:n], k_[:, 0:n], pch[:, 10:11], None, op0=ALU.mult)
            P.act(ksq[:, 0:n], kx[:, 0:n], AF.Square)
            p2 = nps()
            P.mm(p2[:, 0:n], bones[:], ksq[:, 0:n])
            P.ts(rn[:, 0:n], p2[:, 0:n], 1e-24, None, op0=ALU.max)
            P.recip(rn[:, 0:n], rn[:, 0:n])
            P.act(rn[:, 0:n], rn[:, 0:n], AF.Sqrt)
            P.tt(kk[:, 0:n], kx[:, 0:n], rn[:, 0:n], ALU.mult)
            P.act(tw[:, 0:n], zs["wlo"][:, 0:n], AF.Tanh)
            for d in range(2):
                ds_ = slice(d * 32, (d + 1) * 32)
                pw = nps()
                P.mm(pw[:, 0:n], dw2[ds_, :], tw[ds_, 0:n])
                P.act(ed[d][:, 0:n], pw[:, 0:n], AF.Sigmoid, bias=pch[:, 6 + d:7 + d])
                P.ts(lw[d][:, 0:n], ed[d][:, 0:n], -0.6065306597126334, None, op0=ALU.mult)
                pa = nps()
                P.mm(pa[:, 0:n], aw2[ds_, :], zs["alo"][ds_, 0:n])
                P.act(ad[d][:, 0:n], pa[:, 0:n], AF.Sigmoid, bias=pch[:, 8 + d:9 + d])
                P.stt(nkka[d][:, 0:n], ad[d][:, 0:n], -1.0, kk[:, 0:n], ALU.mult, ALU.mult)
                P.ts(tq[:, 0:n], ad[d][:, 0:n], pch[:, 11:12], pder[:, 3:4], op0=ALU.mult, op1=ALU.add)
                P.tt(kd[d][:, 0:n], k_[:, 0:n], tq[:, 0:n], ALU.mult)
            if dsel == 0:
                P.tt(tq[:, 0:n], kd[0][:, 0:n], kd[1][:, 0:n], ALU.add)
                P.stt(tq[:, 0:n], tq[:, 0:n], pch[:, 12:13], r_[:, 0:n], ALU.mult, ALU.mult)
                pbn = nps()
                P.mm(pbn[:, 0:n], bones[:], tq[:, 0:n])
                P.tt(bonb[:, 0:n], pbn[:, 0:n], vS[:, g0:g0 + n], ALU.mult)
                P.dma(Sub(bon_d[:, g0:g0 + n], g0), bonb[:, 0:n])
                P.act(sgl[:, 0:n], zs["glo"][:, 0:n], AF.Sigmoid)
                pgt = nps()
                P.mm(pgt[:, 0:n], gg2[:], sgl[:, 0:n])
                P.copy(gtb[:, 0:n], pgt[:, 0:n], eng="scalar")
                P.dma(Sub(gate_d[:, g0:g0 + n], g0), gtb[:, 0:n])
            d = dsel
            for c in range(nch):
                cs = slice(c * 64, (c + 1) * 64)
                P.op("vector", lambda e, c=c, cs=cs: e.tensor_tensor_scan(Lc[d][:, cs], onesb[:, 0:64], lw[d][:, cs], 0.0, ALU.mult, ALU.add),
                     [onesb[:], lw[d][:]], [Lc[d][:]])
            for c in range(nch):
                P.copy(Lend[d][:, c:c + 1], Lc[d][:, c * 64 + 63:c * 64 + 64])
            if d == 1:
                P.tt(Lt[:, 0:n], lw[d][:, 0:n], Lc[d][:, 0:n], ALU.subtract)
                for c in range(nch):
                    cs = slice(c * 64, (c + 1) * 64)
                    P.ts(Lc[d][:, cs], Lt[:, cs], Lend[d][:, c:c + 1], None, op0=ALU.add)
            P.act(E1[:, 0:n], Lc[d][:, 0:n], AF.Exp)
            P.tt(Lt[:, 0:n], Lc[d][:, 0:n], lw[d][:, 0:n], ALU.subtract)
            P.act(Eex[:, 0:n], Lt[:, 0:n], AF.Exp)
            P.act(Ein[:, 0:n], Lc[d][:, 0:n], AF.Exp, scale=-1.0)
            for c in range(nch):
                cs = slice(c * 64, (c + 1) * 64)
                P.act(Een[:, cs], Lc[d][:, cs], AF.Exp, bias=Lend[d][:, c:c + 1], scale=-1.0)
            P.act(gC[d][:, 0:nch], Lend[d][:, 0:nch], AF.Exp)
            for hh in range(2):
                hs = slice(hh * 64, (hh + 1) * 64)
                e_ = "vector" if hh == 0 else "gpsimd"
                P.tt(QR[d][hs, 0:nch, hh * 64:(hh + 1) * 64], v3(kk[hs, 0:n], n), v3(Eex[hs, 0:n], n), ALU.mult, eng=e_)
                P.tt(nAp[d][hs, 0:nch, hh, :], v3(nkka[d][hs, 0:n], n), v3(Ein[hs, 0:n], n), ALU.mult, eng=e_)
                P.tt(Ktp[d][hs, 0:nch, hh, :], v3(kd[d][hs, 0:n], n), v3(Ein[hs, 0:n], n), ALU.mult, eng=e_)
                P.tt(Aep[d][hs, 0:nch, hh, :], v3(nkka[d][hs, 0:n], n), v3(Een[hs, 0:n], n), ALU.mult, eng=e_)
                P.tt(Kep[d][hs, 0:nch, hh, :], v3(kd[d][hs, 0:n], n), v3(Een[hs, 0:n], n), ALU.mult, eng=e_)
                P.copy(Vp[d][hs, 0:nch, hh, :], v3(vS[hs, g0:g0 + n], n), eng=e_)
            P.tt(QR[d][:, 0:nch, 128:192], v3(r_[:, 0:n], n), v3(E1[:, 0:n], n), ALU.mult)

        def chunk_ops(d, c, g0, par):
            bA, bN, bT, bS = PS[d * 4 + 0], PS[d * 4 + 1], PS[d * 4 + 2], PS[d * 4 + 3]
            T_ = BD[d][par]
            Cc = CP[d][par]
            MS, MST, MI = cm[:, d, 0, :], cm[:, d, 1, :], cm[:, d, 2, 0:64]
            qr = QR[d][:, c, :]
            kkp = QR[d][:, c, 0:128]
            rt = QR[d][:, c, 128:192]
            flat = lambda t_: t_[d][:, c, :, :].rearrange("p h s -> p (h s)")
            off = []
            off.append(lambda: P.mm(bA[:, 0:192], flat(nAp), qr))
            off.append(lambda: P.mm(bA[:, 192:384], flat(Ktp), qr))
            off.append(lambda: P.mm(bA[:, 384:512], kkp, flat(nAp)))
            off.append(lambda: P.tt(T_["XT"][:], bA[:, 0:128], MS, ALU.mult))
            off.append(lambda: P.tt(Cc["Brna"][:], bA[:, 128:192], MI, ALU.mult))
            off.append(lambda: P.tt(T_["AkT"][:], bA[:, 192:320], MS, ALU.mult))
            off.append(lambda: P.tt(Cc["Brk"][:], bA[:, 320:384], MI, ALU.mult))
            off.append(lambda: P.tt(T_["X"][:], bA[:, 384:512], MST, ALU.mult))
            off.append(lambda: P.tt(T_["Pm"][:], T_["XT"][:], ident[:], ALU.add, eng="gpsimd"))
            cur = ("X", "XT")
            nxt = ("Xb", "XTb")
            for lv in range(5):
                xc, xtc = cur
                xn, xtn = nxt
                off.append(lambda xc=xc, xtc=xtc: P.mm(bN[:, 0:128], T_[xtc][:], T_[xc][:]))
                if lv < 4:
                    off.append(lambda xc=xc, xtc=xtc: P.mm(bN[:, 128:256], T_[xc][:], T_[xtc][:]))
                off.append(lambda xn=xn: P.copy(T_[xn][:], bN[:, 0:128], eng="vector"))
                if lv < 4:
                    off.append(lambda xtn=xtn: P.copy(T_[xtn][:], bN[:, 128:256], eng="vector"))
                off.append(lambda xn=xn: P.mm(bN[:, 256:384], T_[xn][:], T_["Pm"][:]))
                off.append(lambda: P.tt(T_["Pm"][:], T_["Pm"][:], bN[:, 256:384], ALU.add))
                cur, nxt = nxt, cur
            off.append(lambda: P.tr(bT[:, 0:128], flat(Aep), ident[:]))
            off.append(lambda: P.tr(bT[:, 128:256], flat(Kep), ident[:]))
            off.append(lambda: P.tr(bT[:, 256:384], flat(Vp), ident[:]))
            off.append(lambda: P.copy(T_["AeT"][:], bT[:, 0:128], eng="vector"))
            off.append(lambda: P.copy(T_["KeT"][:], bT[:, 128:256], eng="vector"))
            off.append(lambda: P.copy(T_["Vbd"][:], bT[:, 256:384], eng="vector"))
            off.append(lambda: P.copy(Cc["Vc"][0:64, :], bT[0:64, 256:320]))
            off.append(lambda: P.copy(Cc["Vc"][64:128, :], bT[64:128, 320:384]))
            off.append(lambda: P.ts(T_["dg"][:], ident[:], gC[d][:, c:c + 1], None, op0=ALU.mult, eng="gpsimd"))
            on = []
            on.append(lambda: P.mm(bS[:, 0:64], kkp, Hc[d][:], start=True, stop=False))
            on.append(lambda: P.mm(bS[:, 0:64], T_["AkT"][:], Cc["Vc"][:], start=False, stop=True))
            on.append(lambda: P.copy(RHc[d][:], bS[:, 0:64], eng="vector"))
            on.append(lambda: P.mm(bS[:, 64:128], T_["Pm"][:], RHc[d][:]))
            on.append(lambda: P.copy(Uc[d][:], bS[:, 64:128], eng="vector"))
            on.append(lambda: P.copy(Ubd[d][0:64, 0:64], bS[0:64, 64:128]))
            on.append(lambda: P.copy(Ubd[d][64:128, 64:128], bS[64:128, 64:128]))
            on.append(lambda: P.mm(bS[:, 192:256], Hbd[d][:], rt, start=True, stop=False))
            on.append(lambda: P.mm(bS[:, 192:256], Ubd[d][:], Cc["Brna"][:], start=False, stop=False))
            on.append(lambda: P.mm(bS[:, 192:256], T_["Vbd"][:], Cc["Brk"][:], start=False, stop=True))
            on.append(lambda: P.mm(bS[:, 128:192], T_["dg"][:], Hc[d][:], start=True, stop=False))
            on.append(lambda: P.mm(bS[:, 128:192], T_["AeT"][:], Uc[d][:], start=False, stop=False))
            on.append(lambda: P.mm(bS[:, 128:192], T_["KeT"][:], Cc["Vc"][:], start=False, stop=True))
            on.append(lambda: P.copy(Od[d][:, g0 + c * 64:g0 + (c + 1) * 64], bS[:, 192:256], eng="vector"))
            on.append(lambda: P.copy(Hc[d][:], bS[:, 128:192], eng="vector"))
            on.append(lambda: P.copy(Hbd[d][0:64, 0:64], bS[0:64, 128:192]))
            on.append(lambda: P.copy(Hbd[d][64:128, 64:128], bS[64:128, 128:192]))
            return off, on

        def merge_run(lists):
            its = [list(l) for l in lists]
            m = max(len(l) for l in its) if its else 0
            for i in range(m):
                for l in its:
                    if i < len(l):
                        l[i]()

        def blocks_of(d):
            out = []
            for (sname, s0, L, zz) in [seqs[1], seqs[0]]:
                bl = list(range(0, L, NB))
                if d == 1:
                    bl = bl[::-1]
                out += [(s0, o, zz) for o in bl]
            return out

        par = [0, 0]
        for (bf, bb) in zip(blocks_of(0), blocks_of(1)):
            prep_block(bf[0], bf[1], NB, bf[2], 0)
            prep_block(bb[0], bb[1], NB, bb[2], 1)
            corder = [list(range(NCH)), list(range(NCH))[::-1]]
            g0s = [bf[0] + bf[1], bb[0] + bb[1]]
            pend_on = [[], []]
            for step in range(NCH + 1):
                lists = []
                for d in range(2):
                    if step < NCH and CHD >= 1:
                        off, on = chunk_ops(d, corder[d][step], g0s[d], par[d])
                        par[d] ^= 1
                        if CHD == 1:
                            on = []
                        if CHD == 5:
                            on = on[:3]
                        if CHD == 6:
                            on = on[:7]
                        if CHD == 7:
                            on = on[:10]
                        if CHD == 8:
                            on = on[:13]
                        if CHD >= 14 and CHD <= 16:
                            on = on[:CHD]
                        if CHD == 3:
                            off = off[:9]
                        if CHD == 4:
                            off = off[:9 + 28]
                    else:
                        off, on = [], []
                    lists.append(off)
                    lists.append(pend_on[d])
                    pend_on[d] = on
                merge_run(lists)
        P.barrier()
        ph4.close()
        ph4 = ExitStack()
        sb2 = lambda n, s, d: ph4.enter_context(nc.sbuf_tensor(n, s, d))
        ksq = sb2("ksq2", [128, 512], F32)
        rn = sb2("rn2", [128, 512], F32)
        bonb = sb2("bonb2", [128, 512], F32)
        gtb = sb2("gtb2", [128, 512], F32)
        wk = sb2("wk", [128, 512], F32)
        cen = sb2("cen", [128, 512], F32)
        rob = [sb2("rob%d" % i, [128, 512], BF16) for i in range(2)]
        bi = 0
        for o in range(0, TS, 512):
            n = min(512, TS - o)
            P.tt(wk[:, 0:n], Od[0][:, o:o + n], Od[1][:, o:o + n], ALU.add)
            pm = nps()
            P.mm(pm[:, 0:n], bones[:], wk[:, 0:n])
            P.stt(cen[:, 0:n], pm[:, 0:n], -1.0 / 64.0, wk[:, 0:n], ALU.mult, ALU.add)
            P.act(ksq[:, 0:n], cen[:, 0:n], AF.Square)
            pv = nps()
            P.mm(pv[:, 0:n], bones[:], ksq[:, 0:n])
            P.ts(rn[:, 0:n], pv[:, 0:n], 1.0 / 64.0, GN_EPS, op0=ALU.mult, op1=ALU.add)
            P.recip(rn[:, 0:n], rn[:, 0:n])
            P.act(rn[:, 0:n], rn[:, 0:n], AF.Sqrt)
            P.tt(cen[:, 0:n], cen[:, 0:n], rn[:, 0:n], ALU.mult)
            P.ts(cen[:, 0:n], cen[:, 0:n], pch[:, 13:14], pch[:, 14:15], op0=ALU.mult, op1=ALU.add)
            P.dma(bonb[:, 0:n], Sub(bon_d[:, o:o + n], None))
            P.dma(gtb[:, 0:n], Sub(gate_d[:, o:o + n], None))
            P.tt(cen[:, 0:n], cen[:, 0:n], bonb[:, 0:n], ALU.add)
            ob = rob[bi % 2]
            bi += 1
            P.tt(ob[:, 0:n], cen[:, 0:n], gtb[:, 0:n], ALU.mult)
            P.dma(brT_o[1, :, o:o + n], ob[:, 0:n], is_out=True)
        P.barrier()
        ph4.close()
        ph.close()
        P.barrier()
        P.finish()
    return nc


def seq_inputs(cfg, W, li, g, hTs):
    gs = slice(g * 128, (g + 1) * 128)
    w_in = W["w_in"][li]
    zr0 = OFF_RWKV
    cols_fm = np.concatenate([
        np.arange(zr0 + g * 128, zr0 + g * 128 + 128), np.arange(zr0 + 512 + g * 128, zr0 + 512 + g * 128 + 128),
        np.arange(zr0 + 1024 + g * 128, zr0 + 1024 + g * 128 + 128), np.arange(zr0 + 1536, zr0 + 1760),
        np.arange(OFF_Q + g * 128, OFF_Q + g * 128 + 128), np.arange(OFF_K + g * 128, OFF_K + g * 128 + 128)])
    cols_tm = np.concatenate([np.arange(g * 128, g * 128 + 128), np.arange(OFF_V + g * 128, OFF_V + g * 128 + 128)])
    mu = W["shift_mu"][li]
    pch = np.zeros((128, 18), np.float32)
    pch[:, 0] = mu[0, gs]; pch[:, 1] = mu[1, gs]
    pch[:, 2] = mu[0, 512 + g * 128:512 + g * 128 + 128]; pch[:, 3] = mu[1, 512 + g * 128:512 + g * 128 + 128]
    pch[:, 4] = mu[0, 1024 + g * 128:1024 + g * 128 + 128]; pch[:, 5] = mu[1, 1024 + g * 128:1024 + g * 128 + 128]
    pch[:, 6] = W["decay_w0"][li][0, gs]; pch[:, 7] = W["decay_w0"][li][1, gs]
    pch[:, 8] = W["iclr_a0"][li][0, gs]; pch[:, 9] = W["iclr_a0"][li][1, gs]
    pch[:, 10] = W["key_kk"][li][gs]; pch[:, 11] = W["key_ka"][li][gs]
    pch[:, 12] = W["bonus_rk"][li].reshape(-1)[gs]
    pch[:, 13] = W["gn_g"][li][gs]; pch[:, 14] = W["gn_b"][li][gs]
    pch[:, 15] = W["pool_scale"][li][gs]
    pch[:, 16] = np.tile(W["nat_qn_g"][li], 2); pch[:, 17] = np.tile(W["nat_kn_g"][li], 2)
    plo = np.stack([mu[0, 1536:1600], mu[1, 1536:1600], mu[0, 1600:1664], mu[1, 1600:1664]], axis=1)
    pgl = np.stack([mu[0, 1664:1760], mu[1, 1664:1760]], axis=1)
    dw2 = W["decay_w2"][li][:, :, gs].reshape(64, 128)
    aw2 = W["iclr_a2"][li][:, :, gs].reshape(64, 128)
    gg2 = W["gate_g2"][li][:, gs]
    plan, tiles = nat_geometry(cfg.T, cfg.GW)
    rpb = W["nat_rpb"][li]
    nbias = np.stack([np.stack([rpb[2 * g + hh][ro, co] * valid for hh in range(2)]) for (valid, ro, co) in tiles]).astype(np.float32)
    nmask = np.stack([valid.astype(np.float32) for (valid, ro, co) in tiles])
    bones = np.kron(np.eye(2, dtype=np.float32), np.ones((64, 64), np.float32))
    hsel = np.kron(np.eye(2, dtype=np.float32), np.ones((1, 64), np.float32))
    opad = np.zeros((2, 128, 128), np.float32)
    opad[0, :, 0:64] = 1.0
    opad[1, :, 64:128] = 1.0
    tri = np.triu(np.ones((64, 64), np.float32), 1)
    tri_i = np.triu(np.ones((64, 64), np.float32), 0)
    cmask = np.zeros((2, 3, 128, 128), np.float32)
    I2 = np.eye(2, dtype=np.float32)
    for d_, (ms, mi) in enumerate([(tri, tri_i), (tri.T, tri_i.T)]):
        cmask[d_, 0] = np.kron(I2, ms)
        cmask[d_, 1] = np.kron(I2, ms.T)
        cmask[d_, 2, :, 0:64] = np.tile(mi, (2, 1))
    f = lambda a: np.ascontiguousarray(a, dtype=np.float32)
    return dict(cmask=cmask, hTs=hTs, w_fm=f(w_in[:, cols_fm]), w_tm=f(w_in[:, cols_tm]), pch=pch, plo=f(plo), pgl=f(pgl), dw2=f(dw2), aw2=f(aw2),
                gg2=f(gg2), poolw=f(W["pool_w"][li][g]), bands=pool_bands(POOL_WINDOWS[g]), nbias=nbias, nmask=nmask,
                ident=np.eye(128, dtype=np.float32), bones=bones, hsel=hsel, opad=opad)


import ml_dtypes

_BF = ml_dtypes.bfloat16
_PROGS = {}


def _prog(key, fn):
    if key not in _PROGS:
        _PROGS[key] = fn()
    return _PROGS[key]


def _run(nc, in_maps):
    res = run_bass_kernel_spmd(nc, in_maps, core_ids=list(range(8)))
    return res.results


def kernel(**inputs):
    W = {k: np.asarray(v) for k, v in inputs.items()}
    x = W["x"]
    ctx = W["ctx"]
    Bsz, T, D = x.shape
    CTX = ctx.shape[1]
    DFF = W["ffn_w1"].shape[2]
    NE = W["router"].shape[2]
    depth = W["w_mod"].shape[0]
    cfg = Cfg(T=T, CTX=CTX, DFF=DFF, NE=NE)
    TL, CL, NTOK, TS = cfg.TL, cfg.CL, cfg.NTOK, cfg.TS
    f32 = lambda a: np.ascontiguousarray(a, dtype=np.float32)
    ones = np.ones((128, 128), np.float32)
    ident = np.eye(128, dtype=np.float32)
    cores = [(b, j) for b in range(2) for j in range(4)]
    xT = []
    for (b, j) in cores:
        a = np.zeros((D, NTOK), np.float32)
        a[:, :TL] = x[b, j * TL:(j + 1) * TL].T
        a[:, TL:TL + CL] = ctx[b, j * CL:(j + 1) * CL].T
        xT.append(a)
    cvecs = [f32(np.stack([W["c"][b], W["c_ctx"]], axis=1)) for b in range(2)]
    ncA = _prog(("A", T, CTX), lambda: build_tok(cfg, "A"))
    ncS = _prog(("S", T, CTX), lambda: build_seq(cfg))
    for li in range(depth):
        moe = (li % 2 == 1)
        jj = li // 2
        wm = W["w_mod"][li]
        bm = W["b_mod"][li]
        wmA = f32(wm[:, :2 * D])
        bmA = f32(bm[:2 * D])
        gmix = f32(W["norm_mix_g"][li])
        mapsA = [dict(xT=xT[c], cvec=cvecs[b], w_mod=wmA, b_mod=bmA, g_mix=gmix, ones=ones, ident=ident) for c, (b, j) in enumerate(cores)]
        rA = _run(ncA, mapsA)
        hTs = []
        for b in range(2):
            a = np.zeros((D, TS), _BF)
            for j in range(4):
                h = rA[b * 4 + j]["hT"]
                a[:, j * TL:(j + 1) * TL] = h[:, :TL]
                a[:, T + j * CL:T + (j + 1) * CL] = h[:, TL:TL + CL]
            hTs.append(a)
        mapsS = [seq_inputs(cfg, W, li, g, hTs[b]) for (b, g) in cores]
        rS = _run(ncS, mapsS)
        brs = []
        for (b, j) in cores:
            a = np.zeros((3, 512, NTOK), _BF)
            for g in range(4):
                o = rS[b * 4 + g]["brTo"]
                a[:, g * 128:(g + 1) * 128, :TL] = o[:, :, j * TL:(j + 1) * TL]
                a[:, g * 128:(g + 1) * 128, TL:TL + CL] = o[:, :, T + j * CL:T + (j + 1) * CL]
            brs.append(a)
        ncB = _prog(("B", T, CTX, DFF, moe), lambda: build_tok(cfg, "B", moe=moe))
        common = dict(w_mod=f32(wm), b_mod=f32(bm), g_mix=gmix, g_ffn=f32(W["norm_ffn_g"][li]),
                      w_gate=f32(W["w_in"][li][:, OFF_GATE:]), w_branch=f32(W["w_branch"][li]), w_out=f32(W["w_out"][li]),
                      ones=ones, ident=ident)
        if moe:
            common.update(router=f32(W["router"][jj]), w1=f32(W["moe_w1"][jj]), w3=f32(W["moe_w3"][jj]), w2=f32(W["moe_w2"][jj]))
        else:
            common.update(w1=f32(W["ffn_w1"][jj][None]), w3=f32(W["ffn_w3"][jj][None]), w2=f32(W["ffn_w2"][jj][None]))
        mapsB = [dict(xT=xT[c], cvec=cvecs[b], brT=brs[c], **common) for c, (b, j) in enumerate(cores)]
        rB = _run(ncB, mapsB)
        xT = [f32(rB[c]["xo"]) for c in range(8)]
    out = np.zeros((Bsz, T, D), np.float32)
    for c, (b, j) in enumerate(cores):
        out[b, j * TL:(j + 1) * TL] = xT[c][:, :TL].T
    return out
```
